# Optimizing a Trainium2 kernel written in Bass

```python
import jax, jax.numpy as jnp
from jax import lax
import numpy as np

D_MODEL = 2048
BATCH = 4
SEQ = 4096
DEPTH = 1

HEAD_DIM = 128
H_A = 8
KV_A = 2
G_A = H_A // KV_A
H_B = 8
KV_B = 2
G_B = H_B // KV_B
MIX_WIDTH = (H_A + H_B) * HEAD_DIM
WINDOW = 128
BLOCK = 128
GRID_W = 64
ROPE_THETA = 10000.0
N_EXPERTS = 16
CAPACITY_FACTOR = 2
D_FF = 2 * D_MODEL
EPS = 1e-6
NEG_INF = -1e30
SCALE = HEAD_DIM ** -0.5
COLS = [H_A * HEAD_DIM, KV_A * HEAD_DIM, KV_A * HEAD_DIM,
        H_B * HEAD_DIM, KV_B * HEAD_DIM, KV_B * HEAD_DIM]
D_IN = sum(COLS)
SPLITS = [int(v) for v in np.cumsum(COLS)[:-1]]

kernel_name = "hymba_window_sink_axialrope_ec_moe"


def rms_norm(x, g):
    xf = x.astype(jnp.float32)
    y = xf * lax.rsqrt(jnp.mean(xf * xf, axis=-1, keepdims=True) + EPS)
    return (y * g.astype(jnp.float32)).astype(x.dtype)


def alibi_slopes(n):
    return jnp.asarray(2.0 ** (-8.0 * np.arange(1, n + 1) / n), dtype=jnp.float32)


def windowed_sink_attention(q, k, v, sink):
    B, S = q.shape[0], q.shape[1]
    nb = S // BLOCK
    qb = q.reshape(B, nb, BLOCK, KV_A, G_A, HEAD_DIM)

    def band(t):
        tp = jnp.pad(t, ((0, 0), (BLOCK, BLOCK), (0, 0), (0, 0)))
        tp = tp.reshape(B, nb + 2, BLOCK, KV_A, HEAD_DIM)
        return jnp.concatenate([tp[:, :-2], tp[:, 1:-1], tp[:, 2:]], axis=2)

    kw, vw = band(k), band(v)
    s = jnp.einsum('bnqkgd,bnjkd->bnkgqj', qb, kw,
                   preferred_element_type=jnp.float32) * SCALE
    qi = jnp.arange(BLOCK)
    kj = jnp.arange(3 * BLOCK)
    dist = qi[:, None] + BLOCK - kj[None, :]
    kpos = jnp.arange(nb)[:, None] * BLOCK - BLOCK + kj[None, :]
    valid = (jnp.abs(dist) <= WINDOW)[None] & ((kpos >= 0) & (kpos < S))[:, None, :]
    slopes = alibi_slopes(H_A).reshape(KV_A, G_A)
    bias = -slopes[:, :, None, None] * jnp.abs(dist).astype(jnp.float32)[None, None]
    s = jnp.where(valid[None, :, None, None], s + bias[None, None], NEG_INF)
    sinkc = jnp.broadcast_to(sink.astype(jnp.float32).reshape(KV_A, G_A, 1, 1),
                             s.shape[:-1] + (1,))
    p = jax.nn.softmax(jnp.concatenate([s, sinkc], axis=-1), axis=-1)[..., :-1]
    o = jnp.einsum('bnkgqj,bnjkd->bnqkgd', p.astype(v.dtype), vw)
    return o.reshape(B, S, H_A * HEAD_DIM)


def axial_rope(t, row, col):
    half = HEAD_DIM // 2
    inv_freq = ROPE_THETA ** (-jnp.arange(0, half, 2, dtype=jnp.float32) / half)

    def rotate(seg, pos):
        ang = pos.astype(jnp.float32)[:, None] * inv_freq[None, :]
        cos = jnp.cos(ang)[None, :, None, :]
        sin = jnp.sin(ang)[None, :, None, :]
        s1, s2 = jnp.split(seg.astype(jnp.float32), 2, axis=-1)
        return jnp.concatenate([s1 * cos - s2 * sin, s2 * cos + s1 * sin], axis=-1)

    out = jnp.concatenate([rotate(t[..., :half], row), rotate(t[..., half:], col)], axis=-1)
    return out.astype(t.dtype)


def grid_attention(q, k, v):
    B, S = q.shape[0], q.shape[1]
    nb = S // BLOCK
    qb = jnp.moveaxis(q.reshape(B, nb, BLOCK, KV_B, G_B, HEAD_DIM), 1, 0)

    def one_block(qblk):
        s = jnp.einsum('bqkgd,bskd->bkgqs', qblk, k,
                       preferred_element_type=jnp.float32) * SCALE
        p = jax.nn.softmax(s, axis=-1)
        return jnp.einsum('bkgqs,bskd->bqkgd', p.astype(v.dtype), v)

    o = lax.map(one_block, qb)
    return jnp.moveaxis(o, 0, 1).reshape(B, S, H_B * HEAD_DIM)


def expert_choice_ffn(h, w_router, w_gate, w_up, w_down):
    B, S, _ = h.shape
    cap = CAPACITY_FACTOR * S // N_EXPERTS
    logits = jnp.einsum('bsd,de->bse', h, w_router, preferred_element_type=jnp.float32)
    aff = jax.nn.softmax(logits, axis=-1)
    gates, idx = lax.top_k(jnp.swapaxes(aff, 1, 2), cap)
    b_idx = jnp.arange(B)[:, None, None]
    xg = h[b_idx, idx]
    a = jnp.einsum('becd,edf->becf', xg, w_gate)
    u = jnp.einsum('becd,edf->becf', xg, w_up)
    eo = jnp.einsum('becf,efd->becd', jax.nn.silu(a) * u, w_down)
    eo = eo * gates[..., None].astype(eo.dtype)
    return jnp.zeros_like(h).at[b_idx, idx].add(eo)


def hybrid_layer(x, row, col, norm_mix, w_in, sink_a, q_norm_b, k_norm_b, w_out,
                 norm_ffn, w_router, w_gate, w_up, w_down):
    B, S, _ = x.shape
    h = rms_norm(x, norm_mix)
    proj = jnp.einsum('bsd,de->bse', h, w_in)
    qa, ka, va, qb, kb, vb = jnp.split(proj, SPLITS, axis=-1)
    qa = qa.reshape(B, S, H_A, HEAD_DIM)
    ka = ka.reshape(B, S, KV_A, HEAD_DIM)
    va = va.reshape(B, S, KV_A, HEAD_DIM)
    qb = axial_rope(rms_norm(qb.reshape(B, S, H_B, HEAD_DIM), q_norm_b), row, col)
    kb = axial_rope(rms_norm(kb.reshape(B, S, KV_B, HEAD_DIM), k_norm_b), row, col)
    vb = vb.reshape(B, S, KV_B, HEAD_DIM)
    oa = windowed_sink_attention(qa, ka, va, sink_a)
    ob = grid_attention(qb, kb, vb)
    mix = jnp.einsum('bse,ed->bsd', jnp.concatenate([oa, ob], axis=-1), w_out)
    x = x + mix
    x = x + expert_choice_ffn(rms_norm(x, norm_ffn), w_router, w_gate, w_up, w_down)
    return x


def setup_inputs(seed: int = 0) -> dict:
    key = jax.random.key(seed)
    ks = jax.random.split(key, 13)
    f32 = jnp.float32
    L = DEPTH
    return {
        'x': jax.random.normal(ks[0], (BATCH, SEQ, D_MODEL), f32),
        'norm_mix': 1.0 + 0.02 * jax.random.normal(ks[1], (L, D_MODEL), f32),
        'w_in': jax.random.normal(ks[2], (L, D_MODEL, D_IN), f32) * D_MODEL ** -0.5,
        'sink_a': 0.5 * jax.random.normal(ks[3], (L, H_A), f32),
        'q_norm_b': 1.0 + 0.02 * jax.random.normal(ks[4], (L, HEAD_DIM), f32),
        'k_norm_b': 1.0 + 0.02 * jax.random.normal(ks[5], (L, HEAD_DIM), f32),
        'w_out': jax.random.normal(ks[6], (L, MIX_WIDTH, D_MODEL), f32) * MIX_WIDTH ** -0.5,
        'norm_ffn': 1.0 + 0.02 * jax.random.normal(ks[7], (L, D_MODEL), f32),
        'w_router': jax.random.normal(ks[8], (L, D_MODEL, N_EXPERTS), f32) * D_MODEL ** -0.5,
        'w_gate': jax.random.normal(ks[9], (L, N_EXPERTS, D_MODEL, D_FF), f32) * D_MODEL ** -0.5,
        'w_up': jax.random.normal(ks[10], (L, N_EXPERTS, D_MODEL, D_FF), f32) * D_MODEL ** -0.5,
        'w_down': jax.random.normal(ks[11], (L, N_EXPERTS, D_FF, D_MODEL), f32) * D_FF ** -0.5,
        'norm_final': 1.0 + 0.02 * jax.random.normal(ks[12], (D_MODEL,), f32),
    }


def reference(x, norm_mix, w_in, sink_a, q_norm_b, k_norm_b, w_out, norm_ffn,
              w_router, w_gate, w_up, w_down, norm_final):
    S = x.shape[1]
    rows = S // GRID_W
    row = jnp.broadcast_to(jnp.arange(rows)[:, None], (rows, GRID_W)).reshape(S)
    col = jnp.broadcast_to(jnp.arange(GRID_W)[None, :], (rows, GRID_W)).reshape(S)
    for l in range(DEPTH):
        x = hybrid_layer(x, row, col, norm_mix[l], w_in[l], sink_a[l], q_norm_b[l],
                         k_norm_b[l], w_out[l], norm_ffn[l], w_router[l], w_gate[l],
                         w_up[l], w_down[l])
    return rms_norm(x, norm_final)
```

```python
import numpy as np
import ml_dtypes
from contextlib import ExitStack
import concourse.bass as bass
import concourse.mybir as mybir
from concourse.bass_utils import run_bass_kernel_spmd

F32 = mybir.dt.float32
BF16 = mybir.dt.bfloat16
I32 = mybir.dt.int32
U32 = mybir.dt.uint32
AF = mybir.ActivationFunctionType
ALU = mybir.AluOpType
AX = mybir.AxisListType

D = 2048
S = 4096
NT = S // 128
HD = 128
DIN = 3072
NE = 16
CAP = 512
DFF = 4096
EPS = 1e-6
SCALE = HD ** -0.5
N_CORES = 8
DBG = {}


class Res:
    __slots__ = ("w", "r", "const", "name")

    def __init__(self, name, const=False):
        self.w = None
        self.r = {}
        self.const = const
        self.name = name


class Ctx:
    def __init__(self, nc, es):
        self.nc = nc
        self.eng = dict(pe=nc.tensor, dve=nc.vector, act=nc.scalar, pool=nc.gpsimd, sp=nc.sync)
        self.semobj = {}
        for k in self.eng:
            self.semobj[k] = es.enter_context(nc.semaphore("s_" + k))
        self.cnt = {k: 0 for k in self.eng}
        self.known = {k: {} for k in self.eng}
        self.dq = {"sp": 14, "pool": 10, "act": 8}
        self.dval = {}
        self.drr = {}
        for q, n in self.dq.items():
            self.dval[q] = [0] * n
            self.drr[q] = 0
            for i in range(n):
                self.semobj[(q, i)] = es.enter_context(nc.semaphore("d_%s%d" % (q, i)))

    def wait(self, e, tok):
        if tok is None:
            return
        s, v, prod = tok
        if prod == "pe" and e == "pe":
            return
        kn = self.known[e]
        if kn.get(s, 0) >= v:
            return
        self.eng[e].wait_ge(self.semobj[s], v)
        kn[s] = v

    def deps(self, e, reads, writes):
        for r in reads:
            self.wait(e, r.w)
        for w in writes:
            self.wait(e, w.w)
            for s, (v, prod) in w.r.items():
                self.wait(e, (s, v, prod))

    def commit(self, tok, reads, writes):
        s, v, prod = tok
        for r in reads:
            if not r.const:
                r.r[s] = (v, prod)
        for w in writes:
            w.w = tok
            w.r = {}

    def op(self, e, fn, reads=(), writes=()):
        self.deps(e, reads, writes)
        ins = fn(self.eng[e])
        self.cnt[e] += 1
        ins.then_inc(self.semobj[e], 1)
        tok = (e, self.cnt[e], e)
        self.known[e][e] = max(self.known[e].get(e, 0), 0)
        self.commit(tok, reads, writes)
        return tok

    def dma(self, q, fn, reads=(), writes=()):
        n = self.dq[q]
        i = self.drr[q]
        self.drr[q] = (i + 1) % n
        key = (q, i)
        prev = self.dval[q][i]
        if prev:
            self.wait(q, (key, prev, "dma"))
        self.deps(q, reads, writes)
        ins = fn(self.eng[q])
        v = prev + 16
        ins.then_inc(self.semobj[key], 16)
        self.dval[q][i] = v
        tok = (key, v, "dma")
        self.commit(tok, reads, writes)
        return tok

    def barrier(self):
        toks = [(k, self.cnt[k], k) for k in self.eng if self.cnt[k] > 0]
        for q, n in self.dq.items():
            for i in range(n):
                if self.dval[q][i]:
                    toks.append(((q, i), self.dval[q][i], "dma"))
        for e in self.eng:
            for t in toks:
                if t[0] == e and e == "pe":
                    continue
                self.wait(e, t)


def build_nc(debug=False, stop=99, n_exp=NE):
    nc = bass.Bass("TRN2", target_bir_lowering=False)
    skind = dict(kind="ExternalOutput") if debug else {}

    def din(name, shape, dt=F32):
        return nc.dram_tensor(name, list(shape), dt, kind="ExternalInput")

    x = din("x", [S, D])
    w_in = din("w_in", [D, DIN])
    w_out = din("w_out", [D, D])
    w_router = din("w_router", [D, NE])
    NEW = max(n_exp, 1) if debug else NE
    w_gate = din("w_gate", [NEW, D, DFF])
    w_up = din("w_up", [NEW, D, DFF])
    w_down = din("w_down", [NEW, DFF, D])
    g_mix = din("g_mix", [128, D])
    g_ffn = din("g_ffn", [128, D])
    g_fin = din("g_fin", [128, D])
    sink_b = din("sink_b", [128, 8])
    gq_col = din("gq_col", [128, 1])
    gk_col = din("gk_col", [128, 1])
    c_identb = din("c_identb", [128, 128], BF16)
    c_identf = din("c_identf", [128, 128])
    c_rotT = din("c_rotT", [128, 128])
    c_ones = din("c_ones", [128, 128])
    c_cos = din("c_cos", [128, S])
    c_sin = din("c_sin", [128, S])
    c_bias = din("c_bias", [128, 8 * 384])
    c_iota = din("c_iota", [128, CAP])
    c_tokid = din("c_tokid", [128, NT * 2], BF16)
    out = nc.dram_tensor("out", [S, D], F32, kind="ExternalOutput")

    QKT = nc.dram_tensor("QKT", [20, 128, S], BF16, **skind)
    VA = nc.dram_tensor("VA", [S, 256], BF16, **skind)
    VB = nc.dram_tensor("VB", [S, 256], BF16, **skind)
    OS = nc.dram_tensor("OS", [S, D], BF16, **skind)
    Y = nc.dram_tensor("Y", [S, D], F32, **skind)
    H2W = D + NE
    H2 = nc.dram_tensor("H2", [S, H2W], F32, **skind)
    if debug:
        dbg_pm = nc.dram_tensor("dbg_pm", [128, NT * NE], F32, kind="ExternalOutput")
        dbg_idx = nc.dram_tensor("dbg_idx", [128, 4], I32, kind="ExternalOutput")
        dbg_negmb = nc.dram_tensor("dbg_negmb", [128, 1], F32, kind="ExternalOutput")

    with ExitStack() as es:
        cx = Ctx(nc, es)

        def sb(st, name, shape, dt=F32):
            return st.enter_context(nc.sbuf_tensor(name, list(shape), dt))

        PS = [es.enter_context(nc.psum_tensor("ps%d" % i, [128, 512], F32)) for i in range(6)]
        PT = [es.enter_context(nc.psum_tensor("pt%d" % i, [128, 512], BF16)) for i in range(2)]
        rPS = [Res("ps%d" % i) for i in range(6)]
        rPT = [Res("pt%d" % i) for i in range(2)]

        identb = sb(es, "identb", [128, 128], BF16)
        identf = sb(es, "identf", [128, 128])
        ones = sb(es, "ones", [128, 128])
        sinkb = sb(es, "sinkb", [128, 8])
        rC = Res("consts")
        cx.dma("sp", lambda e: e.dma_start(out=identb[:], in_=c_identb[:, :]), writes=[rC])
        cx.dma("sp", lambda e: e.dma_start(out=identf[:], in_=c_identf[:, :]), writes=[rC])
        cx.dma("sp", lambda e: e.dma_start(out=ones[:], in_=c_ones[:, :]), writes=[rC])
        cx.dma("sp", lambda e: e.dma_start(out=sinkb[:], in_=sink_b[:, :]), writes=[rC])
        cx.barrier()
        rC.const = True

        NSTG = 3
        NWB = 3
        wst = [sb(es, "wst%d" % i, [128, 4096]) for i in range(NSTG)]
        wbf = [sb(es, "wbf%d" % i, [128, 4096], BF16) for i in range(NWB)]
        rwst = [Res("wst%d" % i) for i in range(NSTG)]
        rwbf = [Res("wbf%d" % i) for i in range(NWB)]
        wstate = {"i": 0, "c": 0}
        cast_eng = ["act", "pool", "dve", "act", "dve", "pool", "act"]

        def wgroup_load(src_ap, k):
            i = wstate["i"] % NSTG
            wstate["i"] += 1
            dst = wst[i][:, :].rearrange("p (k c) -> p k c", k=k)
            cx.dma("sp", lambda e: e.dma_start(out=dst, in_=src_ap), writes=[rwst[i]])
            return i

        def wgroup_cast(i):
            j = wstate["c"] % NWB
            ce = cast_eng[wstate["c"] % len(cast_eng)]
            wstate["c"] += 1
            if ce == "act":
                cx.op("act", lambda e: e.activation(out=wbf[j][:], in_=wst[i][:], func=AF.Copy),
                      reads=[rwst[i]], writes=[rwbf[j]])
            else:
                cx.op(ce, lambda e: e.tensor_copy(wbf[j][:], wst[i][:]), reads=[rwst[i]], writes=[rwbf[j]])
            return j

        class WStream:
            def __init__(self, specs, depth=2):
                self.specs = specs
                self.n = len(specs)
                self.q = []
                self.pos = 0
                self.depth = depth
                for _ in range(min(depth, self.n)):
                    self._issue()

            def _issue(self):
                ap, k = self.specs[self.pos]
                self.pos += 1
                self.q.append((wgroup_load(ap, k), k))

            def next(self):
                i, k = self.q.pop(0)
                j = wgroup_cast(i)
                if self.pos < self.n:
                    self._issue()
                return wbf[j][:, :].rearrange("p (k c) -> p k c", k=k), rwbf[j]

        def rmsnorm_rstd(st_tiles, src, rsrc, junk, rjunk, name):
            ss, rss, std, rstd = st_tiles
            cx.op("act", lambda e: e.activation(out=junk, in_=src, func=AF.Square, accum_out=ss[:, 0:1]),
                  reads=[rsrc], writes=[rjunk, rss])
            cx.op("act", lambda e: e.activation(out=std[:, 0:1], in_=ss[:, 0:1], func=AF.Sqrt, scale=1.0 / D, bias=epsc[:, 0:1]),
                  reads=[rss], writes=[rss])
            cx.op("dve", lambda e: e.reciprocal(rstd[:, 0:1], std[:, 0:1]), reads=[rss], writes=[rss])

        epsc = sb(es, "epsc", [128, 1])
        eps128 = sb(es, "eps128", [128, 1])
        rE = Res("eps")
        cx.op("dve", lambda e: e.memset(epsc[:], EPS), writes=[rE])
        cx.op("dve", lambda e: e.memset(eps128[:], EPS), writes=[rE])
        cx.barrier()
        rE.const = True

        xv = x.ap().rearrange("(t p) d -> t p d", p=128)
        w_in_v = w_in.ap().rearrange("(k p) c -> p k c", p=128)
        w_out_v = w_out.ap().rearrange("(k p) c -> p k c", p=128)

        with ExitStack() as st:
          if stop >= 1:
            gm = sb(st, "gm", [128, D])
            cosT = sb(st, "cosT", [128, S])
            sinT = sb(st, "sinT", [128, S])
            rotT = sb(st, "rotT", [128, 128])
            gqc = sb(st, "gqc", [128, 1])
            gkc = sb(st, "gkc", [128, 1])
            rK = Res("p1c")
            for dst, src in ((gm, g_mix), (cosT, c_cos), (sinT, c_sin), (rotT, c_rotT), (gqc, gq_col), (gkc, gk_col)):
                cx.dma("sp", (lambda d_, s_: (lambda e: e.dma_start(out=d_[:], in_=s_[:, :])))(dst, src), writes=[rK])
            cx.barrier()
            rK.const = True
            xt = [sb(st, "xt%d" % i, [128, D]) for i in range(2)]
            rxt = [Res("xt%d" % i) for i in range(2)]
            hb = [sb(st, "hb%d" % i, [128, D], BF16) for i in range(2)]
            rhb = [Res("hb%d" % i) for i in range(2)]
            hT = [sb(st, "hT%d" % i, [128, 16, 512], BF16) for i in range(2)]
            rhT = [Res("hT%d" % i) for i in range(2)]
            ss = sb(st, "ss", [128, 1]); std = sb(st, "std", [128, 1]); rstd = sb(st, "rstd", [128, 1])
            rss = Res("ss")
            qf = sb(st, "qf", [128, 512]); rqf = Res("qf")
            sq = sb(st, "sq", [128, 512]); rsq = Res("sq")
            sd = sb(st, "sd", [128, 512]); rsd = Res("sd")
            rs_ = sb(st, "rs_", [128, 512]); rrs = Res("rs_")
            qn = sb(st, "qn", [128, 512]); rqn = Res("qn")
            t1 = sb(st, "t1", [128, 512]); rt1 = Res("t1")
            t2 = sb(st, "t2", [128, 512]); rt2 = Res("t2")
            ob = [sb(st, "ob%d" % i, [128, 512], BF16) for i in range(3)]
            rob = [Res("ob%d" % i) for i in range(3)]
            obi = 0
            tcount = 0
            for tc in range(DBG.get('p1_chunks', 8)):
                if DBG.get('p1_skipall'):
                    break
                hTc = hT[tc % 2]; rhTc = rhT[tc % 2]
                for t in range(4):
                    tg = tc * 4 + t
                    xi = tg % 2
                    cx.dma("sp", lambda e: e.dma_start(out=xt[xi][:], in_=xv[tg]), writes=[rxt[xi]])
                    rmsnorm_rstd((ss, rss, std, rstd), xt[xi][:], rxt[xi], hb[xi][:], rhb[xi], "x")
                    cx.op("dve", lambda e: e.scalar_tensor_tensor(out=hb[xi][:], in0=xt[xi][:], scalar=rstd[:, 0:1], in1=gm[:],
                                                                  op0=ALU.mult, op1=ALU.mult),
                          reads=[rxt[xi], rss], writes=[rhb[xi]])
                    for k4 in range(DBG.get('p1_k4', 4)):
                        pi = tcount % 2; tcount += 1
                        for kk in range(4):
                            k = k4 * 4 + kk
                            cx.op("pe", lambda e: e.transpose(PT[pi][:, kk * 128:(kk + 1) * 128], hb[xi][:, k * 128:(k + 1) * 128], identb[:]),
                                  reads=[rhb[xi]], writes=[rPT[pi]])
                        evac = "act" if pi == 0 else "dve"
                        dst = hTc[:, k4 * 4:(k4 + 1) * 4, t * 128:(t + 1) * 128]
                        srcp = PT[pi][:, :].rearrange("p (k c) -> p k c", k=4)
                        if evac == "dve":
                            cx.op("dve", lambda e: e.tensor_copy(dst, srcp), reads=[rPT[pi]], writes=[rhTc])
                        else:
                            cx.op("act", lambda e: e.activation(out=dst, in_=srcp, func=AF.Copy), reads=[rPT[pi]], writes=[rhTc])
                if DBG.get('p1_noproj'):
                    continue
                specs = [(w_in_v[:, :, cg * 256:(cg + 1) * 256], 16) for cg in range(12)]
                ws = WStream(specs)
                for cg in range(12):
                    wv, rw = ws.next()
                    if cg in (5, 11):
                        Vd = VA if cg == 5 else VB
                        for t in range(4):
                            pb = (t % 2)
                            for k in range(16):
                                cx.op("pe", lambda e: e.matmul(PS[pb][:, 0:256], hTc[:, k, t * 128:(t + 1) * 128], wv[:, k, :],
                                                                start=(k == 0), stop=(k == 15)),
                                      reads=[rhTc, rw], writes=[rPS[pb]])
                            oi = obi % 3; obi += 1
                            cx.op("act", lambda e: e.activation(out=ob[oi][:, 0:256], in_=PS[pb][:, 0:256], func=AF.Copy),
                                  reads=[rPS[pb]], writes=[rob[oi]])
                            r0 = (tc * 4 + t) * 128
                            cx.dma("pool", lambda e: e.dma_start(out=Vd[r0:r0 + 128, :], in_=ob[oi][:, 0:256]), reads=[rob[oi]])
                        continue
                    for half in range(2):
                        col = cg * 256 + half * 128
                        if col < 1024:
                            cc = col // 128; typ = "A"
                        elif col < 1280:
                            cc = 8 + (col - 1024) // 128; typ = "A"
                        elif col < 2560:
                            cc = 10 + (col - 1536) // 128; typ = "Bq"
                        else:
                            cc = 18 + (col - 2560) // 128; typ = "Bk"
                        pb = 2 + (half % 2)
                        for k in range(16):
                            cx.op("pe", lambda e: e.matmul(PS[pb][:, :], wv[:, k, half * 128:(half + 1) * 128], hTc[:, k, :],
                                                            start=(k == 0), stop=(k == 15)),
                                  reads=[rhTc, rw], writes=[rPS[pb]])
                        oi = obi % 3; obi += 1
                        if typ == "A" or DBG.get("p1_noB"):
                            cx.op("act", lambda e: e.activation(out=ob[oi][:], in_=PS[pb][:, :], func=AF.Copy),
                                  reads=[rPS[pb]], writes=[rob[oi]])
                        else:
                            gc = gqc if typ == "Bq" else gkc
                            cx.op("dve", lambda e: e.tensor_copy(qf[:], PS[pb][:, :]), reads=[rPS[pb]], writes=[rqf])
                            cx.op("pool", lambda e: e.tensor_tensor(out=sq[:], in0=qf[:], in1=qf[:], op=ALU.mult), reads=[rqf], writes=[rsq])
                            cx.op("pe", lambda e: e.matmul(PS[4][:, :], ones[:], sq[:], start=True, stop=True), reads=[rsq], writes=[rPS[4]])
                            cx.op("act", lambda e: e.activation(out=sd[:], in_=PS[4][:, :], func=AF.Sqrt, scale=1.0 / HD, bias=eps128[:, 0:1]),
                                  reads=[rPS[4]], writes=[rsd])
                            cx.op("dve", lambda e: e.reciprocal(rs_[:], sd[:]), reads=[rsd], writes=[rrs])
                            cx.op("dve", lambda e: e.scalar_tensor_tensor(out=qn[:], in0=qf[:], scalar=gc[:, 0:1], in1=rs_[:], op0=ALU.mult, op1=ALU.mult),
                                  reads=[rqf, rrs], writes=[rqn])
                            cx.op("pe", lambda e: e.matmul(PS[4][:, :], rotT[:], qn[:], start=True, stop=True), reads=[rqn], writes=[rPS[4]])
                            cx.op("pool", lambda e: e.tensor_tensor(out=t1[:], in0=qn[:], in1=cosT[:, tc * 512:(tc + 1) * 512], op=ALU.mult),
                                  reads=[rqn], writes=[rt1])
                            cx.op("dve", lambda e: e.tensor_tensor(out=t2[:], in0=PS[4][:, :], in1=sinT[:, tc * 512:(tc + 1) * 512], op=ALU.mult),
                                  reads=[rPS[4]], writes=[rt2])
                            cx.op("dve", lambda e: e.tensor_tensor(out=ob[oi][:], in0=t1[:], in1=t2[:], op=ALU.add),
                                  reads=[rt1, rt2], writes=[rob[oi]])
                        cx.dma("pool", lambda e: e.dma_start(out=QKT[cc, :, tc * 512:(tc + 1) * 512], in_=ob[oi][:]), reads=[rob[oi]])
            cx.barrier()

        with ExitStack() as st:
          if stop >= 2:
            biasA = sb(st, "biasA", [128, 8, 384])
            negMB = sb(st, "negMB", [128, 1])
            gqa = sb(st, "gqa", [128, 2]); gneg = sb(st, "gneg", [128, 2]); rowm = sb(st, "rowm", [2, 128]); mx = sb(st, "mx", [2, 1]); mprod = sb(st, "mprod", [1, 1])
            rK = Res("p2c")
            cx.dma("sp", lambda e: e.dma_start(out=biasA[:].rearrange("p h c -> p (h c)"), in_=c_bias[:, :]), writes=[rK])
            cx.dma("sp", lambda e: e.dma_start(out=gqa[:, 0:1], in_=gq_col[:, :]), writes=[rK])
            cx.dma("sp", lambda e: e.dma_start(out=gqa[:, 1:2], in_=gk_col[:, :]), writes=[rK])
            cx.op("dve", lambda e: e.tensor_scalar(out=gneg[:], in0=gqa[:], scalar1=-1.0, scalar2=None, op0=ALU.mult), reads=[rK], writes=[rK])
            cx.op("dve", lambda e: e.tensor_tensor(out=gqa[:], in0=gqa[:], in1=gneg[:], op=ALU.max), reads=[rK], writes=[rK])
            cx.op("pe", lambda e: e.transpose(PS[4][0:2, 0:128], gqa[:, 0:2], identf[:]), reads=[rK], writes=[rPS[4]])
            cx.op("dve", lambda e: e.tensor_copy(rowm[:], PS[4][0:2, 0:128]), reads=[rPS[4]], writes=[rK])
            cx.op("dve", lambda e: e.reduce_max(out=mx[:, 0:1], in_=rowm[:], axis=AX.X), reads=[rK], writes=[rK])
            cx.op("pe", lambda e: e.transpose(PS[4][0:1, 0:2], mx[0:2, 0:1], identf[0:2, 0:2]), reads=[rK], writes=[rPS[4]])
            cx.op("dve", lambda e: e.tensor_copy(rowm[0:1, 0:2], PS[4][0:1, 0:2]), reads=[rPS[4]], writes=[rK])
            cx.op("dve", lambda e: e.scalar_tensor_tensor(out=mprod[:], in0=rowm[0:1, 0:1], scalar=-SCALE * HD, in1=rowm[0:1, 1:2],
                                                          op0=ALU.mult, op1=ALU.mult), reads=[rK], writes=[rK])
            cx.op("pe", lambda e: e.matmul(PS[4][:, 0:1], ones[0:1, :], mprod[0:1, 0:1], start=True, stop=True), reads=[rK], writes=[rPS[4]])
            cx.op("dve", lambda e: e.tensor_copy(negMB[:], PS[4][:, 0:1]), reads=[rPS[4]], writes=[rK])
            cx.barrier()
            rK.const = True

            KT = sb(st, "KT", [128, S], BF16); rKT = Res("KT")
            Vt = sb(st, "Vt", [128, NT, 128], BF16); rVt = Res("Vt")
            QT = [sb(st, "QT%d" % i, [128, S], BF16) for i in range(4)]
            rQT = [Res("QT%d" % i) for i in range(4)]
            sbt = [sb(st, "sbt%d" % i, [128, 384]) for i in range(2)]; rsbt = [Res("sbt%d" % i) for i in range(2)]
            Pb = [sb(st, "Pb%d" % i, [128, 512], BF16) for i in range(3)]; rPb = [Res("Pb%d" % i) for i in range(3)]
            PTs = [sb(st, "PTs%d" % i, [128, 512], BF16) for i in range(3)]; rPTs = [Res("PTs%d" % i) for i in range(3)]
            Og = [sb(st, "Og%d" % i, [128, 512], BF16) for i in range(2)]; rOg = [Res("Og%d" % i) for i in range(2)]
            sm = [sb(st, "sm%d" % i, [128, 16]) for i in range(2)]; rsm = [Res("sm%d" % i) for i in range(2)]
            cnt = {"s": 0, "p": 0, "o": 0, "og": 0, "sm": 0, "sb": 0}

            for grp in ("A", "B"):
                for g in range(2):
                    kcc = (8 + g) if grp == "A" else (18 + g)
                    Vd = VA if grp == "A" else VB
                    cx.dma("sp", lambda e: e.dma_start(out=KT[:], in_=QKT[kcc, :, :]), writes=[rKT])
                    vsrc = Vd.ap().rearrange("(t p) c -> p t c", p=128)
                    for vq in range(4):
                        cx.dma("sp", lambda e: e.dma_start(out=Vt[:, vq * 8:(vq + 1) * 8, :], in_=vsrc[:, vq * 8:(vq + 1) * 8, g * 128:(g + 1) * 128]),
                               writes=[rVt])
                    for hh in range(4):
                        qcc = (g * 4 + hh) if grp == "A" else (10 + g * 4 + hh)
                        cx.dma("sp", (lambda hh_, qcc_: (lambda e: e.dma_start(out=QT[hh_][:], in_=QKT[qcc_, :, :])))(hh, qcc), writes=[rQT[hh]])
                    for n in range(NT):
                        ogi = cnt["og"] % 2; cnt["og"] += 1
                        for hh in range(4):
                            h = g * 4 + hh
                            smi = cnt["sm"] % 2; cnt["sm"] += 1
                            smt = sm[smi]; rsmt = rsm[smi]
                            po = 2 + (cnt["o"] % 2); cnt["o"] += 1
                            if grp == "A":
                                k0 = max(n - 1, 0); k1 = min(n + 1, NT - 1); nk = k1 - k0 + 1
                                off = (k0 - (n - 1)) * 128
                                W = nk * 128
                                psi = cnt["s"] % 2; cnt["s"] += 1
                                cx.op("pe", lambda e: e.matmul(PS[psi][:, 0:W], QT[hh][:, n * 128:(n + 1) * 128], KT[:, k0 * 128:(k1 + 1) * 128],
                                                                start=True, stop=True), reads=[rQT[hh], rKT], writes=[rPS[psi]])
                                si = cnt["sb"] % 2; cnt["sb"] += 1
                                cx.op("dve", lambda e: e.scalar_tensor_tensor(out=sbt[si][:, 0:W], in0=PS[psi][:, 0:W], scalar=SCALE,
                                                                              in1=biasA[:, h, off:off + W], op0=ALU.mult, op1=ALU.add),
                                      reads=[rPS[psi]], writes=[rsbt[si]])
                                cx.op("dve", lambda e: e.reduce_max(out=smt[:, 0:1], in_=sbt[si][:, 0:W], axis=AX.X), reads=[rsbt[si]], writes=[rsmt])
                                cx.op("dve", lambda e: e.tensor_tensor(out=smt[:, 1:2], in0=smt[:, 0:1], in1=sinkb[:, h:h + 1], op=ALU.max),
                                      reads=[rsmt], writes=[rsmt])
                                cx.op("dve", lambda e: e.tensor_scalar(out=smt[:, 2:3], in0=smt[:, 1:2], scalar1=-1.0, scalar2=None, op0=ALU.mult),
                                      reads=[rsmt], writes=[rsmt])
                                pi = cnt["p"] % 3; cnt["p"] += 1
                                cx.op("act", lambda e: e.activation(out=Pb[pi][:, 0:W], in_=sbt[si][:, 0:W], func=AF.Exp, bias=smt[:, 2:3],
                                                                    accum_out=smt[:, 3:4]), reads=[rsbt[si], rsmt], writes=[rPb[pi], rsmt])
                                cx.op("act", lambda e: e.activation(out=smt[:, 4:5], in_=sinkb[:, h:h + 1], func=AF.Exp, bias=smt[:, 2:3]),
                                      reads=[rsmt], writes=[rsmt])
                                cx.op("dve", lambda e: e.tensor_tensor(out=smt[:, 5:6], in0=smt[:, 3:4], in1=smt[:, 4:5], op=ALU.add),
                                      reads=[rsmt], writes=[rsmt])
                                cx.op("dve", lambda e: e.reciprocal(smt[:, 6:7], smt[:, 5:6]), reads=[rsmt], writes=[rsmt])
                                pti = pi % 2
                                for j in range(nk):
                                    cx.op("pe", lambda e: e.transpose(PT[pti][:, j * 128:(j + 1) * 128], Pb[pi][:, j * 128:(j + 1) * 128], identb[:]),
                                          reads=[rPb[pi]], writes=[rPT[pti]])
                                cx.op("dve", lambda e: e.tensor_copy(PTs[pi][:, 0:W], PT[pti][:, 0:W]), reads=[rPT[pti]], writes=[rPTs[pi]])
                                for j in range(nk):
                                    cx.op("pe", lambda e: e.matmul(PS[po][:, 0:128], PTs[pi][:, j * 128:(j + 1) * 128], Vt[:, k0 + j, :],
                                                                    start=(j == 0), stop=(j == nk - 1)), reads=[rPTs[pi], rVt], writes=[rPS[po]])
                            else:
                                for kc in range(8):
                                    psi = cnt["s"] % 2; cnt["s"] += 1
                                    cx.op("pe", lambda e: e.matmul(PS[psi][:, :], QT[hh][:, n * 128:(n + 1) * 128], KT[:, kc * 512:(kc + 1) * 512],
                                                                    start=True, stop=True), reads=[rQT[hh], rKT], writes=[rPS[psi]])
                                    pi = cnt["p"] % 3; cnt["p"] += 1
                                    cx.op("act", lambda e: e.activation(out=Pb[pi][:], in_=PS[psi][:, :], func=AF.Exp, bias=negMB[:, 0:1], scale=SCALE,
                                                                        accum_out=smt[:, 8 + kc:9 + kc]), reads=[rPS[psi]], writes=[rPb[pi], rsmt])
                                    pti = pi % 2
                                    for j in range(4):
                                        cx.op("pe", lambda e: e.transpose(PT[pti][:, j * 128:(j + 1) * 128], Pb[pi][:, j * 128:(j + 1) * 128], identb[:]),
                                              reads=[rPb[pi]], writes=[rPT[pti]])
                                    cx.op("dve", lambda e: e.tensor_copy(PTs[pi][:], PT[pti][:, :]), reads=[rPT[pti]], writes=[rPTs[pi]])
                                    for j in range(4):
                                        cx.op("pe", lambda e: e.matmul(PS[po][:, 0:128], PTs[pi][:, j * 128:(j + 1) * 128], Vt[:, kc * 4 + j, :],
                                                                        start=(kc == 0 and j == 0), stop=(kc == 7 and j == 3)),
                                              reads=[rPTs[pi], rVt], writes=[rPS[po]])
                                cx.op("dve", lambda e: e.reduce_sum(out=smt[:, 5:6], in_=smt[:, 8:16], axis=AX.X), reads=[rsmt], writes=[rsmt])
                                cx.op("dve", lambda e: e.reciprocal(smt[:, 6:7], smt[:, 5:6]), reads=[rsmt], writes=[rsmt])
                            cx.op("act", lambda e: e.activation(out=Og[ogi][:, hh * 128:(hh + 1) * 128], in_=PS[po][:, 0:128], func=AF.Copy,
                                                                scale=smt[:, 6:7]), reads=[rPS[po], rsmt], writes=[rOg[ogi]])
                        c0 = (0 if grp == "A" else 1024) + g * 512
                        cx.dma("pool", lambda e: e.dma_start(out=OS[n * 128:(n + 1) * 128, c0:c0 + 512], in_=Og[ogi][:]), reads=[rOg[ogi]])
            cx.barrier()

        affAll = sb(es, "affAll", [128, NT, NE])
        rAff = Res("affAll")
        osv = OS.ap().rearrange("(t p) d -> t p d", p=128)
        yv = Y.ap().rearrange("(t p) d -> t p d", p=128)
        h2v = H2.ap().rearrange("(t p) d -> t p d", p=128)
        with ExitStack() as st:
          if stop >= 3:
            gf = sb(st, "gf", [128, D])
            wr = sb(st, "wr", [128, 16, NE])
            rK = Res("p3c")
            cx.dma("sp", lambda e: e.dma_start(out=gf[:], in_=g_ffn[:, :]), writes=[rK])
            cx.dma("sp", lambda e: e.dma_start(out=wr[:], in_=w_router.ap().rearrange("(k p) c -> p k c", p=128)), writes=[rK])
            cx.barrier()
            rK.const = True
            x1 = [sb(st, "x1_%d" % i, [128, D]) for i in range(4)]; rx1 = [Res("x1_%d" % i) for i in range(4)]
            otb = [sb(st, "otb%d" % i, [128, D], BF16) for i in range(2)]; rotb = [Res("otb%d" % i) for i in range(2)]
            oT = sb(st, "oT", [128, 16, 512], BF16); roT = Res("oT")
            h2t = [sb(st, "h2t%d" % i, [128, H2W]) for i in range(2)]; rh2t = [Res("h2t%d" % i) for i in range(2)]
            h2T = sb(st, "h2T", [128, 16, 128]); rh2T = Res("h2T")
            jk = sb(st, "jk", [128, D], BF16); rjk = Res("jk")
            ss = sb(st, "ss3", [128, 1]); std = sb(st, "std3", [128, 1]); rstd = sb(st, "rstd3", [128, 1]); rss = Res("ss3")
            sm3 = sb(st, "sm3", [128, 8]); rsm3 = Res("sm3")
            ex = sb(st, "ex", [128, NE]); rex = Res("ex")
            tcount = 0
            for tc in range(8):
                for t in range(4):
                    tg = tc * 4 + t
                    oi = tg % 2
                    cx.dma("sp", lambda e: e.dma_start(out=otb[oi][:], in_=osv[tg]), writes=[rotb[oi]])
                    cx.dma("sp", lambda e: e.dma_start(out=x1[t][:], in_=xv[tg]), writes=[rx1[t]])
                    for k4 in range(4):
                        pi = tcount % 2; tcount += 1
                        for kk in range(4):
                            k = k4 * 4 + kk
                            cx.op("pe", lambda e: e.transpose(PT[pi][:, kk * 128:(kk + 1) * 128], otb[oi][:, k * 128:(k + 1) * 128], identb[:]),
                                  reads=[rotb[oi]], writes=[rPT[pi]])
                        dst = oT[:, k4 * 4:(k4 + 1) * 4, t * 128:(t + 1) * 128]
                        srcp = PT[pi][:, :].rearrange("p (k c) -> p k c", k=4)
                        if pi == 1:
                            cx.op("dve", lambda e: e.tensor_copy(dst, srcp), reads=[rPT[pi]], writes=[roT])
                        else:
                            cx.op("act", lambda e: e.activation(out=dst, in_=srcp, func=AF.Copy), reads=[rPT[pi]], writes=[roT])
                ws = WStream([(w_out_v[:, :, cg * 256:(cg + 1) * 256], 16) for cg in range(8)])
                for cg in range(8):
                    wv, rw = ws.next()
                    for t in range(4):
                        pb = 2 + (t % 2)
                        for k in range(16):
                            cx.op("pe", lambda e: e.matmul(PS[pb][:, 0:256], oT[:, k, t * 128:(t + 1) * 128], wv[:, k, :], start=(k == 0), stop=(k == 15)),
                                  reads=[roT, rw], writes=[rPS[pb]])
                        cx.op("dve", lambda e: e.tensor_tensor(out=x1[t][:, cg * 256:(cg + 1) * 256], in0=PS[pb][:, 0:256],
                                                               in1=x1[t][:, cg * 256:(cg + 1) * 256], op=ALU.add),
                              reads=[rPS[pb], rx1[t]], writes=[rx1[t]])
                for t in range(4):
                    tg = tc * 4 + t
                    hi = tg % 2
                    cx.dma("pool", lambda e: e.dma_start(out=yv[tg], in_=x1[t][:]), reads=[rx1[t]])
                    rmsnorm_rstd((ss, rss, std, rstd), x1[t][:], rx1[t], jk[:], rjk, "x1")
                    cx.op("dve", lambda e: e.scalar_tensor_tensor(out=h2t[hi][:, 0:D], in0=x1[t][:], scalar=rstd[:, 0:1], in1=gf[:],
                                                                  op0=ALU.mult, op1=ALU.mult), reads=[rx1[t], rss], writes=[rh2t[hi]])
                    for k4 in range(4):
                        pb = k4 % 2
                        for kk in range(4):
                            k = k4 * 4 + kk
                            cx.op("pe", lambda e: e.transpose(PS[pb][:, kk * 128:(kk + 1) * 128], h2t[hi][:, k * 128:(k + 1) * 128], identf[:]),
                                  reads=[rh2t[hi]], writes=[rPS[pb]])
                        dst = h2T[:, k4 * 4:(k4 + 1) * 4, :]
                        srcp = PS[pb][:, :].rearrange("p (k c) -> p k c", k=4)
                        if k4 % 2 == 0:
                            cx.op("dve", lambda e: e.tensor_copy(dst, srcp), reads=[rPS[pb]], writes=[rh2T])
                        else:
                            cx.op("act", lambda e: e.activation(out=dst, in_=srcp, func=AF.Copy), reads=[rPS[pb]], writes=[rh2T])
                    for k in range(16):
                        cx.op("pe", lambda e: e.matmul(PS[4][:, 0:NE], h2T[:, k, :], wr[:, k, :], start=(k == 0), stop=(k == 15)),
                              reads=[rh2T], writes=[rPS[4]])
                    cx.op("dve", lambda e: e.reduce_max(out=sm3[:, 0:1], in_=PS[4][:, 0:NE], axis=AX.X), reads=[rPS[4]], writes=[rsm3])
                    cx.op("dve", lambda e: e.tensor_scalar(out=sm3[:, 1:2], in0=sm3[:, 0:1], scalar1=-1.0, scalar2=None, op0=ALU.mult),
                          reads=[rsm3], writes=[rsm3])
                    cx.op("act", lambda e: e.activation(out=ex[:], in_=PS[4][:, 0:NE], func=AF.Exp, bias=sm3[:, 1:2], accum_out=sm3[:, 2:3]),
                          reads=[rPS[4], rsm3], writes=[rex, rsm3])
                    cx.op("dve", lambda e: e.reciprocal(sm3[:, 3:4], sm3[:, 2:3]), reads=[rsm3], writes=[rsm3])
                    cx.op("dve", lambda e: e.tensor_scalar(out=h2t[hi][:, D:D + NE], in0=ex[:], scalar1=sm3[:, 3:4], scalar2=None, op0=ALU.mult),
                          reads=[rex, rsm3], writes=[rh2t[hi]])
                    cx.op("dve", lambda e: e.tensor_copy(affAll[:, tg, :], h2t[hi][:, D:D + NE]), reads=[rh2t[hi]], writes=[rAff])
                    cx.dma("pool", lambda e: e.dma_start(out=h2v[tg], in_=h2t[hi][:]), reads=[rh2t[hi]])
            cx.barrier()

        pmTok = sb(es, "pmTok", [128, NT, NE])
        rpm = Res("pmTok")
        with ExitStack() as st:
          if stop >= 4:
            affT = sb(st, "affT", [NE, S]); raffT = Res("affT")
            junk = sb(st, "junk4", [NE, S]); rjunk = Res("junk4")
            onesr = sb(st, "onesr", [NE, S])
            thr = sb(st, "thr", [NE, 1], U32); cand = sb(st, "cand", [NE, 1], U32); selm = sb(st, "selm", [NE, 1], U32)
            cntt = sb(st, "cntt", [NE, 1])
            rT = Res("thr")
            for tg in range(NT):
                pb = tg % 2
                cx.op("pe", lambda e: e.transpose(PS[pb][0:NE, 0:128], affAll[:, tg, :], identf[:]), reads=[rAff], writes=[rPS[pb]])
                cx.op("dve", lambda e: e.tensor_copy(affT[:, tg * 128:(tg + 1) * 128], PS[pb][0:NE, 0:128]), reads=[rPS[pb]], writes=[raffT])
            cx.op("dve", lambda e: e.memset(thr[:], 0), writes=[rT])
            cx.op("dve", lambda e: e.memset(onesr[:], 1.0), writes=[rT])
            for b in range(30, -1, -1):
                cx.op("dve", lambda e: e.tensor_scalar(out=cand[:], in0=thr[:], scalar1=(1 << b), scalar2=None, op0=ALU.bitwise_or),
                      reads=[rT], writes=[rT])
                cx.op("dve", lambda e: e.tensor_scalar(out=junk[:], in0=affT[:], scalar1=cand[:].bitcast(F32)[:, 0:1], scalar2=None,
                                                       op0=ALU.is_ge, op1=ALU.add, accum_out=cntt[:, 0:1]),
                      reads=[rT, raffT], writes=[rjunk, rT])
                cx.op("dve", lambda e: e.tensor_scalar(out=selm[:], in0=cntt[:], scalar1=float(CAP), scalar2=None, op0=ALU.is_ge),
                      reads=[rT], writes=[rT])
                cx.op("dve", lambda e: e.copy_predicated(out=thr[:], mask=selm[:], data=cand[:]), reads=[rT], writes=[rT])
            cx.op("dve", lambda e: e.tensor_scalar(out=junk[:], in0=affT[:], scalar1=thr[:].bitcast(F32)[:, 0:1], scalar2=None, op0=ALU.is_ge),
                  reads=[rT, raffT], writes=[rjunk])
            cx.op("dve", lambda e: e.tensor_tensor_scan(out=affT[:], data0=onesr[:], data1=junk[:], initial=0.0, op0=ALU.mult, op1=ALU.add),
                  reads=[rjunk, rT], writes=[raffT])
            cx.op("dve", lambda e: e.tensor_tensor(out=affT[:], in0=affT[:], in1=junk[:], op=ALU.mult), reads=[rjunk, raffT], writes=[raffT])
            cx.op("dve", lambda e: e.tensor_scalar(out=affT[:], in0=affT[:], scalar1=-1.0, scalar2=None, op0=ALU.add), reads=[raffT], writes=[raffT])
            for tg in range(NT):
                pb = tg % 2
                cx.op("pe", lambda e: e.transpose(PS[pb][:, 0:NE], affT[:, tg * 128:(tg + 1) * 128], identf[0:NE, 0:NE]), reads=[raffT], writes=[rPS[pb]])
                cx.op("dve", lambda e: e.tensor_copy(pmTok[:, tg, :], PS[pb][:, 0:NE]), reads=[rPS[pb]], writes=[rpm])
            if debug:
                cx.dma("sp", lambda e: e.dma_start(out=dbg_pm[:, :], in_=pmTok[:].rearrange("p t c -> p (t c)")), reads=[rpm])
            cx.barrier()

        rY = Res("Y")
        rH2 = Res("H2", const=True)
        with ExitStack() as st:
          if stop >= 5:
            iot = sb(st, "iot", [128, CAP])
            tokid = sb(st, "tokid", [128, NT, 2], BF16)
            rK = Res("p5c")
            cx.dma("sp", lambda e: e.dma_start(out=iot[:], in_=c_iota[:, :]), writes=[rK])
            cx.dma("sp", lambda e: e.dma_start(out=tokid[:].rearrange("p t c -> p (t c)"), in_=c_tokid[:, :]), writes=[rK])
            cx.barrier()
            rK.const = True
            OH = [sb(st, "OH%d" % i, [128, CAP], BF16) for i in range(2)]; rOH = [Res("OH%d" % i) for i in range(2)]
            idr = sb(st, "idr", [2, CAP]); ridr = Res("idr")
            idf = sb(st, "idf", [128, 4]); idp = sb(st, "idp", [128, 8]); idi = [sb(st, "idi%d" % i, [128, 4], I32) for i in range(2)]
            ridi = [Res("idi%d" % i) for i in range(2)]
            ridf = Res("idf")
            gates = [sb(st, "gates%d" % i, [128, 4]) for i in range(2)]; rgates = [Res("gates%d" % i) for i in range(2)]
            xg = [sb(st, "xg%d" % i, [128, H2W]) for i in range(2)]; rxg = [Res("xg%d" % i) for i in range(2)]
            xgT = sb(st, "xgT", [128, 16, CAP], BF16); rxgT = Res("xgT")
            hTm = sb(st, "hTm", [128, 32, CAP], BF16); rhTm = Res("hTm")
            sg = [sb(st, "sg%d" % i, [128, CAP]) for i in range(2)]; rsg = [Res("sg%d" % i) for i in range(2)]
            eo = [sb(st, "eo%d" % i, [128, D]) for i in range(4)]; reo = [Res("eo%d" % i) for i in range(4)]
            ohc = 0; xgc = 0; trc = 0; guc = 0
            for ex_ in range(n_exp):
                ei = ex_ % 2
                for tg in range(NT):
                    oi = ohc % 2; ohc += 1
                    cx.op("dve", lambda e: e.tensor_scalar(out=OH[oi][:], in0=iot[:], scalar1=pmTok[:, tg, ex_:ex_ + 1], scalar2=None, op0=ALU.is_equal),
                          reads=[rpm], writes=[rOH[oi]])
                    cx.op("pe", lambda e: e.matmul(PS[4][0:2, :], tokid[:, tg, :], OH[oi][:], start=(tg == 0), stop=(tg == NT - 1)),
                          reads=[rOH[oi]], writes=[rPS[4]])
                cx.op("dve", lambda e: e.tensor_copy(idr[:], PS[4][0:2, :]), reads=[rPS[4]], writes=[ridr])
                for c in range(4):
                    cx.op("pe", lambda e: e.transpose(PS[4][:, c * 2:(c + 1) * 2], idr[0:2, c * 128:(c + 1) * 128], identf[0:2, 0:2]),
                          reads=[ridr], writes=[rPS[4]])
                cx.op("dve", lambda e: e.tensor_copy(idp[:], PS[4][:, 0:8]), reads=[rPS[4]], writes=[ridf])
                pv = idp[:, :].rearrange("p (c two) -> p c two", two=2)
                cx.op("dve", lambda e: e.scalar_tensor_tensor(out=idf[:], in0=pv[:, :, 0], scalar=128.0, in1=pv[:, :, 1], op0=ALU.mult, op1=ALU.add),
                      reads=[ridf], writes=[ridf])
                cx.op("dve", lambda e: e.tensor_copy(idi[ei][:], idf[:]), reads=[ridf], writes=[ridi[ei]])
                if debug and ex_ == 0:
                    cx.dma("sp", lambda e: e.dma_start(out=dbg_idx[:, :], in_=idi[ei][:]), reads=[ridi[ei]])
                for c in range(4):
                    xi = xgc % 2; xgc += 1
                    cx.dma("pool", lambda e: e.indirect_dma_start(out=xg[xi][:], out_offset=None, in_=H2[:, :],
                                                                  in_offset=bass.IndirectOffsetOnAxis(ap=idi[ei][:, c:c + 1], axis=0)),
                           reads=[ridi[ei], rH2], writes=[rxg[xi]])
                    cx.op("dve", lambda e: e.tensor_copy(gates[ei][:, c:c + 1], xg[xi][:, D + ex_:D + ex_ + 1]), reads=[rxg[xi]], writes=[rgates[ei]])
                    for k4 in range(4):
                        pb = trc % 2; trc += 1
                        for kk in range(4):
                            k = k4 * 4 + kk
                            cx.op("pe", lambda e: e.transpose(PS[pb][:, kk * 128:(kk + 1) * 128], xg[xi][:, k * 128:(k + 1) * 128], identf[:]),
                                  reads=[rxg[xi]], writes=[rPS[pb]])
                        dst = xgT[:, k4 * 4:(k4 + 1) * 4, c * 128:(c + 1) * 128]
                        srcp = PS[pb][:, :].rearrange("p (k c) -> p k c", k=4)
                        if k4 % 2 == 0:
                            cx.op("dve", lambda e: e.tensor_copy(dst, srcp), reads=[rPS[pb]], writes=[rxgT])
                        else:
                            cx.op("act", lambda e: e.activation(out=dst, in_=srcp, func=AF.Copy), reads=[rPS[pb]], writes=[rxgT])
                wg_v = w_gate.ap()[ex_].rearrange("(k p) c -> p k c", p=128)
                wu_v = w_up.ap()[ex_].rearrange("(k p) c -> p k c", p=128)
                wd_v = w_down.ap()[ex_].rearrange("(k p) c -> p k c", p=128)
                specs = []
                for fg in range(16):
                    specs.append((wg_v[:, :, fg * 256:(fg + 1) * 256], 16))
                    specs.append((wu_v[:, :, fg * 256:(fg + 1) * 256], 16))
                for c4 in range(4):
                    for q in range(4):
                        specs.append((wd_v[:, q * 8:(q + 1) * 8, c4 * 512:(c4 + 1) * 512], 8))
                ws = WStream(specs)
                for fg in range(16):
                    wgv, rwg = ws.next()
                    wuv, rwu = ws.next()
                    for half in range(2):
                        f = fg * 2 + half
                        pg = guc % 2; pu = 2 + (guc % 2); guc += 1
                        for k in range(16):
                            cx.op("pe", lambda e: e.matmul(PS[pg][:, :], wgv[:, k, half * 128:(half + 1) * 128], xgT[:, k, :], start=(k == 0), stop=(k == 15)),
                                  reads=[rwg, rxgT], writes=[rPS[pg]])
                        for k in range(16):
                            cx.op("pe", lambda e: e.matmul(PS[pu][:, :], wuv[:, k, half * 128:(half + 1) * 128], xgT[:, k, :], start=(k == 0), stop=(k == 15)),
                                  reads=[rwu, rxgT], writes=[rPS[pu]])
                        si = f % 2
                        cx.op("act", lambda e: e.activation(out=sg[si][:], in_=PS[pg][:, :], func=AF.Silu), reads=[rPS[pg]], writes=[rsg[si]])
                        cx.op("dve", lambda e: e.tensor_tensor(out=hTm[:, f, :], in0=sg[si][:], in1=PS[pu][:, :], op=ALU.mult),
                              reads=[rsg[si], rPS[pu]], writes=[rhTm])
                for c4 in range(4):
                    for q in range(4):
                        wdv, rwd = ws.next()
                        for f8 in range(8):
                            f = q * 8 + f8
                            for t in range(4):
                                pb = [4, 5, 0, 1][t] if False else [4, 5, 2, 3][t]
                                cx.op("pe", lambda e: e.matmul(PS[pb][:, :], hTm[:, f, t * 128:(t + 1) * 128], wdv[:, f8, :],
                                                                start=(f == 0), stop=(f == 31)), reads=[rhTm, rwd], writes=[rPS[pb]])
                    for t in range(4):
                        pb = [4, 5, 2, 3][t]
                        if t % 2 == 0:
                            cx.op("act", lambda e: e.activation(out=eo[t][:, c4 * 512:(c4 + 1) * 512], in_=PS[pb][:, :], func=AF.Copy, scale=gates[ei][:, t:t + 1]),
                                  reads=[rPS[pb], rgates[ei]], writes=[reo[t]])
                        else:
                            cx.op("dve", lambda e: e.tensor_scalar(out=eo[t][:, c4 * 512:(c4 + 1) * 512], in0=PS[pb][:, :], scalar1=gates[ei][:, t:t + 1],
                                                                   scalar2=None, op0=ALU.mult), reads=[rPS[pb], rgates[ei]], writes=[reo[t]])
                for c in range(4):
                    cx.dma("pool", lambda e: e.indirect_dma_start(out=Y[:, :], out_offset=bass.IndirectOffsetOnAxis(ap=idi[ei][:, c:c + 1], axis=0),
                                                                  in_=eo[c][:], in_offset=None, compute_op=ALU.add),
                           reads=[reo[c], ridi[ei]], writes=[rY])
            cx.barrier()

        outv = out.ap().rearrange("(t p) d -> t p d", p=128)
        with ExitStack() as st:
          if stop >= 6:
            gfi = sb(st, "gfi", [128, D])
            rK = Res("p6c")
            cx.dma("sp", lambda e: e.dma_start(out=gfi[:], in_=g_fin[:, :]), writes=[rK])
            cx.barrier()
            rK.const = True
            yt = [sb(st, "yt%d" % i, [128, D]) for i in range(3)]; ryt = [Res("yt%d" % i) for i in range(3)]
            ot = [sb(st, "ot%d" % i, [128, D]) for i in range(3)]; rot = [Res("ot%d" % i) for i in range(3)]
            ss = sb(st, "ss6", [128, 1]); std = sb(st, "std6", [128, 1]); rstd = sb(st, "rstd6", [128, 1]); rss = Res("ss6")
            for tg in range(NT):
                i = tg % 3
                cx.dma("sp", lambda e: e.dma_start(out=yt[i][:], in_=yv[tg]), writes=[ryt[i]])
                rmsnorm_rstd((ss, rss, std, rstd), yt[i][:], ryt[i], ot[i][:], rot[i], "y")
                cx.op("dve", lambda e: e.scalar_tensor_tensor(out=ot[i][:], in0=yt[i][:], scalar=rstd[:, 0:1], in1=gfi[:], op0=ALU.mult, op1=ALU.mult),
                      reads=[ryt[i], rss], writes=[rot[i]])
                cx.dma("pool", lambda e: e.dma_start(out=outv[tg], in_=ot[i][:]), reads=[rot[i]])
            cx.barrier()
    return nc


def _consts():
    bf = ml_dtypes.bfloat16
    c = {}
    c["c_identb"] = np.eye(128, dtype=np.float32).astype(bf)
    c["c_identf"] = np.eye(128, dtype=np.float32)
    rotT = np.zeros((128, 128), np.float32)
    for base in (0, 64):
        for i in range(32):
            m = base + i
            rotT[m + 32, m] = -1.0
            rotT[m, m + 32] = 1.0
    c["c_rotT"] = rotT
    c["c_ones"] = np.ones((128, 128), np.float32)
    half = 64
    inv_freq = (10000.0 ** (-np.arange(0, half, 2, dtype=np.float32) / half)).astype(np.float32)
    pos = np.arange(S)
    row = (pos // 64).astype(np.float32)
    colp = (pos % 64).astype(np.float32)
    ang_r = (row[None, :] * inv_freq[:, None]).astype(np.float32)
    ang_c = (colp[None, :] * inv_freq[:, None]).astype(np.float32)
    cosT = np.concatenate([np.cos(ang_r), np.cos(ang_r), np.cos(ang_c), np.cos(ang_c)], 0).astype(np.float32)
    sinT = np.concatenate([np.sin(ang_r), np.sin(ang_r), np.sin(ang_c), np.sin(ang_c)], 0).astype(np.float32)
    c["c_cos"] = np.ascontiguousarray(cosT)
    c["c_sin"] = np.ascontiguousarray(sinT)
    slopes = (2.0 ** (-8.0 * np.arange(1, 9) / 8)).astype(np.float32)
    qi = np.arange(128)[:, None]
    kj = np.arange(384)[None, :]
    dist = np.abs(qi + 128 - kj).astype(np.float32)
    bias = np.where((dist <= 128)[:, None, :], -slopes[None, :, None] * dist[:, None, :], np.float32(-1e30)).astype(np.float32)
    c["c_bias"] = np.ascontiguousarray(bias.reshape(128, 8 * 384))
    c["c_iota"] = np.ascontiguousarray(np.broadcast_to(np.arange(CAP, dtype=np.float32)[None, :], (128, CAP)))
    tok = np.zeros((128, NT, 2), np.float32)
    tok[:, :, 0] = np.arange(NT)[None, :]
    tok[:, :, 1] = np.arange(128)[:, None]
    c["c_tokid"] = np.ascontiguousarray(tok.reshape(128, NT * 2)).astype(bf)
    return c


_NC_CACHE = {}


def kernel(x, norm_mix, w_in, sink_a, q_norm_b, k_norm_b, w_out, norm_ffn,
           w_router, w_gate, w_up, w_down, norm_final):
    f = lambda a: np.ascontiguousarray(np.asarray(a, dtype=np.float32))
    x = f(x)
    B = x.shape[0]
    if "nc" not in _NC_CACHE:
        _NC_CACHE["nc"] = build_nc()
    nc = _NC_CACHE["nc"]
    bc = lambda v, n: np.ascontiguousarray(np.broadcast_to(f(v).reshape(1, n), (128, n)))
    shared = dict(
        w_in=f(w_in)[0], w_out=f(w_out)[0], w_router=f(w_router)[0],
        w_gate=f(w_gate)[0], w_up=f(w_up)[0], w_down=f(w_down)[0],
        g_mix=bc(norm_mix, D), g_ffn=bc(norm_ffn, D), g_fin=bc(norm_final, D),
        sink_b=bc(sink_a, 8),
        gq_col=np.ascontiguousarray(f(q_norm_b).reshape(128, 1)),
        gk_col=np.ascontiguousarray(f(k_norm_b).reshape(128, 1)),
    )
    shared.update(_consts())
    in_maps = []
    for c in range(N_CORES):
        m = dict(shared)
        m["x"] = x[c % B]
        in_maps.append(m)
    res = run_bass_kernel_spmd(nc, in_maps, core_ids=list(range(N_CORES)))
    return np.stack([np.asarray(res.results[b]["out"], dtype=np.float32) for b in range(B)], axis=0)
```

```python
import numpy as np
import ml_dtypes
from contextlib import ExitStack
import concourse.bass as bass
import concourse.mybir as mybir
from concourse.bass_utils import run_bass_kernel_spmd

F32 = mybir.dt.float32
BF16 = mybir.dt.bfloat16
I32 = mybir.dt.int32
U32 = mybir.dt.uint32
AF = mybir.ActivationFunctionType
ALU = mybir.AluOpType
AX = mybir.AxisListType

D = 2048
S = 4096
SH = 2048
NT = S // 128
NTH = SH // 128
NEL = 8
SA = SH + 256
HD = 128
DIN = 3072
NE = 16
CAP = 512
DFF = 4096
EPS = 1e-6
SCALE = HD ** -0.5
N_CORES = 8
DBG = {}


class Res:
    __slots__ = ("w", "r", "const", "name")

    def __init__(self, name, const=False):
        self.w = None
        self.r = {}
        self.const = const
        self.name = name


class Ctx:
    def __init__(self, nc, es):
        self.nc = nc
        self.eng = dict(pe=nc.tensor, dve=nc.vector, act=nc.scalar, pool=nc.gpsimd, sp=nc.sync)
        self.semobj = {}
        for k in self.eng:
            self.semobj[k] = es.enter_context(nc.semaphore("s_" + k))
        self.cnt = {k: 0 for k in self.eng}
        self.known = {k: {} for k in self.eng}
        self.dq = {"sp": 14, "pool": 10, "act": 8}
        self.dval = {}
        self.drr = {}
        self.ccv = {}
        for i in range(16):
            self.semobj["cc%d" % i] = es.enter_context(nc.semaphore("cc%d" % i))
        for q, n in self.dq.items():
            self.dval[q] = [0] * n
            self.drr[q] = 0
            for i in range(n):
                self.semobj[(q, i)] = es.enter_context(nc.semaphore("d_%s%d" % (q, i)))

    def wait(self, e, tok):
        if tok is None:
            return
        s, v, prod = tok
        if prod == "pe" and e == "pe":
            return
        kn = self.known[e]
        if kn.get(s, 0) >= v:
            return
        self.eng[e].wait_ge(self.semobj[s], v)
        kn[s] = v

    def deps(self, e, reads, writes):
        for r in reads:
            self.wait(e, r.w)
        for w in writes:
            self.wait(e, w.w)
            for s, (v, prod) in w.r.items():
                self.wait(e, (s, v, prod))

    def commit(self, tok, reads, writes):
        s, v, prod = tok
        for r in reads:
            if not r.const:
                r.r[s] = (v, prod)
        for w in writes:
            w.w = tok
            w.r = {}

    def op(self, e, fn, reads=(), writes=()):
        self.deps(e, reads, writes)
        ins = fn(self.eng[e])
        self.cnt[e] += 1
        ins.then_inc(self.semobj[e], 1)
        tok = (e, self.cnt[e], e)
        self.known[e][e] = max(self.known[e].get(e, 0), 0)
        self.commit(tok, reads, writes)
        return tok

    def dma(self, q, fn, reads=(), writes=()):
        n = self.dq[q]
        i = self.drr[q]
        self.drr[q] = (i + 1) % n
        key = (q, i)
        prev = self.dval[q][i]
        if prev:
            self.wait(q, (key, prev, "dma"))
        self.deps(q, reads, writes)
        ins = fn(self.eng[q])
        v = prev + 16
        ins.then_inc(self.semobj[key], 16)
        self.dval[q][i] = v
        tok = (key, v, "dma")
        self.commit(tok, reads, writes)
        return tok

    def coll(self, semname, fn, reads=(), writes=()):
        self.deps("pool", reads, writes)
        ins = fn(self.eng["pool"])
        ins.then_inc(self.semobj[semname])
        self.ccv[semname] = self.ccv.get(semname, 0) + 1
        tok = (semname, self.ccv[semname], "dma")
        self.commit(tok, reads, writes)
        return tok

    def barrier(self):
        toks = [(k, self.cnt[k], k) for k in self.eng if self.cnt[k] > 0]
        for q, n in self.dq.items():
            for i in range(n):
                if self.dval[q][i]:
                    toks.append(((q, i), self.dval[q][i], "dma"))
        for nm, v in self.ccv.items():
            toks.append((nm, v, "dma"))
        for e in self.eng:
            for t in toks:
                if t[0] == e and e == "pe":
                    continue
                self.wait(e, t)


def build_nc():
    nc = bass.Bass("TRN2", target_bir_lowering=False)

    def din(name, shape, dt=F32):
        return nc.dram_tensor(name, list(shape), dt, kind="ExternalInput")

    x_own = din("x_own", [SH, D])
    x_oth = din("x_oth", [SH, D])
    w_in = din("w_in", [D, DIN])
    w_out = din("w_out", [D, D])
    w_router = din("w_router", [D, NE])
    w_gate = din("w_gate", [NEL, D, DFF])
    w_up = din("w_up", [NEL, D, DFF])
    w_down = din("w_down", [NEL, DFF, D])
    g_mix = din("g_mix", [128, D])
    g_ffn = din("g_ffn", [128, D])
    g_fin = din("g_fin", [128, D])
    sink_b = din("sink_b", [128, 8])
    gq_col = din("gq_col", [128, 1])
    gk_col = din("gk_col", [128, 1])
    c_identb = din("c_identb", [128, 128], BF16)
    c_identf = din("c_identf", [128, 128])
    c_rotT = din("c_rotT", [128, 128])
    c_ones = din("c_ones", [128, 128])
    c_cos = din("c_cos", [128, S])
    c_sin = din("c_sin", [128, S])
    c_bias = din("c_bias", [128, 3 * 8 * 384])
    c_iota = din("c_iota", [128, CAP])
    c_tokid = din("c_tokid", [128, NT * 2], BF16)
    c_sel16 = din("c_sel16", [NE, NEL])
    c_selB = din("c_selB", [128, NEL * NE])
    c_off = din("c_off", [128, 1])
    c_s01 = din("c_s01", [128, 2])
    c_zero = din("c_zero", [128, D])
    out = nc.dram_tensor("out", [SH, D], F32, kind="ExternalOutput")

    QTd = nc.dram_tensor("QTd", [16, 128, SH], BF16)
    KTB = nc.dram_tensor("KTB", [2, 128, S], BF16)
    VBs = nc.dram_tensor("VBs", [S, 256], BF16)
    KTA = nc.dram_tensor("KTA", [2, 128, SA], BF16)
    VAs = nc.dram_tensor("VAs", [SA, 256], BF16)
    OS = nc.dram_tensor("OS", [SH, D], BF16)
    Yr = nc.dram_tensor("Yr", [S, D], F32)
    H2h = nc.dram_tensor("H2h", [SH, D], BF16)
    AFFh = nc.dram_tensor("AFFh", [SH, NE], F32)
    H2st = [nc.dram_tensor("H2st%d" % k, [1024, D], BF16) for k in range(4)]
    H2f = nc.dram_tensor("H2f", [S, D], BF16)
    AFFf = nc.dram_tensor("AFFf", [S, NE], F32)
    Gst = [nc.dram_tensor("Gst%d" % k, [512, D], F32) for k in range(8)]
    PAIRS = [[0, 1], [2, 3], [4, 5], [6, 7]]

    with ExitStack() as es:
        cx = Ctx(nc, es)

        def sb(st, name, shape, dt=F32):
            return st.enter_context(nc.sbuf_tensor(name, list(shape), dt))

        PS = [es.enter_context(nc.psum_tensor("ps%d" % i, [128, 512], F32)) for i in range(6)]
        PT = [es.enter_context(nc.psum_tensor("pt%d" % i, [128, 512], BF16)) for i in range(2)]
        rPS = [Res("ps%d" % i) for i in range(6)]
        rPT = [Res("pt%d" % i) for i in range(2)]

        identb = sb(es, "identb", [128, 128], BF16)
        identf = sb(es, "identf", [128, 128])
        ones = sb(es, "ones", [128, 128])
        sinkb = sb(es, "sinkb", [128, 8])
        rC = Res("consts")
        cx.dma("sp", lambda e: e.dma_start(out=identb[:], in_=c_identb[:, :]), writes=[rC])
        cx.dma("sp", lambda e: e.dma_start(out=identf[:], in_=c_identf[:, :]), writes=[rC])
        cx.dma("sp", lambda e: e.dma_start(out=ones[:], in_=c_ones[:, :]), writes=[rC])
        cx.dma("sp", lambda e: e.dma_start(out=sinkb[:], in_=sink_b[:, :]), writes=[rC])
        cx.barrier()
        rC.const = True

        NSTG = 3
        NWB = 3
        wst = [sb(es, "wst%d" % i, [128, 4096]) for i in range(NSTG)]
        wbf = [sb(es, "wbf%d" % i, [128, 4096], BF16) for i in range(NWB)]
        rwst = [Res("wst%d" % i) for i in range(NSTG)]
        rwbf = [Res("wbf%d" % i) for i in range(NWB)]
        wstate = {"i": 0, "c": 0}
        cast_eng = ["act", "pool", "dve", "act", "dve", "pool", "act"]

        def wgroup_load(src_ap, k):
            i = wstate["i"] % NSTG
            wstate["i"] += 1
            dst = wst[i][:, :].rearrange("p (k c) -> p k c", k=k)
            cx.dma("sp", lambda e: e.dma_start(out=dst, in_=src_ap), writes=[rwst[i]])
            return i

        def wgroup_cast(i):
            j = wstate["c"] % NWB
            ce = cast_eng[wstate["c"] % len(cast_eng)]
            wstate["c"] += 1
            if ce == "act":
                cx.op("act", lambda e: e.activation(out=wbf[j][:], in_=wst[i][:], func=AF.Copy),
                      reads=[rwst[i]], writes=[rwbf[j]])
            else:
                cx.op(ce, lambda e: e.tensor_copy(wbf[j][:], wst[i][:]), reads=[rwst[i]], writes=[rwbf[j]])
            return j

        class WStream:
            def __init__(self, specs, depth=2):
                self.specs = specs
                self.n = len(specs)
                self.q = []
                self.pos = 0
                for _ in range(min(depth, self.n)):
                    self._issue()

            def _issue(self):
                ap, k = self.specs[self.pos]
                self.pos += 1
                self.q.append((wgroup_load(ap, k), k))

            def next(self):
                i, k = self.q.pop(0)
                j = wgroup_cast(i)
                if self.pos < self.n:
                    self._issue()
                return wbf[j][:, :].rearrange("p (k c) -> p k c", k=k), rwbf[j]

        epsc = sb(es, "epsc", [128, 1])
        eps128 = sb(es, "eps128", [128, 1])
        rE = Res("eps")
        cx.op("dve", lambda e: e.memset(epsc[:], EPS), writes=[rE])
        cx.op("dve", lambda e: e.memset(eps128[:], EPS), writes=[rE])
        cx.barrier()
        rE.const = True

        def rmsnorm_rstd(st_tiles, src, rsrc, junk, rjunk):
            ss, rss, std, rstd = st_tiles
            cx.op("act", lambda e: e.activation(out=junk, in_=src, func=AF.Square, accum_out=ss[:, 0:1]),
                  reads=[rsrc], writes=[rjunk, rss])
            cx.op("act", lambda e: e.activation(out=std[:, 0:1], in_=ss[:, 0:1], func=AF.Sqrt, scale=1.0 / D, bias=epsc[:, 0:1]),
                  reads=[rss], writes=[rss])
            cx.op("dve", lambda e: e.reciprocal(rstd[:, 0:1], std[:, 0:1]), reads=[rss], writes=[rss])

        xov = x_own.ap().rearrange("(t p) d -> t p d", p=128)
        xtv = x_oth.ap().rearrange("(t p) d -> t p d", p=128)
        w_in_v = w_in.ap().rearrange("(k p) c -> p k c", p=128)
        w_out_v = w_out.ap().rearrange("(k p) c -> p k c", p=128)

        with ExitStack() as st:
            gm = sb(st, "gm", [128, D])
            cosT = sb(st, "cosT", [128, S])
            sinT = sb(st, "sinT", [128, S])
            rotT = sb(st, "rotT", [128, 128])
            gqc = sb(st, "gqc", [128, 1])
            gkc = sb(st, "gkc", [128, 1])
            rK = Res("p1c")
            for dst, src in ((gm, g_mix), (cosT, c_cos), (sinT, c_sin), (rotT, c_rotT), (gqc, gq_col), (gkc, gk_col)):
                cx.dma("sp", (lambda d_, s_: (lambda e: e.dma_start(out=d_[:], in_=s_[:, :])))(dst, src), writes=[rK])
            cx.barrier()
            rK.const = True
            xt = [sb(st, "xt%d" % i, [128, D]) for i in range(2)]
            rxt = [Res("xt%d" % i) for i in range(2)]
            hb = [sb(st, "hb%d" % i, [128, D], BF16) for i in range(2)]
            rhb = [Res("hb%d" % i) for i in range(2)]
            hT = [sb(st, "hT%d" % i, [128, 16, 512], BF16) for i in range(2)]
            rhT = [Res("hT%d" % i) for i in range(2)]
            ss = sb(st, "ss", [128, 1]); std = sb(st, "std", [128, 1]); rstd = sb(st, "rstd", [128, 1])
            rss = Res("ss")
            qf = sb(st, "qf", [128, 512]); rqf = Res("qf")
            sq = sb(st, "sq", [128, 512]); rsq = Res("sq")
            sd = sb(st, "sd", [128, 512]); rsd = Res("sd")
            rs_ = sb(st, "rs_", [128, 512]); rrs = Res("rs_")
            qn = sb(st, "qn", [128, 512]); rqn = Res("qn")
            t1 = sb(st, "t1", [128, 512]); rt1 = Res("t1")
            t2 = sb(st, "t2", [128, 512]); rt2 = Res("t2")
            ob = [sb(st, "ob%d" % i, [128, 512], BF16) for i in range(3)]
            rob = [Res("ob%d" % i) for i in range(3)]
            obi = 0
            tcount = 0
            for ci in range(8):
                own = ci < 4
                hTc = hT[ci % 2]; rhTc = rhT[ci % 2]
                for t in range(4):
                    tg = ci * 4 + t
                    xi = tg % 2
                    src_x = xov[tg] if own else xtv[tg - 16]
                    cx.dma("sp", lambda e: e.dma_start(out=xt[xi][:], in_=src_x), writes=[rxt[xi]])
                    rmsnorm_rstd((ss, rss, std, rstd), xt[xi][:], rxt[xi], hb[xi][:], rhb[xi])
                    cx.op("dve", lambda e: e.scalar_tensor_tensor(out=hb[xi][:], in0=xt[xi][:], scalar=rstd[:, 0:1], in1=gm[:],
                                                                  op0=ALU.mult, op1=ALU.mult),
                          reads=[rxt[xi], rss], writes=[rhb[xi]])
                    for k4 in range(4):
                        pi = tcount % 2; tcount += 1
                        for kk in range(4):
                            k = k4 * 4 + kk
                            cx.op("pe", lambda e: e.transpose(PT[pi][:, kk * 128:(kk + 1) * 128], hb[xi][:, k * 128:(k + 1) * 128], identb[:]),
                                  reads=[rhb[xi]], writes=[rPT[pi]])
                        dst = hTc[:, k4 * 4:(k4 + 1) * 4, t * 128:(t + 1) * 128]
                        srcp = PT[pi][:, :].rearrange("p (k c) -> p k c", k=4)
                        if pi == 1:
                            cx.op("dve", lambda e: e.tensor_copy(dst, srcp), reads=[rPT[pi]], writes=[rhTc])
                        else:
                            cx.op("act", lambda e: e.activation(out=dst, in_=srcp, func=AF.Copy), reads=[rPT[pi]], writes=[rhTc])
                if own:
                    cgs = list(range(12))
                elif ci in (4, 7):
                    cgs = [4, 5, 10, 11]
                else:
                    cgs = [10, 11]
                ws = WStream([(w_in_v[:, :, cg * 256:(cg + 1) * 256], 16) for cg in cgs])
                for cg in cgs:
                    wv, rw = ws.next()
                    if cg in (5, 11):
                        for t in range(4):
                            if cg == 5 and not own and not ((ci == 4 and t == 0) or (ci == 7 and t == 3)):
                                continue
                            pb = (t % 2)
                            for k in range(16):
                                cx.op("pe", lambda e: e.matmul(PS[pb][:, 0:256], hTc[:, k, t * 128:(t + 1) * 128], wv[:, k, :],
                                                                start=(k == 0), stop=(k == 15)),
                                      reads=[rhTc, rw], writes=[rPS[pb]])
                            oi = obi % 3; obi += 1
                            cx.op("act", lambda e: e.activation(out=ob[oi][:, 0:256], in_=PS[pb][:, 0:256], func=AF.Copy),
                                  reads=[rPS[pb]], writes=[rob[oi]])
                            if cg == 11:
                                r0 = ci * 512 + t * 128
                                cx.dma("pool", lambda e: e.dma_start(out=VBs[r0:r0 + 128, :], in_=ob[oi][:, 0:256]), reads=[rob[oi]])
                            else:
                                if own:
                                    r0 = 128 + ci * 512 + t * 128
                                elif ci == 4:
                                    r0 = SA - 128
                                else:
                                    r0 = 0
                                cx.dma("pool", lambda e: e.dma_start(out=VAs[r0:r0 + 128, :], in_=ob[oi][:, 0:256]), reads=[rob[oi]])
                        continue
                    for half in range(2):
                        col = cg * 256 + half * 128
                        if col < 1024:
                            typ = "A"; dsts = [(QTd[col // 128, :, ci * 512:(ci + 1) * 512], 0, 512)]
                        elif col < 1280:
                            typ = "A"; g_ = (col - 1024) // 128
                            if own:
                                dsts = [(KTA[g_, :, 128 + ci * 512:128 + (ci + 1) * 512], 0, 512)]
                            elif ci == 4:
                                dsts = [(KTA[g_, :, SA - 128:SA], 0, 128)]
                            else:
                                dsts = [(KTA[g_, :, 0:128], 384, 512)]
                        elif col < 2560:
                            typ = "Bq"; dsts = [(QTd[8 + (col - 1536) // 128, :, ci * 512:(ci + 1) * 512], 0, 512)]
                        else:
                            typ = "Bk"; dsts = [(KTB[(col - 2560) // 128, :, ci * 512:(ci + 1) * 512], 0, 512)]
                        pb = 2 + (half % 2)
                        for k in range(16):
                            cx.op("pe", lambda e: e.matmul(PS[pb][:, :], wv[:, k, half * 128:(half + 1) * 128], hTc[:, k, :],
                                                            start=(k == 0), stop=(k == 15)),
                                  reads=[rhTc, rw], writes=[rPS[pb]])
                        oi = obi % 3; obi += 1
                        if typ == "A":
                            cx.op("act", lambda e: e.activation(out=ob[oi][:], in_=PS[pb][:, :], func=AF.Copy),
                                  reads=[rPS[pb]], writes=[rob[oi]])
                        else:
                            gc = gqc if typ == "Bq" else gkc
                            cx.op("dve", lambda e: e.tensor_copy(qf[:], PS[pb][:, :]), reads=[rPS[pb]], writes=[rqf])
                            cx.op("pool", lambda e: e.tensor_tensor(out=sq[:], in0=qf[:], in1=qf[:], op=ALU.mult), reads=[rqf], writes=[rsq])
                            cx.op("pe", lambda e: e.matmul(PS[4][:, :], ones[:], sq[:], start=True, stop=True), reads=[rsq], writes=[rPS[4]])
                            cx.op("act", lambda e: e.activation(out=sd[:], in_=PS[4][:, :], func=AF.Sqrt, scale=1.0 / HD, bias=eps128[:, 0:1]),
                                  reads=[rPS[4]], writes=[rsd])
                            cx.op("dve", lambda e: e.reciprocal(rs_[:], sd[:]), reads=[rsd], writes=[rrs])
                            cx.op("dve", lambda e: e.scalar_tensor_tensor(out=qn[:], in0=qf[:], scalar=gc[:, 0:1], in1=rs_[:], op0=ALU.mult, op1=ALU.mult),
                                  reads=[rqf, rrs], writes=[rqn])
                            cx.op("pe", lambda e: e.matmul(PS[4][:, :], rotT[:], qn[:], start=True, stop=True), reads=[rqn], writes=[rPS[4]])
                            cx.op("pool", lambda e: e.tensor_tensor(out=t1[:], in0=qn[:], in1=cosT[:, ci * 512:(ci + 1) * 512], op=ALU.mult),
                                  reads=[rqn], writes=[rt1])
                            cx.op("dve", lambda e: e.tensor_tensor(out=t2[:], in0=PS[4][:, :], in1=sinT[:, ci * 512:(ci + 1) * 512], op=ALU.mult),
                                  reads=[rPS[4]], writes=[rt2])
                            cx.op("dve", lambda e: e.tensor_tensor(out=ob[oi][:], in0=t1[:], in1=t2[:], op=ALU.add),
                                  reads=[rt1, rt2], writes=[rob[oi]])
                        for (dap, c0_, c1_) in dsts:
                            cx.dma("pool", lambda e: e.dma_start(out=dap, in_=ob[oi][:, c0_:c1_]), reads=[rob[oi]])
            cx.barrier()

        with ExitStack() as st:
            biasA = sb(st, "biasA", [128, 24, 384])
            negMB = sb(st, "negMB", [128, 1])
            gqa = sb(st, "gqa", [128, 2]); gneg = sb(st, "gneg", [128, 2]); rowm = sb(st, "rowm", [2, 128]); mx = sb(st, "mx", [2, 1]); mprod = sb(st, "mprod", [1, 1])
            rK = Res("p2c")
            cx.dma("sp", lambda e: e.dma_start(out=biasA[:].rearrange("p h c -> p (h c)"), in_=c_bias[:, :]), writes=[rK])
            cx.dma("sp", lambda e: e.dma_start(out=gqa[:, 0:1], in_=gq_col[:, :]), writes=[rK])
            cx.dma("sp", lambda e: e.dma_start(out=gqa[:, 1:2], in_=gk_col[:, :]), writes=[rK])
            cx.op("dve", lambda e: e.tensor_scalar(out=gneg[:], in0=gqa[:], scalar1=-1.0, scalar2=None, op0=ALU.mult), reads=[rK], writes=[rK])
            cx.op("dve", lambda e: e.tensor_tensor(out=gqa[:], in0=gqa[:], in1=gneg[:], op=ALU.max), reads=[rK], writes=[rK])
            cx.op("pe", lambda e: e.transpose(PS[4][0:2, 0:128], gqa[:, 0:2], identf[:]), reads=[rK], writes=[rPS[4]])
            cx.op("dve", lambda e: e.tensor_copy(rowm[:], PS[4][0:2, 0:128]), reads=[rPS[4]], writes=[rK])
            cx.op("dve", lambda e: e.reduce_max(out=mx[:, 0:1], in_=rowm[:], axis=AX.X), reads=[rK], writes=[rK])
            cx.op("pe", lambda e: e.transpose(PS[4][0:1, 0:2], mx[0:2, 0:1], identf[0:2, 0:2]), reads=[rK], writes=[rPS[4]])
            cx.op("dve", lambda e: e.tensor_copy(rowm[0:1, 0:2], PS[4][0:1, 0:2]), reads=[rPS[4]], writes=[rK])
            cx.op("dve", lambda e: e.scalar_tensor_tensor(out=mprod[:], in0=rowm[0:1, 0:1], scalar=-SCALE * HD, in1=rowm[0:1, 1:2],
                                                          op0=ALU.mult, op1=ALU.mult), reads=[rK], writes=[rK])
            cx.op("pe", lambda e: e.matmul(PS[4][:, 0:1], ones[0:1, :], mprod[0:1, 0:1], start=True, stop=True), reads=[rK], writes=[rPS[4]])
            cx.op("dve", lambda e: e.tensor_copy(negMB[:], PS[4][:, 0:1]), reads=[rPS[4]], writes=[rK])
            cx.barrier()
            rK.const = True

            KT = sb(st, "KT", [128, S], BF16); rKT = Res("KT")
            Vt = sb(st, "Vt", [128, NT, 128], BF16); rVt = Res("Vt")
            QT = [sb(st, "QT%d" % i, [128, SH], BF16) for i in range(4)]
            rQT = [Res("QT%d" % i) for i in range(4)]
            sbt = [sb(st, "sbt%d" % i, [128, 384]) for i in range(2)]; rsbt = [Res("sbt%d" % i) for i in range(2)]
            Pb = [sb(st, "Pb%d" % i, [128, 512], BF16) for i in range(3)]; rPb = [Res("Pb%d" % i) for i in range(3)]
            PTs = [sb(st, "PTs%d" % i, [128, 512], BF16) for i in range(3)]; rPTs = [Res("PTs%d" % i) for i in range(3)]
            Og = [sb(st, "Og%d" % i, [128, 512], BF16) for i in range(2)]; rOg = [Res("Og%d" % i) for i in range(2)]
            sm = [sb(st, "sm%d" % i, [128, 16]) for i in range(2)]; rsm = [Res("sm%d" % i) for i in range(2)]
            cnt = {"s": 0, "p": 0, "o": 0, "og": 0, "sm": 0, "sb": 0}

            for grp in ("A", "B"):
                for g in range(2):
                    if grp == "A":
                        cx.dma("sp", lambda e: e.dma_start(out=KT[:, 0:SA], in_=KTA[g, :, :]), writes=[rKT])
                        vsrc = VAs.ap().rearrange("(t p) c -> p t c", p=128)
                        for vq in range(3):
                            cx.dma("sp", lambda e: e.dma_start(out=Vt[:, vq * 6:(vq + 1) * 6, :], in_=vsrc[:, vq * 6:(vq + 1) * 6, g * 128:(g + 1) * 128]),
                                   writes=[rVt])
                    else:
                        cx.dma("sp", lambda e: e.dma_start(out=KT[:], in_=KTB[g, :, :]), writes=[rKT])
                        vsrc = VBs.ap().rearrange("(t p) c -> p t c", p=128)
                        for vq in range(4):
                            cx.dma("sp", lambda e: e.dma_start(out=Vt[:, vq * 8:(vq + 1) * 8, :], in_=vsrc[:, vq * 8:(vq + 1) * 8, g * 128:(g + 1) * 128]),
                                   writes=[rVt])
                    for hh in range(4):
                        qcc = (g * 4 + hh) if grp == "A" else (8 + g * 4 + hh)
                        cx.dma("sp", (lambda hh_, qcc_: (lambda e: e.dma_start(out=QT[hh_][:], in_=QTd[qcc_, :, :])))(hh, qcc), writes=[rQT[hh]])
                    for n in range(NTH):
                        ogi = cnt["og"] % 2; cnt["og"] += 1
                        for hh in range(4):
                            h = g * 4 + hh
                            smi = cnt["sm"] % 2; cnt["sm"] += 1
                            smt = sm[smi]; rsmt = rsm[smi]
                            po = 2 + (cnt["o"] % 2); cnt["o"] += 1
                            if grp == "A":
                                W = 384
                                bi = 0 if n == 0 else (2 if n == NTH - 1 else 1)
                                psi = cnt["s"] % 2; cnt["s"] += 1
                                cx.op("pe", lambda e: e.matmul(PS[psi][:, 0:W], QT[hh][:, n * 128:(n + 1) * 128], KT[:, n * 128:n * 128 + W],
                                                                start=True, stop=True), reads=[rQT[hh], rKT], writes=[rPS[psi]])
                                si = cnt["sb"] % 2; cnt["sb"] += 1
                                cx.op("dve", lambda e: e.scalar_tensor_tensor(out=sbt[si][:, 0:W], in0=PS[psi][:, 0:W], scalar=SCALE,
                                                                              in1=biasA[:, bi * 8 + h, :], op0=ALU.mult, op1=ALU.add),
                                      reads=[rPS[psi]], writes=[rsbt[si]])
                                cx.op("dve", lambda e: e.reduce_max(out=smt[:, 0:1], in_=sbt[si][:, 0:W], axis=AX.X), reads=[rsbt[si]], writes=[rsmt])
                                cx.op("dve", lambda e: e.tensor_tensor(out=smt[:, 1:2], in0=smt[:, 0:1], in1=sinkb[:, h:h + 1], op=ALU.max),
                                      reads=[rsmt], writes=[rsmt])
                                cx.op("dve", lambda e: e.tensor_scalar(out=smt[:, 2:3], in0=smt[:, 1:2], scalar1=-1.0, scalar2=None, op0=ALU.mult),
                                      reads=[rsmt], writes=[rsmt])
                                pi = cnt["p"] % 3; cnt["p"] += 1
                                cx.op("act", lambda e: e.activation(out=Pb[pi][:, 0:W], in_=sbt[si][:, 0:W], func=AF.Exp, bias=smt[:, 2:3],
                                                                    accum_out=smt[:, 3:4]), reads=[rsbt[si], rsmt], writes=[rPb[pi], rsmt])
                                cx.op("act", lambda e: e.activation(out=smt[:, 4:5], in_=sinkb[:, h:h + 1], func=AF.Exp, bias=smt[:, 2:3]),
                                      reads=[rsmt], writes=[rsmt])
                                cx.op("dve", lambda e: e.tensor_tensor(out=smt[:, 5:6], in0=smt[:, 3:4], in1=smt[:, 4:5], op=ALU.add),
                                      reads=[rsmt], writes=[rsmt])
                                cx.op("dve", lambda e: e.reciprocal(smt[:, 6:7], smt[:, 5:6]), reads=[rsmt], writes=[rsmt])
                                pti = pi % 2
                                for j in range(3):
                                    cx.op("pe", lambda e: e.transpose(PT[pti][:, j * 128:(j + 1) * 128], Pb[pi][:, j * 128:(j + 1) * 128], identb[:]),
                                          reads=[rPb[pi]], writes=[rPT[pti]])
                                cx.op("dve", lambda e: e.tensor_copy(PTs[pi][:, 0:W], PT[pti][:, 0:W]), reads=[rPT[pti]], writes=[rPTs[pi]])
                                for j in range(3):
                                    cx.op("pe", lambda e: e.matmul(PS[po][:, 0:128], PTs[pi][:, j * 128:(j + 1) * 128], Vt[:, n + j, :],
                                                                    start=(j == 0), stop=(j == 2)), reads=[rPTs[pi], rVt], writes=[rPS[po]])
                            else:
                                for kc in range(8):
                                    psi = cnt["s"] % 2; cnt["s"] += 1
                                    cx.op("pe", lambda e: e.matmul(PS[psi][:, :], QT[hh][:, n * 128:(n + 1) * 128], KT[:, kc * 512:(kc + 1) * 512],
                                                                    start=True, stop=True), reads=[rQT[hh], rKT], writes=[rPS[psi]])
                                    pi = cnt["p"] % 3; cnt["p"] += 1
                                    cx.op("act", lambda e: e.activation(out=Pb[pi][:], in_=PS[psi][:, :], func=AF.Exp, bias=negMB[:, 0:1], scale=SCALE,
                                                                        accum_out=smt[:, 8 + kc:9 + kc]), reads=[rPS[psi]], writes=[rPb[pi], rsmt])
                                    pti = pi % 2
                                    for j in range(4):
                                        cx.op("pe", lambda e: e.transpose(PT[pti][:, j * 128:(j + 1) * 128], Pb[pi][:, j * 128:(j + 1) * 128], identb[:]),
                                              reads=[rPb[pi]], writes=[rPT[pti]])
                                    cx.op("dve", lambda e: e.tensor_copy(PTs[pi][:], PT[pti][:, :]), reads=[rPT[pti]], writes=[rPTs[pi]])
                                    for j in range(4):
                                        cx.op("pe", lambda e: e.matmul(PS[po][:, 0:128], PTs[pi][:, j * 128:(j + 1) * 128], Vt[:, kc * 4 + j, :],
                                                                        start=(kc == 0 and j == 0), stop=(kc == 7 and j == 3)),
                                              reads=[rPTs[pi], rVt], writes=[rPS[po]])
                                cx.op("dve", lambda e: e.reduce_sum(out=smt[:, 5:6], in_=smt[:, 8:16], axis=AX.X), reads=[rsmt], writes=[rsmt])
                                cx.op("dve", lambda e: e.reciprocal(smt[:, 6:7], smt[:, 5:6]), reads=[rsmt], writes=[rsmt])
                            cx.op("act", lambda e: e.activation(out=Og[ogi][:, hh * 128:(hh + 1) * 128], in_=PS[po][:, 0:128], func=AF.Copy,
                                                                scale=smt[:, 6:7]), reads=[rPS[po], rsmt], writes=[rOg[ogi]])
                        c0 = (0 if grp == "A" else 1024) + g * 512
                        cx.dma("pool", lambda e: e.dma_start(out=OS[n * 128:(n + 1) * 128, c0:c0 + 512], in_=Og[ogi][:]), reads=[rOg[ogi]])
            cx.barrier()

        osv = OS.ap().rearrange("(t p) d -> t p d", p=128)
        yv = Yr.ap().rearrange("(t p) d -> t p d", p=128)
        h2hv = H2h.ap().rearrange("(t p) d -> t p d", p=128)
        affhv = AFFh.ap().rearrange("(t p) d -> t p d", p=128)
        with ExitStack() as st:
            gf = sb(st, "gf", [128, D])
            wr = sb(st, "wr", [128, 16, NE])
            rK = Res("p3c")
            cx.dma("sp", lambda e: e.dma_start(out=gf[:], in_=g_ffn[:, :]), writes=[rK])
            cx.dma("sp", lambda e: e.dma_start(out=wr[:], in_=w_router.ap().rearrange("(k p) c -> p k c", p=128)), writes=[rK])
            cx.barrier()
            rK.const = True
            x1 = [sb(st, "x1_%d" % i, [128, D]) for i in range(4)]; rx1 = [Res("x1_%d" % i) for i in range(4)]
            cx.dma("sp", lambda e: e.dma_start(out=x1[3][:], in_=c_zero[:, :]), writes=[rx1[3]])
            for tg in range(NTH, NT):
                cx.dma("pool", lambda e: e.dma_start(out=yv[tg], in_=x1[3][:]), reads=[rx1[3]])
            otb = [sb(st, "otb%d" % i, [128, D], BF16) for i in range(2)]; rotb = [Res("otb%d" % i) for i in range(2)]
            oT = sb(st, "oT", [128, 16, 512], BF16); roT = Res("oT")
            h2t = [sb(st, "h2t%d" % i, [128, D]) for i in range(2)]; rh2t = [Res("h2t%d" % i) for i in range(2)]
            h2b = [sb(st, "h2b%d" % i, [128, D], BF16) for i in range(2)]; rh2b = [Res("h2b%d" % i) for i in range(2)]
            aft = [sb(st, "aft%d" % i, [128, NE]) for i in range(2)]; raft = [Res("aft%d" % i) for i in range(2)]
            h2T = sb(st, "h2T", [128, 16, 128]); rh2T = Res("h2T")
            jk = sb(st, "jk", [128, D], BF16); rjk = Res("jk")
            ss = sb(st, "ss3", [128, 1]); std = sb(st, "std3", [128, 1]); rstd = sb(st, "rstd3", [128, 1]); rss = Res("ss3")
            sm3 = sb(st, "sm3", [128, 8]); rsm3 = Res("sm3")
            ex = sb(st, "ex", [128, NE]); rex = Res("ex")
            tcount = 0
            for tc in range(4):
                for t in range(4):
                    tg = tc * 4 + t
                    oi = tg % 2
                    cx.dma("sp", lambda e: e.dma_start(out=otb[oi][:], in_=osv[tg]), writes=[rotb[oi]])
                    cx.dma("sp", lambda e: e.dma_start(out=x1[t][:], in_=xov[tg]), writes=[rx1[t]])
                    for k4 in range(4):
                        pi = tcount % 2; tcount += 1
                        for kk in range(4):
                            k = k4 * 4 + kk
                            cx.op("pe", lambda e: e.transpose(PT[pi][:, kk * 128:(kk + 1) * 128], otb[oi][:, k * 128:(k + 1) * 128], identb[:]),
                                  reads=[rotb[oi]], writes=[rPT[pi]])
                        dst = oT[:, k4 * 4:(k4 + 1) * 4, t * 128:(t + 1) * 128]
                        srcp = PT[pi][:, :].rearrange("p (k c) -> p k c", k=4)
                        if pi == 1:
                            cx.op("dve", lambda e: e.tensor_copy(dst, srcp), reads=[rPT[pi]], writes=[roT])
                        else:
                            cx.op("act", lambda e: e.activation(out=dst, in_=srcp, func=AF.Copy), reads=[rPT[pi]], writes=[roT])
                ws = WStream([(w_out_v[:, :, cg * 256:(cg + 1) * 256], 16) for cg in range(8)])
                for cg in range(8):
                    wv, rw = ws.next()
                    for t in range(4):
                        pb = 2 + (t % 2)
                        for k in range(16):
                            cx.op("pe", lambda e: e.matmul(PS[pb][:, 0:256], oT[:, k, t * 128:(t + 1) * 128], wv[:, k, :], start=(k == 0), stop=(k == 15)),
                                  reads=[roT, rw], writes=[rPS[pb]])
                        cx.op("dve", lambda e: e.tensor_tensor(out=x1[t][:, cg * 256:(cg + 1) * 256], in0=PS[pb][:, 0:256],
                                                               in1=x1[t][:, cg * 256:(cg + 1) * 256], op=ALU.add),
                              reads=[rPS[pb], rx1[t]], writes=[rx1[t]])
                for t in range(4):
                    tg = tc * 4 + t
                    hi = tg % 2
                    cx.dma("pool", lambda e: e.dma_start(out=yv[tg], in_=x1[t][:]), reads=[rx1[t]])
                    rmsnorm_rstd((ss, rss, std, rstd), x1[t][:], rx1[t], jk[:], rjk)
                    cx.op("dve", lambda e: e.scalar_tensor_tensor(out=h2t[hi][:], in0=x1[t][:], scalar=rstd[:, 0:1], in1=gf[:],
                                                                  op0=ALU.mult, op1=ALU.mult), reads=[rx1[t], rss], writes=[rh2t[hi]])
                    cx.op("pool", lambda e: e.tensor_copy(h2b[hi][:], h2t[hi][:]), reads=[rh2t[hi]], writes=[rh2b[hi]])
                    cx.dma("pool", lambda e: e.dma_start(out=h2hv[tg], in_=h2b[hi][:]), reads=[rh2b[hi]])
                    for k4 in range(4):
                        pb = k4 % 2
                        for kk in range(4):
                            k = k4 * 4 + kk
                            cx.op("pe", lambda e: e.transpose(PS[pb][:, kk * 128:(kk + 1) * 128], h2t[hi][:, k * 128:(k + 1) * 128], identf[:]),
                                  reads=[rh2t[hi]], writes=[rPS[pb]])
                        dst = h2T[:, k4 * 4:(k4 + 1) * 4, :]
                        srcp = PS[pb][:, :].rearrange("p (k c) -> p k c", k=4)
                        if k4 % 2 == 0:
                            cx.op("dve", lambda e: e.tensor_copy(dst, srcp), reads=[rPS[pb]], writes=[rh2T])
                        else:
                            cx.op("act", lambda e: e.activation(out=dst, in_=srcp, func=AF.Copy), reads=[rPS[pb]], writes=[rh2T])
                    for k in range(16):
                        cx.op("pe", lambda e: e.matmul(PS[4][:, 0:NE], h2T[:, k, :], wr[:, k, :], start=(k == 0), stop=(k == 15)),
                              reads=[rh2T], writes=[rPS[4]])
                    cx.op("dve", lambda e: e.reduce_max(out=sm3[:, 0:1], in_=PS[4][:, 0:NE], axis=AX.X), reads=[rPS[4]], writes=[rsm3])
                    cx.op("dve", lambda e: e.tensor_scalar(out=sm3[:, 1:2], in0=sm3[:, 0:1], scalar1=-1.0, scalar2=None, op0=ALU.mult),
                          reads=[rsm3], writes=[rsm3])
                    cx.op("act", lambda e: e.activation(out=ex[:], in_=PS[4][:, 0:NE], func=AF.Exp, bias=sm3[:, 1:2], accum_out=sm3[:, 2:3]),
                          reads=[rPS[4], rsm3], writes=[rex, rsm3])
                    cx.op("dve", lambda e: e.reciprocal(sm3[:, 3:4], sm3[:, 2:3]), reads=[rsm3], writes=[rsm3])
                    cx.op("dve", lambda e: e.tensor_scalar(out=aft[hi][:], in0=ex[:], scalar1=sm3[:, 3:4], scalar2=None, op0=ALU.mult),
                          reads=[rex, rsm3], writes=[raft[hi]])
                    cx.dma("pool", lambda e: e.dma_start(out=affhv[tg], in_=aft[hi][:]), reads=[raft[hi]])
            cx.barrier()

        rX = Res("xchg")
        for k in range(4):
            cx.coll("cc%d" % k, lambda e: e.collective_compute("AllGather", ALU.bypass, replica_groups=PAIRS,
                                                               ins=[H2h[k * 512:(k + 1) * 512, :]], outs=[H2st[k].ap().opt()]),
                    writes=[rX])
        cx.coll("cc4", lambda e: e.collective_compute("AllGather", ALU.bypass, replica_groups=PAIRS,
                                                      ins=[AFFh.ap().opt()], outs=[AFFf.ap().opt()]), writes=[rX])
        cx.barrier()
        for k in range(4):
            for hf in range(2):
                r0 = hf * SH + k * 512
                cx.dma("sp", lambda e: e.dma_start(out=H2f[r0:r0 + 512, :], in_=H2st[k][hf * 512:(hf + 1) * 512, :]))
        cx.barrier()

        pmTok = sb(es, "pmTok", [128, NT, NEL])
        rpm = Res("pmTok")
        with ExitStack() as st:
            affAll = sb(st, "affAll", [128, NT, NE]); rAff = Res("affAll")
            sel16 = sb(st, "sel16", [NE, NEL])
            affT = sb(st, "affT", [NE, S]); raffT = Res("affT")
            junk = sb(st, "junk4", [NE, S]); rjunk = Res("junk4")
            onesr = sb(st, "onesr", [NE, S])
            thr = sb(st, "thr", [NE, 1], U32); cand = sb(st, "cand", [NE, 1], U32); selm = sb(st, "selm", [NE, 1], U32)
            cntt = sb(st, "cntt", [NE, 1])
            rT = Res("thr")
            afv = AFFf.ap().rearrange("(t p) c -> p t c", p=128)
            for vq in range(4):
                cx.dma("sp", lambda e: e.dma_start(out=affAll[:, vq * 8:(vq + 1) * 8, :], in_=afv[:, vq * 8:(vq + 1) * 8, :]), writes=[rAff])
            cx.dma("sp", lambda e: e.dma_start(out=sel16[:], in_=c_sel16[:, :]), writes=[rT])
            for tg in range(NT):
                pb = tg % 2
                cx.op("pe", lambda e: e.transpose(PS[pb][0:NE, 0:128], affAll[:, tg, :], identf[:]), reads=[rAff], writes=[rPS[pb]])
                cx.op("dve", lambda e: e.tensor_copy(affT[:, tg * 128:(tg + 1) * 128], PS[pb][0:NE, 0:128]), reads=[rPS[pb]], writes=[raffT])
            cx.op("dve", lambda e: e.memset(thr[:], 0), writes=[rT])
            cx.op("dve", lambda e: e.memset(onesr[:], 1.0), writes=[rT])
            for b in range(30, -1, -1):
                cx.op("dve", lambda e: e.tensor_scalar(out=cand[:], in0=thr[:], scalar1=(1 << b), scalar2=None, op0=ALU.bitwise_or),
                      reads=[rT], writes=[rT])
                cx.op("dve", lambda e: e.tensor_scalar(out=junk[:], in0=affT[:], scalar1=cand[:].bitcast(F32)[:, 0:1], scalar2=None,
                                                       op0=ALU.is_ge, op1=ALU.add, accum_out=cntt[:, 0:1]),
                      reads=[rT, raffT], writes=[rjunk, rT])
                cx.op("dve", lambda e: e.tensor_scalar(out=selm[:], in0=cntt[:], scalar1=float(CAP), scalar2=None, op0=ALU.is_ge),
                      reads=[rT], writes=[rT])
                cx.op("dve", lambda e: e.copy_predicated(out=thr[:], mask=selm[:], data=cand[:]), reads=[rT], writes=[rT])
            cx.op("dve", lambda e: e.tensor_scalar(out=junk[:], in0=affT[:], scalar1=thr[:].bitcast(F32)[:, 0:1], scalar2=None, op0=ALU.is_ge),
                  reads=[rT, raffT], writes=[rjunk])
            cx.op("dve", lambda e: e.tensor_tensor_scan(out=affT[:], data0=onesr[:], data1=junk[:], initial=0.0, op0=ALU.mult, op1=ALU.add),
                  reads=[rjunk, rT], writes=[raffT])
            cx.op("dve", lambda e: e.tensor_tensor(out=affT[:], in0=affT[:], in1=junk[:], op=ALU.mult), reads=[rjunk, raffT], writes=[raffT])
            cx.op("dve", lambda e: e.tensor_scalar(out=affT[:], in0=affT[:], scalar1=-1.0, scalar2=None, op0=ALU.add), reads=[raffT], writes=[raffT])
            for tg in range(NT):
                pb = tg % 2
                cx.op("pe", lambda e: e.matmul(PS[pb][:, 0:NEL], affT[:, tg * 128:(tg + 1) * 128], sel16[:, :], start=True, stop=True),
                      reads=[raffT, rT], writes=[rPS[pb]])
                cx.op("dve", lambda e: e.tensor_copy(pmTok[:, tg, :], PS[pb][:, 0:NEL]), reads=[rPS[pb]], writes=[rpm])
            cx.barrier()

        rY = Res("Y")
        rH2 = Res("H2", const=True)
        with ExitStack() as st:
            iot = sb(st, "iot", [128, CAP])
            tokid = sb(st, "tokid", [128, NT, 2], BF16)
            selB = sb(st, "selB", [128, NEL, NE])
            offc = sb(st, "offc", [128, 1])
            rK = Res("p5c")
            cx.dma("sp", lambda e: e.dma_start(out=iot[:], in_=c_iota[:, :]), writes=[rK])
            cx.dma("sp", lambda e: e.dma_start(out=tokid[:].rearrange("p t c -> p (t c)"), in_=c_tokid[:, :]), writes=[rK])
            cx.dma("sp", lambda e: e.dma_start(out=selB[:].rearrange("p j c -> p (j c)"), in_=c_selB[:, :]), writes=[rK])
            cx.dma("sp", lambda e: e.dma_start(out=offc[:], in_=c_off[:, :]), writes=[rK])
            cx.barrier()
            rK.const = True
            OH = [sb(st, "OH%d" % i, [128, CAP], BF16) for i in range(2)]; rOH = [Res("OH%d" % i) for i in range(2)]
            idr = sb(st, "idr", [2, CAP]); ridr = Res("idr")
            idf = sb(st, "idf", [128, 4]); idp = sb(st, "idp", [128, 8]); idl = sb(st, "idl", [128, 4]); idm = sb(st, "idm", [128, 4])
            ridf = Res("idf")
            idi = [sb(st, "idi%d" % i, [128, 4], I32) for i in range(2)]; ridi = [Res("idi%d" % i) for i in range(2)]
            idc = [sb(st, "idc%d" % i, [128, 4], I32) for i in range(2)]; ridc = [Res("idc%d" % i) for i in range(2)]
            gates = [sb(st, "gates%d" % i, [128, 4]) for i in range(2)]; rgates = [Res("gates%d" % i) for i in range(2)]
            xgb = [sb(st, "xgb%d" % i, [128, D], BF16) for i in range(4)]; rxgb = [Res("xgb%d" % i) for i in range(4)]
            afg = [sb(st, "afg%d" % i, [128, NE]) for i in range(4)]; rafg = [Res("afg%d" % i) for i in range(4)]
            gj = sb(st, "gj", [128, NE]); rgj = Res("gj")
            xgT = sb(st, "xgT", [128, 16, CAP], BF16); rxgT = Res("xgT")
            hTm = sb(st, "hTm", [128, 32, CAP], BF16); rhTm = Res("hTm")
            sg = [sb(st, "sg%d" % i, [128, CAP]) for i in range(2)]; rsg = [Res("sg%d" % i) for i in range(2)]
            eo = [sb(st, "eo%d" % i, [128, D]) for i in range(4)]; reo = [Res("eo%d" % i) for i in range(4)]
            cn = {"oh": 0, "tr": 0, "gu": 0}

            def prep_a(ex_):
                ei = ex_ % 2
                for tg in range(NT):
                    oi = cn["oh"] % 2; cn["oh"] += 1
                    cx.op("dve", lambda e: e.tensor_scalar(out=OH[oi][:], in0=iot[:], scalar1=pmTok[:, tg, ex_:ex_ + 1], scalar2=None, op0=ALU.is_equal),
                          reads=[rpm], writes=[rOH[oi]])
                    cx.op("pe", lambda e: e.matmul(PS[4][0:2, :], tokid[:, tg, :], OH[oi][:], start=(tg == 0), stop=(tg == NT - 1)),
                          reads=[rOH[oi]], writes=[rPS[4]])
                cx.op("dve", lambda e: e.tensor_copy(idr[:], PS[4][0:2, :]), reads=[rPS[4]], writes=[ridr])
                for c in range(4):
                    cx.op("pe", lambda e: e.transpose(PS[4][:, c * 2:(c + 1) * 2], idr[0:2, c * 128:(c + 1) * 128], identf[0:2, 0:2]),
                          reads=[ridr], writes=[rPS[4]])
                cx.op("dve", lambda e: e.tensor_copy(idp[:], PS[4][:, 0:8]), reads=[rPS[4]], writes=[ridf])
                pv = idp[:, :].rearrange("p (c two) -> p c two", two=2)
                cx.op("dve", lambda e: e.scalar_tensor_tensor(out=idf[:], in0=pv[:, :, 0], scalar=128.0, in1=pv[:, :, 1], op0=ALU.mult, op1=ALU.add),
                      reads=[ridf], writes=[ridf])
                cx.op("dve", lambda e: e.tensor_copy(idi[ei][:], idf[:]), reads=[ridf], writes=[ridi[ei]])
                cx.op("dve", lambda e: e.tensor_scalar(out=idl[:], in0=idf[:], scalar1=offc[:, 0:1], scalar2=None, op0=ALU.add), reads=[ridf], writes=[ridf])
                cx.op("dve", lambda e: e.tensor_scalar(out=idm[:], in0=idl[:], scalar1=float(S), scalar2=-float(S), op0=ALU.is_ge, op1=ALU.mult),
                      reads=[ridf], writes=[ridf])
                cx.op("dve", lambda e: e.tensor_tensor(out=idl[:], in0=idl[:], in1=idm[:], op=ALU.add), reads=[ridf], writes=[ridf])
                cx.op("dve", lambda e: e.tensor_copy(idc[ei][:], idl[:]), reads=[ridf], writes=[ridc[ei]])
                for c in range(4):
                    cx.dma("pool", lambda e: e.indirect_dma_start(out=xgb[c][:], out_offset=None, in_=H2f[:, :],
                                                                  in_offset=bass.IndirectOffsetOnAxis(ap=idi[ei][:, c:c + 1], axis=0)),
                           reads=[ridi[ei], rH2], writes=[rxgb[c]])
                    cx.dma("pool", lambda e: e.indirect_dma_start(out=afg[c][:], out_offset=None, in_=AFFf[:, :],
                                                                  in_offset=bass.IndirectOffsetOnAxis(ap=idi[ei][:, c:c + 1], axis=0)),
                           reads=[ridi[ei], rH2], writes=[rafg[c]])
                    cx.op("dve", lambda e: e.scalar_tensor_tensor(out=gj[:], in0=afg[c][:], scalar=1.0, in1=selB[:, ex_, :], op0=ALU.mult, op1=ALU.mult,
                                                                  accum_out=gates[ei][:, c:c + 1]),
                          reads=[rafg[c]], writes=[rgj, rgates[ei]])

            def prep_b(ex_):
                for c in range(4):
                    for k4 in range(4):
                        pi = cn["tr"] % 2; cn["tr"] += 1
                        for kk in range(4):
                            k = k4 * 4 + kk
                            cx.op("pe", lambda e: e.transpose(PT[pi][:, kk * 128:(kk + 1) * 128], xgb[c][:, k * 128:(k + 1) * 128], identb[:]),
                                  reads=[rxgb[c]], writes=[rPT[pi]])
                        dst = xgT[:, k4 * 4:(k4 + 1) * 4, c * 128:(c + 1) * 128]
                        srcp = PT[pi][:, :].rearrange("p (k c) -> p k c", k=4)
                        if pi == 1:
                            cx.op("dve", lambda e: e.tensor_copy(dst, srcp), reads=[rPT[pi]], writes=[rxgT])
                        else:
                            cx.op("act", lambda e: e.activation(out=dst, in_=srcp, func=AF.Copy), reads=[rPT[pi]], writes=[rxgT])

            specs = []
            for ex_ in range(NEL):
                wg_v = w_gate.ap()[ex_].rearrange("(k p) c -> p k c", p=128)
                wu_v = w_up.ap()[ex_].rearrange("(k p) c -> p k c", p=128)
                wd_v = w_down.ap()[ex_].rearrange("(k p) c -> p k c", p=128)
                for fg in range(16):
                    specs.append((wg_v[:, :, fg * 256:(fg + 1) * 256], 16))
                    specs.append((wu_v[:, :, fg * 256:(fg + 1) * 256], 16))
                for c4 in range(4):
                    for q in range(4):
                        specs.append((wd_v[:, q * 8:(q + 1) * 8, c4 * 512:(c4 + 1) * 512], 8))
            ws = WStream(specs)
            prep_a(0)
            prep_b(0)
            for ex_ in range(NEL):
                ei = ex_ % 2
                if ex_ + 1 < NEL:
                    prep_a(ex_ + 1)
                for fg in range(16):
                    wgv, rwg = ws.next()
                    wuv, rwu = ws.next()
                    for half in range(2):
                        f = fg * 2 + half
                        pg = cn["gu"] % 2; pu = 2 + (cn["gu"] % 2); cn["gu"] += 1
                        for k in range(16):
                            cx.op("pe", lambda e: e.matmul(PS[pg][:, :], wgv[:, k, half * 128:(half + 1) * 128], xgT[:, k, :], start=(k == 0), stop=(k == 15)),
                                  reads=[rwg, rxgT], writes=[rPS[pg]])
                        for k in range(16):
                            cx.op("pe", lambda e: e.matmul(PS[pu][:, :], wuv[:, k, half * 128:(half + 1) * 128], xgT[:, k, :], start=(k == 0), stop=(k == 15)),
                                  reads=[rwu, rxgT], writes=[rPS[pu]])
                        si = f % 2
                        cx.op("act", lambda e: e.activation(out=sg[si][:], in_=PS[pg][:, :], func=AF.Silu), reads=[rPS[pg]], writes=[rsg[si]])
                        cx.op("dve", lambda e: e.tensor_tensor(out=hTm[:, f, :], in0=sg[si][:], in1=PS[pu][:, :], op=ALU.mult),
                              reads=[rsg[si], rPS[pu]], writes=[rhTm])
                if ex_ + 1 < NEL:
                    prep_b(ex_ + 1)
                for c4 in range(4):
                    for q in range(4):
                        wdv, rwd = ws.next()
                        for f8 in range(8):
                            f = q * 8 + f8
                            for t in range(4):
                                pb = [4, 5, 2, 3][t]
                                cx.op("pe", lambda e: e.matmul(PS[pb][:, :], hTm[:, f, t * 128:(t + 1) * 128], wdv[:, f8, :],
                                                                start=(f == 0), stop=(f == 31)), reads=[rhTm, rwd], writes=[rPS[pb]])
                    for t in range(4):
                        pb = [4, 5, 2, 3][t]
                        if t % 2 == 0:
                            cx.op("act", lambda e: e.activation(out=eo[t][:, c4 * 512:(c4 + 1) * 512], in_=PS[pb][:, :], func=AF.Copy, scale=gates[ei][:, t:t + 1]),
                                  reads=[rPS[pb], rgates[ei]], writes=[reo[t]])
                        else:
                            cx.op("dve", lambda e: e.tensor_scalar(out=eo[t][:, c4 * 512:(c4 + 1) * 512], in0=PS[pb][:, :], scalar1=gates[ei][:, t:t + 1],
                                                                   scalar2=None, op0=ALU.mult), reads=[rPS[pb], rgates[ei]], writes=[reo[t]])
                for c in range(4):
                    cx.dma("pool", lambda e: e.indirect_dma_start(out=Yr[:, :], out_offset=bass.IndirectOffsetOnAxis(ap=idc[ei][:, c:c + 1], axis=0),
                                                                  in_=eo[c][:], in_offset=None, compute_op=ALU.add),
                           reads=[reo[c], ridc[ei]], writes=[rY])
            cx.barrier()

        for k in range(8):
            cx.coll("cc%d" % (5 + k), lambda e: e.collective_compute("AllGather", ALU.bypass, replica_groups=PAIRS,
                                                                    ins=[Yr[SH + k * 256:SH + (k + 1) * 256, :]], outs=[Gst[k].ap().opt()]),
                    writes=[rX])
        cx.barrier()

        outv = out.ap().rearrange("(t p) d -> t p d", p=128)
        with ExitStack() as st:
            gfi = sb(st, "gfi", [128, D])
            s01 = sb(st, "s01", [128, 2])
            rK = Res("p6c")
            cx.dma("sp", lambda e: e.dma_start(out=gfi[:], in_=g_fin[:, :]), writes=[rK])
            cx.dma("sp", lambda e: e.dma_start(out=s01[:], in_=c_s01[:, :]), writes=[rK])
            cx.barrier()
            rK.const = True
            yt = [sb(st, "yt%d" % i, [128, D]) for i in range(2)]; ryt = [Res("yt%d" % i) for i in range(2)]
            ga = [sb(st, "ga%d" % i, [128, D]) for i in range(2)]; rga = [Res("ga%d" % i) for i in range(2)]
            gb = [sb(st, "gb%d" % i, [128, D]) for i in range(2)]; rgb = [Res("gb%d" % i) for i in range(2)]
            ot = [sb(st, "ot%d" % i, [128, D]) for i in range(2)]; rot = [Res("ot%d" % i) for i in range(2)]
            ss = sb(st, "ss6", [128, 1]); std = sb(st, "std6", [128, 1]); rstd = sb(st, "rstd6", [128, 1]); rss = Res("ss6")
            for tg in range(NTH):
                i = tg % 2
                k = tg // 2; sub = tg % 2
                cx.dma("sp", lambda e: e.dma_start(out=yt[i][:], in_=yv[tg]), writes=[ryt[i]])
                cx.dma("sp", lambda e: e.dma_start(out=ga[i][:], in_=Gst[k][sub * 128:(sub + 1) * 128, :]), writes=[rga[i]])
                cx.dma("sp", lambda e: e.dma_start(out=gb[i][:], in_=Gst[k][256 + sub * 128:256 + (sub + 1) * 128, :]), writes=[rgb[i]])
                cx.op("dve", lambda e: e.scalar_tensor_tensor(out=yt[i][:], in0=ga[i][:], scalar=s01[:, 0:1], in1=yt[i][:], op0=ALU.mult, op1=ALU.add),
                      reads=[rga[i], ryt[i]], writes=[ryt[i]])
                cx.op("dve", lambda e: e.scalar_tensor_tensor(out=yt[i][:], in0=gb[i][:], scalar=s01[:, 1:2], in1=yt[i][:], op0=ALU.mult, op1=ALU.add),
                      reads=[rgb[i], ryt[i]], writes=[ryt[i]])
                rmsnorm_rstd((ss, rss, std, rstd), yt[i][:], ryt[i], ot[i][:], rot[i])
                cx.op("dve", lambda e: e.scalar_tensor_tensor(out=ot[i][:], in0=yt[i][:], scalar=rstd[:, 0:1], in1=gfi[:], op0=ALU.mult, op1=ALU.mult),
                      reads=[ryt[i], rss], writes=[rot[i]])
                cx.dma("pool", lambda e: e.dma_start(out=outv[tg], in_=ot[i][:]), reads=[rot[i]])
            cx.barrier()
    return nc


def _consts():
    bf = ml_dtypes.bfloat16
    c = {}
    c["c_identb"] = np.eye(128, dtype=np.float32).astype(bf)
    c["c_identf"] = np.eye(128, dtype=np.float32)
    rotT = np.zeros((128, 128), np.float32)
    for base in (0, 64):
        for i in range(32):
            m = base + i
            rotT[m + 32, m] = -1.0
            rotT[m, m + 32] = 1.0
    c["c_rotT"] = rotT
    c["c_ones"] = np.ones((128, 128), np.float32)
    c["c_iota"] = np.ascontiguousarray(np.broadcast_to(np.arange(CAP, dtype=np.float32)[None, :], (128, CAP)))
    tok = np.zeros((128, NT, 2), np.float32)
    tok[:, :, 0] = np.arange(NT)[None, :]
    tok[:, :, 1] = np.arange(128)[:, None]
    c["c_tokid"] = np.ascontiguousarray(tok.reshape(128, NT * 2)).astype(bf)
    c["c_zero"] = np.zeros((128, D), np.float32)
    return c


def _core_consts(r):
    c = {}
    half = 64
    inv_freq = (10000.0 ** (-np.arange(0, half, 2, dtype=np.float32) / half)).astype(np.float32)
    own = np.arange(r * SH, (r + 1) * SH)
    oth = np.arange((1 - r) * SH, (2 - r) * SH)
    pos = np.concatenate([own, oth])
    row = (pos // 64).astype(np.float32)
    colp = (pos % 64).astype(np.float32)
    ang_r = (row[None, :] * inv_freq[:, None]).astype(np.float32)
    ang_c = (colp[None, :] * inv_freq[:, None]).astype(np.float32)
    c["c_cos"] = np.ascontiguousarray(np.concatenate([np.cos(ang_r), np.cos(ang_r), np.cos(ang_c), np.cos(ang_c)], 0).astype(np.float32))
    c["c_sin"] = np.ascontiguousarray(np.concatenate([np.sin(ang_r), np.sin(ang_r), np.sin(ang_c), np.sin(ang_c)], 0).astype(np.float32))
    slopes = (2.0 ** (-8.0 * np.arange(1, 9) / 8)).astype(np.float32)
    qi = np.arange(128)[:, None]
    kj = np.arange(384)[None, :]
    dist = np.abs(qi + 128 - kj).astype(np.float32)
    mid = np.where((dist <= 128)[:, None, :], -slopes[None, :, None] * dist[:, None, :], np.float32(-1e30)).astype(np.float32)
    first = mid.copy()
    last = mid.copy()
    if r == 0:
        first[:, :, 0:128] = -1e30
    else:
        last[:, :, 256:384] = -1e30
    c["c_bias"] = np.ascontiguousarray(np.stack([first, mid, last], 1).reshape(128, 3 * 8 * 384))
    sel = np.zeros((NE, NEL), np.float32)
    for j in range(NEL):
        sel[r * NEL + j, j] = 1.0
    c["c_sel16"] = sel
    c["c_selB"] = np.ascontiguousarray(np.broadcast_to(sel.T.reshape(1, NEL * NE), (128, NEL * NE)))
    c["c_off"] = np.full((128, 1), float(r * SH), np.float32)
    s01 = np.zeros((128, 2), np.float32)
    s01[:, 1 - r] = 1.0
    c["c_s01"] = s01
    return c


_NC_CACHE = {}


def kernel(x, norm_mix, w_in, sink_a, q_norm_b, k_norm_b, w_out, norm_ffn,
           w_router, w_gate, w_up, w_down, norm_final):
    f = lambda a: np.ascontiguousarray(np.asarray(a, dtype=np.float32))
    x = f(x)
    B = x.shape[0]
    if "nc" not in _NC_CACHE:
        _NC_CACHE["nc"] = build_nc()
    nc = _NC_CACHE["nc"]
    bc = lambda v, n: np.ascontiguousarray(np.broadcast_to(f(v).reshape(1, n), (128, n)))
    wg, wu, wd = f(w_gate)[0], f(w_up)[0], f(w_down)[0]
    shared = dict(
        w_in=f(w_in)[0], w_out=f(w_out)[0], w_router=f(w_router)[0],
        g_mix=bc(norm_mix, D), g_ffn=bc(norm_ffn, D), g_fin=bc(norm_final, D),
        sink_b=bc(sink_a, 8),
        gq_col=np.ascontiguousarray(f(q_norm_b).reshape(128, 1)),
        gk_col=np.ascontiguousarray(f(k_norm_b).reshape(128, 1)),
    )
    shared.update(_consts())
    cc = [_core_consts(0), _core_consts(1)]
    in_maps = []
    for c in range(N_CORES):
        b, r = c // 2, c % 2
        m = dict(shared)
        m.update(cc[r])
        m["x_own"] = x[b, r * SH:(r + 1) * SH]
        m["x_oth"] = x[b, (1 - r) * SH:(2 - r) * SH]
        m["w_gate"] = wg[r * NEL:(r + 1) * NEL]
        m["w_up"] = wu[r * NEL:(r + 1) * NEL]
        m["w_down"] = wd[r * NEL:(r + 1) * NEL]
        in_maps.append(m)
    res = run_bass_kernel_spmd(nc, in_maps, core_ids=list(range(N_CORES)))
    o = np.empty((B, S, D), np.float32)
    for c in range(N_CORES):
        b, r = c // 2, c % 2
        o[b, r * SH:(r + 1) * SH] = np.asarray(res.results[c]["out"], dtype=np.float32)
    return o
```

```python
import numpy as np
import ml_dtypes
from contextlib import ExitStack
import concourse.bass as bass
import concourse.mybir as mybir
from concourse.bass_utils import run_bass_kernel_spmd

F32 = mybir.dt.float32
BF16 = mybir.dt.bfloat16
I32 = mybir.dt.int32
U32 = mybir.dt.uint32
AF = mybir.ActivationFunctionType
ALU = mybir.AluOpType
AX = mybir.AxisListType

D = 2048
S = 4096
SH = 2048
NT = S // 128
NTH = SH // 128
NEL = 8
SA = SH + 256
HD = 128
DIN = 3072
NE = 16
CAP = 512
DFF = 4096
EPS = 1e-6
SCALE = HD ** -0.5
N_CORES = 8
DBG = {}


class Res:
    __slots__ = ("w", "r", "const", "name")

    def __init__(self, name, const=False):
        self.w = None
        self.r = {}
        self.const = const
        self.name = name


class Ctx:
    def __init__(self, nc, es):
        self.nc = nc
        self.eng = dict(pe=nc.tensor, dve=nc.vector, act=nc.scalar, pool=nc.gpsimd, sp=nc.sync)
        self.semobj = {}
        for k in self.eng:
            self.semobj[k] = es.enter_context(nc.semaphore("s_" + k))
        self.cnt = {k: 0 for k in self.eng}
        self.known = {k: {} for k in self.eng}
        self.dq = {"sp": 14, "pool": 10, "act": 8}
        self.dval = {}
        self.drr = {}
        self.ccv = {}
        for i in range(16):
            self.semobj["cc%d" % i] = es.enter_context(nc.semaphore("cc%d" % i))
        for q, n in self.dq.items():
            self.dval[q] = [0] * n
            self.drr[q] = 0
            for i in range(n):
                self.semobj[(q, i)] = es.enter_context(nc.semaphore("d_%s%d" % (q, i)))

    def wait(self, e, tok):
        if tok is None:
            return
        s, v, prod = tok
        if prod == "pe" and e == "pe":
            return
        kn = self.known[e]
        if kn.get(s, 0) >= v:
            return
        self.eng[e].wait_ge(self.semobj[s], v)
        kn[s] = v

    def deps(self, e, reads, writes):
        for r in reads:
            self.wait(e, r.w)
        for w in writes:
            self.wait(e, w.w)
            for s, (v, prod) in w.r.items():
                self.wait(e, (s, v, prod))

    def commit(self, tok, reads, writes):
        s, v, prod = tok
        for r in reads:
            if not r.const:
                r.r[s] = (v, prod)
        for w in writes:
            w.w = tok
            w.r = {}

    def op(self, e, fn, reads=(), writes=()):
        self.deps(e, reads, writes)
        ins = fn(self.eng[e])
        self.cnt[e] += 1
        ins.then_inc(self.semobj[e], 1)
        tok = (e, self.cnt[e], e)
        self.known[e][e] = max(self.known[e].get(e, 0), 0)
        self.commit(tok, reads, writes)
        return tok

    def dma(self, q, fn, reads=(), writes=()):
        n = self.dq[q]
        i = self.drr[q]
        self.drr[q] = (i + 1) % n
        key = (q, i)
        prev = self.dval[q][i]
        if prev:
            self.wait(q, (key, prev, "dma"))
        self.deps(q, reads, writes)
        ins = fn(self.eng[q])
        v = prev + 16
        ins.then_inc(self.semobj[key], 16)
        self.dval[q][i] = v
        tok = (key, v, "dma")
        self.commit(tok, reads, writes)
        return tok

    def coll(self, semname, fn, reads=(), writes=()):
        self.deps("pool", reads, writes)
        ins = fn(self.eng["pool"])
        ins.then_inc(self.semobj[semname])
        self.ccv[semname] = self.ccv.get(semname, 0) + 1
        tok = (semname, self.ccv[semname], "dma")
        self.commit(tok, reads, writes)
        return tok

    def barrier(self):
        toks = [(k, self.cnt[k], k) for k in self.eng if self.cnt[k] > 0]
        for q, n in self.dq.items():
            for i in range(n):
                if self.dval[q][i]:
                    toks.append(((q, i), self.dval[q][i], "dma"))
        for nm, v in self.ccv.items():
            toks.append((nm, v, "dma"))
        for e in self.eng:
            for t in toks:
                if t[0] == e and e == "pe":
                    continue
                self.wait(e, t)


def build_nc():
    nc = bass.Bass("TRN2", target_bir_lowering=False)

    def din(name, shape, dt=F32):
        return nc.dram_tensor(name, list(shape), dt, kind="ExternalInput")

    x_own = din("x_own", [SH, D])
    x_oth = din("x_oth", [SH, D])
    w_in = din("w_in", [D, DIN])
    w_out = din("w_out", [D, D])
    w_router = din("w_router", [D, NE])
    w_gate = din("w_gate", [NEL, D, DFF])
    w_up = din("w_up", [NEL, D, DFF])
    w_down = din("w_down", [NEL, DFF, D])
    g_mix = din("g_mix", [128, D])
    g_ffn = din("g_ffn", [128, D])
    g_fin = din("g_fin", [128, D])
    sink_b = din("sink_b", [128, 8])
    gq_col = din("gq_col", [128, 1])
    gk_col = din("gk_col", [128, 1])
    c_identb = din("c_identb", [128, 128], BF16)
    c_identf = din("c_identf", [128, 128])
    c_rotT = din("c_rotT", [128, 128])
    c_ones = din("c_ones", [128, 128])
    c_cos = din("c_cos", [128, S])
    c_sin = din("c_sin", [128, S])
    c_bias = din("c_bias", [128, 3 * 8 * 384])
    c_iota = din("c_iota", [128, CAP])
    c_tokid = din("c_tokid", [128, NT * 2], BF16)
    c_sel16 = din("c_sel16", [NE, NEL])
    c_selB = din("c_selB", [128, NEL * NE])
    c_off = din("c_off", [128, 1])
    c_s01 = din("c_s01", [128, 2])
    c_zero = din("c_zero", [128, D])
    out = nc.dram_tensor("out", [SH, D], F32, kind="ExternalOutput")

    QTd = nc.dram_tensor("QTd", [16, 128, SH], BF16)
    KTB = nc.dram_tensor("KTB", [2, 128, S], BF16)
    VBs = nc.dram_tensor("VBs", [S, 256], BF16)
    KTA = nc.dram_tensor("KTA", [2, 128, SA], BF16)
    VAs = nc.dram_tensor("VAs", [SA, 256], BF16)
    OS = nc.dram_tensor("OS", [SH, D], BF16)
    Yr = nc.dram_tensor("Yr", [S, D], F32)
    H2h = nc.dram_tensor("H2h", [SH, D], BF16)
    AFFh = nc.dram_tensor("AFFh", [SH, NE], F32)
    H2st = [nc.dram_tensor("H2st%d" % k, [1024, D], BF16) for k in range(4)]
    H2f = nc.dram_tensor("H2f", [S, D], BF16)
    AFFf = nc.dram_tensor("AFFf", [S, NE], F32)
    Gst = [nc.dram_tensor("Gst%d" % k, [512, D], F32) for k in range(8)]
    PAIRS = [[0, 1], [2, 3], [4, 5], [6, 7]]

    with ExitStack() as es:
        cx = Ctx(nc, es)

        def sb(st, name, shape, dt=F32):
            return st.enter_context(nc.sbuf_tensor(name, list(shape), dt))

        PS = [es.enter_context(nc.psum_tensor("ps%d" % i, [128, 512], F32)) for i in range(6)]
        PT = [es.enter_context(nc.psum_tensor("pt%d" % i, [128, 512], BF16)) for i in range(2)]
        rPS = [Res("ps%d" % i) for i in range(6)]
        rPT = [Res("pt%d" % i) for i in range(2)]

        identb = sb(es, "identb", [128, 128], BF16)
        identf = sb(es, "identf", [128, 128])
        ones = sb(es, "ones", [128, 128])
        sinkb = sb(es, "sinkb", [128, 8])
        rC = Res("consts")
        cx.dma("sp", lambda e: e.dma_start(out=identb[:], in_=c_identb[:, :]), writes=[rC])
        cx.dma("sp", lambda e: e.dma_start(out=identf[:], in_=c_identf[:, :]), writes=[rC])
        cx.dma("sp", lambda e: e.dma_start(out=ones[:], in_=c_ones[:, :]), writes=[rC])
        cx.dma("sp", lambda e: e.dma_start(out=sinkb[:], in_=sink_b[:, :]), writes=[rC])
        cx.barrier()
        rC.const = True

        NSTG = 3
        NWB = 3
        wst = [sb(es, "wst%d" % i, [128, 4096]) for i in range(NSTG)]
        wbf = [sb(es, "wbf%d" % i, [128, 4096], BF16) for i in range(NWB)]
        rwst = [Res("wst%d" % i) for i in range(NSTG)]
        rwbf = [Res("wbf%d" % i) for i in range(NWB)]
        wstate = {"i": 0, "c": 0}
        cast_eng = ["act", "pool", "dve", "act", "dve", "pool", "act"]

        def wgroup_load(src_ap, k):
            i = wstate["i"] % NSTG
            wstate["i"] += 1
            dst = wst[i][:, :].rearrange("p (k c) -> p k c", k=k)
            cx.dma("sp", lambda e: e.dma_start(out=dst, in_=src_ap), writes=[rwst[i]])
            return i

        def wgroup_cast(i):
            j = wstate["c"] % NWB
            ce = cast_eng[wstate["c"] % len(cast_eng)]
            wstate["c"] += 1
            if ce == "act":
                cx.op("act", lambda e: e.activation(out=wbf[j][:], in_=wst[i][:], func=AF.Copy),
                      reads=[rwst[i]], writes=[rwbf[j]])
            else:
                cx.op(ce, lambda e: e.tensor_copy(wbf[j][:], wst[i][:]), reads=[rwst[i]], writes=[rwbf[j]])
            return j

        class WStream:
            def __init__(self, specs, depth=2):
                self.specs = specs
                self.n = len(specs)
                self.q = []
                self.pos = 0
                for _ in range(min(depth, self.n)):
                    self._issue()

            def _issue(self):
                ap, k = self.specs[self.pos]
                self.pos += 1
                self.q.append((wgroup_load(ap, k), k))

            def next(self):
                i, k = self.q.pop(0)
                j = wgroup_cast(i)
                if self.pos < self.n:
                    self._issue()
                return wbf[j][:, :].rearrange("p (k c) -> p k c", k=k), rwbf[j]

        epsc = sb(es, "epsc", [128, 1])
        eps128 = sb(es, "eps128", [128, 1])
        rE = Res("eps")
        cx.op("dve", lambda e: e.memset(epsc[:], EPS), writes=[rE])
        cx.op("dve", lambda e: e.memset(eps128[:], EPS), writes=[rE])
        cx.barrier()
        rE.const = True

        def rmsnorm_rstd(st_tiles, src, rsrc, junk, rjunk):
            ss, rss, std, rstd = st_tiles
            cx.op("act", lambda e: e.activation(out=junk, in_=src, func=AF.Square, accum_out=ss[:, 0:1]),
                  reads=[rsrc], writes=[rjunk, rss])
            cx.op("act", lambda e: e.activation(out=std[:, 0:1], in_=ss[:, 0:1], func=AF.Sqrt, scale=1.0 / D, bias=epsc[:, 0:1]),
                  reads=[rss], writes=[rss])
            cx.op("dve", lambda e: e.reciprocal(rstd[:, 0:1], std[:, 0:1]), reads=[rss], writes=[rss])

        xov = x_own.ap().rearrange("(t p) d -> t p d", p=128)
        xtv = x_oth.ap().rearrange("(t p) d -> t p d", p=128)
        w_in_v = w_in.ap().rearrange("(k p) c -> p k c", p=128)
        w_out_v = w_out.ap().rearrange("(k p) c -> p k c", p=128)

        with ExitStack() as st:
            gm = sb(st, "gm", [128, D])
            cosT = sb(st, "cosT", [128, S])
            sinT = sb(st, "sinT", [128, S])
            rotT = sb(st, "rotT", [128, 128])
            gqc = sb(st, "gqc", [128, 1])
            gkc = sb(st, "gkc", [128, 1])
            rK = Res("p1c")
            for dst, src in ((gm, g_mix), (cosT, c_cos), (sinT, c_sin), (rotT, c_rotT), (gqc, gq_col), (gkc, gk_col)):
                cx.dma("sp", (lambda d_, s_: (lambda e: e.dma_start(out=d_[:], in_=s_[:, :])))(dst, src), writes=[rK])
            cx.barrier()
            rK.const = True
            xt = [sb(st, "xt%d" % i, [128, D]) for i in range(2)]
            rxt = [Res("xt%d" % i) for i in range(2)]
            hb = [sb(st, "hb%d" % i, [128, D], BF16) for i in range(2)]
            rhb = [Res("hb%d" % i) for i in range(2)]
            hT = [sb(st, "hT%d" % i, [128, 16, 512], BF16) for i in range(2)]
            rhT = [Res("hT%d" % i) for i in range(2)]
            ss = sb(st, "ss", [128, 1]); std = sb(st, "std", [128, 1]); rstd = sb(st, "rstd", [128, 1])
            rss = Res("ss")
            qf = sb(st, "qf", [128, 512]); rqf = Res("qf")
            sq = sb(st, "sq", [128, 512]); rsq = Res("sq")
            sd = sb(st, "sd", [128, 512]); rsd = Res("sd")
            rs_ = sb(st, "rs_", [128, 512]); rrs = Res("rs_")
            qn = sb(st, "qn", [128, 512]); rqn = Res("qn")
            t1 = sb(st, "t1", [128, 512]); rt1 = Res("t1")
            t2 = sb(st, "t2", [128, 512]); rt2 = Res("t2")
            ob = [sb(st, "ob%d" % i, [128, 512], BF16) for i in range(3)]
            rob = [Res("ob%d" % i) for i in range(3)]
            obi = 0
            tcount = 0
            for ci in range(8):
                own = ci < 4
                hTc = hT[ci % 2]; rhTc = rhT[ci % 2]
                for t in range(4):
                    tg = ci * 4 + t
                    xi = tg % 2
                    src_x = xov[tg] if own else xtv[tg - 16]
                    cx.dma("sp", lambda e: e.dma_start(out=xt[xi][:], in_=src_x), writes=[rxt[xi]])
                    rmsnorm_rstd((ss, rss, std, rstd), xt[xi][:], rxt[xi], hb[xi][:], rhb[xi])
                    cx.op("dve", lambda e: e.scalar_tensor_tensor(out=hb[xi][:], in0=xt[xi][:], scalar=rstd[:, 0:1], in1=gm[:],
                                                                  op0=ALU.mult, op1=ALU.mult),
                          reads=[rxt[xi], rss], writes=[rhb[xi]])
                    for k4 in range(4):
                        pi = tcount % 2; tcount += 1
                        for kk in range(4):
                            k = k4 * 4 + kk
                            cx.op("pe", lambda e: e.transpose(PT[pi][:, kk * 128:(kk + 1) * 128], hb[xi][:, k * 128:(k + 1) * 128], identb[:]),
                                  reads=[rhb[xi]], writes=[rPT[pi]])
                        dst = hTc[:, k4 * 4:(k4 + 1) * 4, t * 128:(t + 1) * 128]
                        srcp = PT[pi][:, :].rearrange("p (k c) -> p k c", k=4)
                        if pi == 1:
                            cx.op("dve", lambda e: e.tensor_copy(dst, srcp), reads=[rPT[pi]], writes=[rhTc])
                        else:
                            cx.op("act", lambda e: e.activation(out=dst, in_=srcp, func=AF.Copy), reads=[rPT[pi]], writes=[rhTc])
                if own:
                    cgs = list(range(12))
                elif ci in (4, 7):
                    cgs = [4, 5, 10, 11]
                else:
                    cgs = [10, 11]
                ws = WStream([(w_in_v[:, :, cg * 256:(cg + 1) * 256], 16) for cg in cgs])
                for cg in cgs:
                    wv, rw = ws.next()
                    if cg in (5, 11):
                        for t in range(4):
                            if cg == 5 and not own and not ((ci == 4 and t == 0) or (ci == 7 and t == 3)):
                                continue
                            pb = (t % 2)
                            for k in range(16):
                                cx.op("pe", lambda e: e.matmul(PS[pb][:, 0:256], hTc[:, k, t * 128:(t + 1) * 128], wv[:, k, :],
                                                                start=(k == 0), stop=(k == 15)),
                                      reads=[rhTc, rw], writes=[rPS[pb]])
                            oi = obi % 3; obi += 1
                            cx.op("act", lambda e: e.activation(out=ob[oi][:, 0:256], in_=PS[pb][:, 0:256], func=AF.Copy),
                                  reads=[rPS[pb]], writes=[rob[oi]])
                            if cg == 11:
                                r0 = ci * 512 + t * 128
                                cx.dma("pool", lambda e: e.dma_start(out=VBs[r0:r0 + 128, :], in_=ob[oi][:, 0:256]), reads=[rob[oi]])
                            else:
                                if own:
                                    r0 = 128 + ci * 512 + t * 128
                                elif ci == 4:
                                    r0 = SA - 128
                                else:
                                    r0 = 0
                                cx.dma("pool", lambda e: e.dma_start(out=VAs[r0:r0 + 128, :], in_=ob[oi][:, 0:256]), reads=[rob[oi]])
                        continue
                    for half in range(2):
                        col = cg * 256 + half * 128
                        if col < 1024:
                            typ = "A"; dsts = [(QTd[col // 128, :, ci * 512:(ci + 1) * 512], 0, 512)]
                        elif col < 1280:
                            typ = "A"; g_ = (col - 1024) // 128
                            if own:
                                dsts = [(KTA[g_, :, 128 + ci * 512:128 + (ci + 1) * 512], 0, 512)]
                            elif ci == 4:
                                dsts = [(KTA[g_, :, SA - 128:SA], 0, 128)]
                            else:
                                dsts = [(KTA[g_, :, 0:128], 384, 512)]
                        elif col < 2560:
                            typ = "Bq"; dsts = [(QTd[8 + (col - 1536) // 128, :, ci * 512:(ci + 1) * 512], 0, 512)]
                        else:
                            typ = "Bk"; dsts = [(KTB[(col - 2560) // 128, :, ci * 512:(ci + 1) * 512], 0, 512)]
                        pb = 2 + (half % 2)
                        for k in range(16):
                            cx.op("pe", lambda e: e.matmul(PS[pb][:, :], wv[:, k, half * 128:(half + 1) * 128], hTc[:, k, :],
                                                            start=(k == 0), stop=(k == 15)),
                                  reads=[rhTc, rw], writes=[rPS[pb]])
                        oi = obi % 3; obi += 1
                        if typ == "A":
                            cx.op("act", lambda e: e.activation(out=ob[oi][:], in_=PS[pb][:, :], func=AF.Copy),
                                  reads=[rPS[pb]], writes=[rob[oi]])
                        else:
                            gc = gqc if typ == "Bq" else gkc
                            cx.op("dve", lambda e: e.tensor_copy(qf[:], PS[pb][:, :]), reads=[rPS[pb]], writes=[rqf])
                            cx.op("pool", lambda e: e.tensor_tensor(out=sq[:], in0=qf[:], in1=qf[:], op=ALU.mult), reads=[rqf], writes=[rsq])
                            cx.op("pe", lambda e: e.matmul(PS[4][:, :], ones[:], sq[:], start=True, stop=True), reads=[rsq], writes=[rPS[4]])
                            cx.op("act", lambda e: e.activation(out=sd[:], in_=PS[4][:, :], func=AF.Sqrt, scale=1.0 / HD, bias=eps128[:, 0:1]),
                                  reads=[rPS[4]], writes=[rsd])
                            cx.op("dve", lambda e: e.reciprocal(rs_[:], sd[:]), reads=[rsd], writes=[rrs])
                            cx.op("dve", lambda e: e.scalar_tensor_tensor(out=qn[:], in0=qf[:], scalar=gc[:, 0:1], in1=rs_[:], op0=ALU.mult, op1=ALU.mult),
                                  reads=[rqf, rrs], writes=[rqn])
                            cx.op("pe", lambda e: e.matmul(PS[4][:, :], rotT[:], qn[:], start=True, stop=True), reads=[rqn], writes=[rPS[4]])
                            cx.op("pool", lambda e: e.tensor_tensor(out=t1[:], in0=qn[:], in1=cosT[:, ci * 512:(ci + 1) * 512], op=ALU.mult),
                                  reads=[rqn], writes=[rt1])
                            cx.op("dve", lambda e: e.tensor_tensor(out=t2[:], in0=PS[4][:, :], in1=sinT[:, ci * 512:(ci + 1) * 512], op=ALU.mult),
                                  reads=[rPS[4]], writes=[rt2])
                            cx.op("dve", lambda e: e.tensor_tensor(out=ob[oi][:], in0=t1[:], in1=t2[:], op=ALU.add),
                                  reads=[rt1, rt2], writes=[rob[oi]])
                        for (dap, c0_, c1_) in dsts:
                            cx.dma("pool", lambda e: e.dma_start(out=dap, in_=ob[oi][:, c0_:c1_]), reads=[rob[oi]])
            cx.barrier()

        with ExitStack() as st:
            biasA = sb(st, "biasA", [128, 24, 384])
            negMB = sb(st, "negMB", [128, 1])
            gqa = sb(st, "gqa", [128, 2]); gneg = sb(st, "gneg", [128, 2]); rowm = sb(st, "rowm", [2, 128]); mx = sb(st, "mx", [2, 1]); mprod = sb(st, "mprod", [1, 1])
            rK = Res("p2c")
            cx.dma("sp", lambda e: e.dma_start(out=biasA[:].rearrange("p h c -> p (h c)"), in_=c_bias[:, :]), writes=[rK])
            cx.dma("sp", lambda e: e.dma_start(out=gqa[:, 0:1], in_=gq_col[:, :]), writes=[rK])
            cx.dma("sp", lambda e: e.dma_start(out=gqa[:, 1:2], in_=gk_col[:, :]), writes=[rK])
            cx.op("dve", lambda e: e.tensor_scalar(out=gneg[:], in0=gqa[:], scalar1=-1.0, scalar2=None, op0=ALU.mult), reads=[rK], writes=[rK])
            cx.op("dve", lambda e: e.tensor_tensor(out=gqa[:], in0=gqa[:], in1=gneg[:], op=ALU.max), reads=[rK], writes=[rK])
            cx.op("pe", lambda e: e.transpose(PS[4][0:2, 0:128], gqa[:, 0:2], identf[:]), reads=[rK], writes=[rPS[4]])
            cx.op("dve", lambda e: e.tensor_copy(rowm[:], PS[4][0:2, 0:128]), reads=[rPS[4]], writes=[rK])
            cx.op("dve", lambda e: e.reduce_max(out=mx[:, 0:1], in_=rowm[:], axis=AX.X), reads=[rK], writes=[rK])
            cx.op("pe", lambda e: e.transpose(PS[4][0:1, 0:2], mx[0:2, 0:1], identf[0:2, 0:2]), reads=[rK], writes=[rPS[4]])
            cx.op("dve", lambda e: e.tensor_copy(rowm[0:1, 0:2], PS[4][0:1, 0:2]), reads=[rPS[4]], writes=[rK])
            cx.op("dve", lambda e: e.scalar_tensor_tensor(out=mprod[:], in0=rowm[0:1, 0:1], scalar=-SCALE * HD, in1=rowm[0:1, 1:2],
                                                          op0=ALU.mult, op1=ALU.mult), reads=[rK], writes=[rK])
            cx.op("pe", lambda e: e.matmul(PS[4][:, 0:1], ones[0:1, :], mprod[0:1, 0:1], start=True, stop=True), reads=[rK], writes=[rPS[4]])
            cx.op("dve", lambda e: e.tensor_copy(negMB[:], PS[4][:, 0:1]), reads=[rPS[4]], writes=[rK])
            cx.barrier()
            rK.const = True

            KT = sb(st, "KT", [128, S], BF16); rKT = Res("KT")
            Vt = sb(st, "Vt", [128, NT, 128], BF16); rVt = Res("Vt")
            QT = [sb(st, "QT%d" % i, [128, SH], BF16) for i in range(4)]
            rQT = [Res("QT%d" % i) for i in range(4)]
            sbt = [sb(st, "sbt%d" % i, [128, 384]) for i in range(2)]; rsbt = [Res("sbt%d" % i) for i in range(2)]
            Pb = [sb(st, "Pb%d" % i, [128, 512], BF16) for i in range(3)]; rPb = [Res("Pb%d" % i) for i in range(3)]
            PTs = [sb(st, "PTs%d" % i, [128, 512], BF16) for i in range(3)]; rPTs = [Res("PTs%d" % i) for i in range(3)]
            Og = [sb(st, "Og%d" % i, [128, 512], BF16) for i in range(2)]; rOg = [Res("Og%d" % i) for i in range(2)]
            sm = [sb(st, "smA%d" % i, [128, 16]) for i in range(4)]; rsm = [Res("smA%d" % i) for i in range(4)]
            tix = {"i": 0}

            for grp in ("A", "B"):
                for g in range(2):
                    if grp == "A":
                        cx.dma("sp", lambda e: e.dma_start(out=KT[:, 0:SA], in_=KTA[g, :, :]), writes=[rKT])
                        vsrc = VAs.ap().rearrange("(t p) c -> p t c", p=128)
                        for vq in range(3):
                            cx.dma("sp", lambda e: e.dma_start(out=Vt[:, vq * 6:(vq + 1) * 6, :], in_=vsrc[:, vq * 6:(vq + 1) * 6, g * 128:(g + 1) * 128]),
                                   writes=[rVt])
                    else:
                        cx.dma("sp", lambda e: e.dma_start(out=KT[:], in_=KTB[g, :, :]), writes=[rKT])
                        vsrc = VBs.ap().rearrange("(t p) c -> p t c", p=128)
                        for vq in range(4):
                            cx.dma("sp", lambda e: e.dma_start(out=Vt[:, vq * 8:(vq + 1) * 8, :], in_=vsrc[:, vq * 8:(vq + 1) * 8, g * 128:(g + 1) * 128]),
                                   writes=[rVt])
                    for hh in range(4):
                        qcc = (g * 4 + hh) if grp == "A" else (8 + g * 4 + hh)
                        cx.dma("sp", (lambda hh_, qcc_: (lambda e: e.dma_start(out=QT[hh_][:], in_=QTd[qcc_, :, :])))(hh, qcc), writes=[rQT[hh]])
                    tiles = []
                    for n in range(NTH):
                        for hh in range(4):
                            for kc in range(1 if grp == "A" else 8):
                                i = tix["i"]; tix["i"] += 1
                                nh = n * 4 + hh
                                tiles.append(dict(n=n, hh=hh, kc=kc, first=(kc == 0), last=(grp == "A" or kc == 7),
                                                  psi=i % 2, pi=i % 3, pti=i % 2, si=i % 2, smi=nh % 4, po=2 + (nh % 2), ogi=n % 2))

                    def s1(T):
                        n, hh, kc = T["n"], T["hh"], T["kc"]
                        h = g * 4 + hh
                        psi, pi, si = T["psi"], T["pi"], T["si"]
                        smt = sm[T["smi"]]; rsmt = rsm[T["smi"]]
                        if grp == "A":
                            W = 384
                            bi = 0 if n == 0 else (2 if n == NTH - 1 else 1)
                            cx.op("pe", lambda e: e.matmul(PS[psi][:, 0:W], QT[hh][:, n * 128:(n + 1) * 128], KT[:, n * 128:n * 128 + W],
                                                            start=True, stop=True), reads=[rQT[hh], rKT], writes=[rPS[psi]])
                            cx.op("dve", lambda e: e.scalar_tensor_tensor(out=sbt[si][:, 0:W], in0=PS[psi][:, 0:W], scalar=SCALE,
                                                                          in1=biasA[:, bi * 8 + h, :], op0=ALU.mult, op1=ALU.add),
                                  reads=[rPS[psi]], writes=[rsbt[si]])
                            cx.op("dve", lambda e: e.reduce_max(out=smt[:, 0:1], in_=sbt[si][:, 0:W], axis=AX.X), reads=[rsbt[si]], writes=[rsmt])
                            cx.op("dve", lambda e: e.tensor_tensor(out=smt[:, 1:2], in0=smt[:, 0:1], in1=sinkb[:, h:h + 1], op=ALU.max),
                                  reads=[rsmt], writes=[rsmt])
                            cx.op("dve", lambda e: e.tensor_scalar(out=smt[:, 2:3], in0=smt[:, 1:2], scalar1=-1.0, scalar2=None, op0=ALU.mult),
                                  reads=[rsmt], writes=[rsmt])
                            cx.op("act", lambda e: e.activation(out=Pb[pi][:, 0:W], in_=sbt[si][:, 0:W], func=AF.Exp, bias=smt[:, 2:3],
                                                                accum_out=smt[:, 3:4]), reads=[rsbt[si], rsmt], writes=[rPb[pi], rsmt])
                            cx.op("act", lambda e: e.activation(out=smt[:, 4:5], in_=sinkb[:, h:h + 1], func=AF.Exp, bias=smt[:, 2:3]),
                                  reads=[rsmt], writes=[rsmt])
                            cx.op("dve", lambda e: e.tensor_tensor(out=smt[:, 5:6], in0=smt[:, 3:4], in1=smt[:, 4:5], op=ALU.add),
                                  reads=[rsmt], writes=[rsmt])
                            cx.op("dve", lambda e: e.reciprocal(smt[:, 6:7], smt[:, 5:6]), reads=[rsmt], writes=[rsmt])
                        else:
                            cx.op("pe", lambda e: e.matmul(PS[psi][:, :], QT[hh][:, n * 128:(n + 1) * 128], KT[:, kc * 512:(kc + 1) * 512],
                                                            start=True, stop=True), reads=[rQT[hh], rKT], writes=[rPS[psi]])
                            wr_ = [rPb[pi]] + ([rsmt] if (T["first"] or T["last"]) else [])
                            cx.op("act", lambda e: e.activation(out=Pb[pi][:], in_=PS[psi][:, :], func=AF.Exp, bias=negMB[:, 0:1], scale=SCALE,
                                                                accum_out=smt[:, 8 + kc:9 + kc]), reads=[rPS[psi]], writes=wr_)

                    def s2(T):
                        pi, pti = T["pi"], T["pti"]
                        nj = 3 if grp == "A" else 4
                        W = nj * 128
                        for j in range(nj):
                            cx.op("pe", lambda e: e.transpose(PT[pti][:, j * 128:(j + 1) * 128], Pb[pi][:, j * 128:(j + 1) * 128], identb[:]),
                                  reads=[rPb[pi]], writes=[rPT[pti]])
                        cx.op("dve", lambda e: e.tensor_copy(PTs[pi][:, 0:W], PT[pti][:, 0:W]), reads=[rPT[pti]], writes=[rPTs[pi]])

                    def s3(T):
                        n, hh, kc = T["n"], T["hh"], T["kc"]
                        pi, po, ogi = T["pi"], T["po"], T["ogi"]
                        smt = sm[T["smi"]]; rsmt = rsm[T["smi"]]
                        nj = 3 if grp == "A" else 4
                        for j in range(nj):
                            vb_ = (n + j) if grp == "A" else (kc * 4 + j)
                            cx.op("pe", lambda e: e.matmul(PS[po][:, 0:128], PTs[pi][:, j * 128:(j + 1) * 128], Vt[:, vb_, :],
                                                            start=(T["first"] and j == 0), stop=(T["last"] and j == nj - 1)),
                                  reads=[rPTs[pi], rVt], writes=[rPS[po]])
                        if T["last"]:
                            if grp == "B":
                                cx.op("dve", lambda e: e.reduce_sum(out=smt[:, 5:6], in_=smt[:, 8:16], axis=AX.X), reads=[rsmt], writes=[rsmt])
                                cx.op("dve", lambda e: e.reciprocal(smt[:, 6:7], smt[:, 5:6]), reads=[rsmt], writes=[rsmt])
                            cx.op("act", lambda e: e.activation(out=Og[ogi][:, hh * 128:(hh + 1) * 128], in_=PS[po][:, 0:128], func=AF.Copy,
                                                                scale=smt[:, 6:7]), reads=[rPS[po], rsmt], writes=[rOg[ogi]])
                            if hh == 3:
                                c0 = (0 if grp == "A" else 1024) + g * 512
                                cx.dma("pool", lambda e: e.dma_start(out=OS[n * 128:(n + 1) * 128, c0:c0 + 512], in_=Og[ogi][:]), reads=[rOg[ogi]])

                    L = len(tiles)
                    for i in range(L + 2):
                        if i < L:
                            s1(tiles[i])
                        if 0 <= i - 1 < L:
                            s2(tiles[i - 1])
                        if 0 <= i - 2 < L:
                            s3(tiles[i - 2])
            cx.barrier()

        osv = OS.ap().rearrange("(t p) d -> t p d", p=128)
        yv = Yr.ap().rearrange("(t p) d -> t p d", p=128)
        h2hv = H2h.ap().rearrange("(t p) d -> t p d", p=128)
        affhv = AFFh.ap().rearrange("(t p) d -> t p d", p=128)
        with ExitStack() as st:
            gf = sb(st, "gf", [128, D])
            wr = sb(st, "wr", [128, 16, NE])
            rK = Res("p3c")
            cx.dma("sp", lambda e: e.dma_start(out=gf[:], in_=g_ffn[:, :]), writes=[rK])
            cx.dma("sp", lambda e: e.dma_start(out=wr[:], in_=w_router.ap().rearrange("(k p) c -> p k c", p=128)), writes=[rK])
            cx.barrier()
            rK.const = True
            x1 = [sb(st, "x1_%d" % i, [128, D]) for i in range(4)]; rx1 = [Res("x1_%d" % i) for i in range(4)]
            cx.dma("sp", lambda e: e.dma_start(out=x1[3][:], in_=c_zero[:, :]), writes=[rx1[3]])
            for tg in range(NTH, NT):
                cx.dma("pool", lambda e: e.dma_start(out=yv[tg], in_=x1[3][:]), reads=[rx1[3]])
            otb = [sb(st, "otb%d" % i, [128, D], BF16) for i in range(2)]; rotb = [Res("otb%d" % i) for i in range(2)]
            oT = sb(st, "oT", [128, 16, 512], BF16); roT = Res("oT")
            h2t = [sb(st, "h2t%d" % i, [128, D]) for i in range(2)]; rh2t = [Res("h2t%d" % i) for i in range(2)]
            h2b = [sb(st, "h2b%d" % i, [128, D], BF16) for i in range(2)]; rh2b = [Res("h2b%d" % i) for i in range(2)]
            aft = [sb(st, "aft%d" % i, [128, NE]) for i in range(2)]; raft = [Res("aft%d" % i) for i in range(2)]
            h2T = sb(st, "h2T", [128, 16, 128]); rh2T = Res("h2T")
            jk = sb(st, "jk", [128, D], BF16); rjk = Res("jk")
            ss = sb(st, "ss3", [128, 1]); std = sb(st, "std3", [128, 1]); rstd = sb(st, "rstd3", [128, 1]); rss = Res("ss3")
            sm3 = sb(st, "sm3", [128, 8]); rsm3 = Res("sm3")
            ex = sb(st, "ex", [128, NE]); rex = Res("ex")
            tcount = 0
            for tc in range(4):
                for t in range(4):
                    tg = tc * 4 + t
                    oi = tg % 2
                    cx.dma("sp", lambda e: e.dma_start(out=otb[oi][:], in_=osv[tg]), writes=[rotb[oi]])
                    cx.dma("sp", lambda e: e.dma_start(out=x1[t][:], in_=xov[tg]), writes=[rx1[t]])
                    for k4 in range(4):
                        pi = tcount % 2; tcount += 1
                        for kk in range(4):
                            k = k4 * 4 + kk
                            cx.op("pe", lambda e: e.transpose(PT[pi][:, kk * 128:(kk + 1) * 128], otb[oi][:, k * 128:(k + 1) * 128], identb[:]),
                                  reads=[rotb[oi]], writes=[rPT[pi]])
                        dst = oT[:, k4 * 4:(k4 + 1) * 4, t * 128:(t + 1) * 128]
                        srcp = PT[pi][:, :].rearrange("p (k c) -> p k c", k=4)
                        if pi == 1:
                            cx.op("dve", lambda e: e.tensor_copy(dst, srcp), reads=[rPT[pi]], writes=[roT])
                        else:
                            cx.op("act", lambda e: e.activation(out=dst, in_=srcp, func=AF.Copy), reads=[rPT[pi]], writes=[roT])
                ws = WStream([(w_out_v[:, :, cg * 256:(cg + 1) * 256], 16) for cg in range(8)])
                for cg in range(8):
                    wv, rw = ws.next()
                    for t in range(4):
                        pb = 2 + (t % 2)
                        for k in range(16):
                            cx.op("pe", lambda e: e.matmul(PS[pb][:, 0:256], oT[:, k, t * 128:(t + 1) * 128], wv[:, k, :], start=(k == 0), stop=(k == 15)),
                                  reads=[roT, rw], writes=[rPS[pb]])
                        cx.op("dve", lambda e: e.tensor_tensor(out=x1[t][:, cg * 256:(cg + 1) * 256], in0=PS[pb][:, 0:256],
                                                               in1=x1[t][:, cg * 256:(cg + 1) * 256], op=ALU.add),
                              reads=[rPS[pb], rx1[t]], writes=[rx1[t]])
                for t in range(4):
                    tg = tc * 4 + t
                    hi = tg % 2
                    cx.dma("pool", lambda e: e.dma_start(out=yv[tg], in_=x1[t][:]), reads=[rx1[t]])
                    rmsnorm_rstd((ss, rss, std, rstd), x1[t][:], rx1[t], jk[:], rjk)
                    cx.op("dve", lambda e: e.scalar_tensor_tensor(out=h2t[hi][:], in0=x1[t][:], scalar=rstd[:, 0:1], in1=gf[:],
                                                                  op0=ALU.mult, op1=ALU.mult), reads=[rx1[t], rss], writes=[rh2t[hi]])
                    cx.op("pool", lambda e: e.tensor_copy(h2b[hi][:], h2t[hi][:]), reads=[rh2t[hi]], writes=[rh2b[hi]])
                    cx.dma("pool", lambda e: e.dma_start(out=h2hv[tg], in_=h2b[hi][:]), reads=[rh2b[hi]])
                    for k4 in range(4):
                        pb = k4 % 2
                        for kk in range(4):
                            k = k4 * 4 + kk
                            cx.op("pe", lambda e: e.transpose(PS[pb][:, kk * 128:(kk + 1) * 128], h2t[hi][:, k * 128:(k + 1) * 128], identf[:]),
                                  reads=[rh2t[hi]], writes=[rPS[pb]])
                        dst = h2T[:, k4 * 4:(k4 + 1) * 4, :]
                        srcp = PS[pb][:, :].rearrange("p (k c) -> p k c", k=4)
                        if k4 % 2 == 0:
                            cx.op("dve", lambda e: e.tensor_copy(dst, srcp), reads=[rPS[pb]], writes=[rh2T])
                        else:
                            cx.op("act", lambda e: e.activation(out=dst, in_=srcp, func=AF.Copy), reads=[rPS[pb]], writes=[rh2T])
                    for k in range(16):
                        cx.op("pe", lambda e: e.matmul(PS[4][:, 0:NE], h2T[:, k, :], wr[:, k, :], start=(k == 0), stop=(k == 15)),
                              reads=[rh2T], writes=[rPS[4]])
                    cx.op("dve", lambda e: e.reduce_max(out=sm3[:, 0:1], in_=PS[4][:, 0:NE], axis=AX.X), reads=[rPS[4]], writes=[rsm3])
                    cx.op("dve", lambda e: e.tensor_scalar(out=sm3[:, 1:2], in0=sm3[:, 0:1], scalar1=-1.0, scalar2=None, op0=ALU.mult),
                          reads=[rsm3], writes=[rsm3])
                    cx.op("act", lambda e: e.activation(out=ex[:], in_=PS[4][:, 0:NE], func=AF.Exp, bias=sm3[:, 1:2], accum_out=sm3[:, 2:3]),
                          reads=[rPS[4], rsm3], writes=[rex, rsm3])
                    cx.op("dve", lambda e: e.reciprocal(sm3[:, 3:4], sm3[:, 2:3]), reads=[rsm3], writes=[rsm3])
                    cx.op("dve", lambda e: e.tensor_scalar(out=aft[hi][:], in0=ex[:], scalar1=sm3[:, 3:4], scalar2=None, op0=ALU.mult),
                          reads=[rex, rsm3], writes=[raft[hi]])
                    cx.dma("pool", lambda e: e.dma_start(out=affhv[tg], in_=aft[hi][:]), reads=[raft[hi]])
            cx.barrier()

        rX = Res("xchg")
        for k in range(4):
            cx.coll("cc%d" % k, lambda e: e.collective_compute("AllGather", ALU.bypass, replica_groups=PAIRS,
                                                               ins=[H2h[k * 512:(k + 1) * 512, :]], outs=[H2st[k].ap().opt()]),
                    writes=[rX])
        cx.coll("cc4", lambda e: e.collective_compute("AllGather", ALU.bypass, replica_groups=PAIRS,
                                                      ins=[AFFh.ap().opt()], outs=[AFFf.ap().opt()]), writes=[rX])
        cx.barrier()
        for k in range(4):
            for hf in range(2):
                r0 = hf * SH + k * 512
                cx.dma("sp", lambda e: e.dma_start(out=H2f[r0:r0 + 512, :], in_=H2st[k][hf * 512:(hf + 1) * 512, :]))
        cx.barrier()

        pmTok = sb(es, "pmTok", [128, NT, NEL])
        rpm = Res("pmTok")
        with ExitStack() as st:
            affAll = sb(st, "affAll", [128, NT, NE]); rAff = Res("affAll")
            sel16 = sb(st, "sel16", [NE, NEL])
            affT = sb(st, "affT", [NE, S]); raffT = Res("affT")
            junk = sb(st, "junk4", [NE, S]); rjunk = Res("junk4")
            onesr = sb(st, "onesr", [NE, S])
            thr = sb(st, "thr", [NE, 1], U32); cand = sb(st, "cand", [NE, 1], U32); selm = sb(st, "selm", [NE, 1], U32)
            cntt = sb(st, "cntt", [NE, 1])
            rT = Res("thr")
            afv = AFFf.ap().rearrange("(t p) c -> p t c", p=128)
            for vq in range(4):
                cx.dma("sp", lambda e: e.dma_start(out=affAll[:, vq * 8:(vq + 1) * 8, :], in_=afv[:, vq * 8:(vq + 1) * 8, :]), writes=[rAff])
            cx.dma("sp", lambda e: e.dma_start(out=sel16[:], in_=c_sel16[:, :]), writes=[rT])
            for tg in range(NT):
                pb = tg % 2
                cx.op("pe", lambda e: e.transpose(PS[pb][0:NE, 0:128], affAll[:, tg, :], identf[:]), reads=[rAff], writes=[rPS[pb]])
                cx.op("dve", lambda e: e.tensor_copy(affT[:, tg * 128:(tg + 1) * 128], PS[pb][0:NE, 0:128]), reads=[rPS[pb]], writes=[raffT])
            cx.op("dve", lambda e: e.memset(thr[:], 0), writes=[rT])
            cx.op("dve", lambda e: e.memset(onesr[:], 1.0), writes=[rT])
            for b in range(30, -1, -1):
                cx.op("dve", lambda e: e.tensor_scalar(out=cand[:], in0=thr[:], scalar1=(1 << b), scalar2=None, op0=ALU.bitwise_or),
                      reads=[rT], writes=[rT])
                cx.op("dve", lambda e: e.tensor_scalar(out=junk[:], in0=affT[:], scalar1=cand[:].bitcast(F32)[:, 0:1], scalar2=None,
                                                       op0=ALU.is_ge, op1=ALU.add, accum_out=cntt[:, 0:1]),
                      reads=[rT, raffT], writes=[rjunk, rT])
                cx.op("dve", lambda e: e.tensor_scalar(out=selm[:], in0=cntt[:], scalar1=float(CAP), scalar2=None, op0=ALU.is_ge),
                      reads=[rT], writes=[rT])
                cx.op("dve", lambda e: e.copy_predicated(out=thr[:], mask=selm[:], data=cand[:]), reads=[rT], writes=[rT])
            cx.op("dve", lambda e: e.tensor_scalar(out=junk[:], in0=affT[:], scalar1=thr[:].bitcast(F32)[:, 0:1], scalar2=None, op0=ALU.is_ge),
                  reads=[rT, raffT], writes=[rjunk])
            cx.op("dve", lambda e: e.tensor_tensor_scan(out=affT[:], data0=onesr[:], data1=junk[:], initial=0.0, op0=ALU.mult, op1=ALU.add),
                  reads=[rjunk, rT], writes=[raffT])
            cx.op("dve", lambda e: e.tensor_tensor(out=affT[:], in0=affT[:], in1=junk[:], op=ALU.mult), reads=[rjunk, raffT], writes=[raffT])
            cx.op("dve", lambda e: e.tensor_scalar(out=affT[:], in0=affT[:], scalar1=-1.0, scalar2=None, op0=ALU.add), reads=[raffT], writes=[raffT])
            for tg in range(NT):
                pb = tg % 2
                cx.op("pe", lambda e: e.matmul(PS[pb][:, 0:NEL], affT[:, tg * 128:(tg + 1) * 128], sel16[:, :], start=True, stop=True),
                      reads=[raffT, rT], writes=[rPS[pb]])
                cx.op("dve", lambda e: e.tensor_copy(pmTok[:, tg, :], PS[pb][:, 0:NEL]), reads=[rPS[pb]], writes=[rpm])
            cx.barrier()

        rY = Res("Y")
        rH2 = Res("H2", const=True)
        with ExitStack() as st:
            iot = sb(st, "iot", [128, CAP])
            tokid = sb(st, "tokid", [128, NT, 2], BF16)
            selB = sb(st, "selB", [128, NEL, NE])
            offc = sb(st, "offc", [128, 1])
            rK = Res("p5c")
            cx.dma("sp", lambda e: e.dma_start(out=iot[:], in_=c_iota[:, :]), writes=[rK])
            cx.dma("sp", lambda e: e.dma_start(out=tokid[:].rearrange("p t c -> p (t c)"), in_=c_tokid[:, :]), writes=[rK])
            cx.dma("sp", lambda e: e.dma_start(out=selB[:].rearrange("p j c -> p (j c)"), in_=c_selB[:, :]), writes=[rK])
            cx.dma("sp", lambda e: e.dma_start(out=offc[:], in_=c_off[:, :]), writes=[rK])
            cx.barrier()
            rK.const = True
            OH = [sb(st, "OH%d" % i, [128, CAP], BF16) for i in range(2)]; rOH = [Res("OH%d" % i) for i in range(2)]
            idr = sb(st, "idr", [2, CAP]); ridr = Res("idr")
            idf = sb(st, "idf", [128, 4]); idp = sb(st, "idp", [128, 8]); idl = sb(st, "idl", [128, 4]); idm = sb(st, "idm", [128, 4])
            ridf = Res("idf")
            idi = [sb(st, "idi%d" % i, [128, 4], I32) for i in range(2)]; ridi = [Res("idi%d" % i) for i in range(2)]
            idc = [sb(st, "idc%d" % i, [128, 4], I32) for i in range(2)]; ridc = [Res("idc%d" % i) for i in range(2)]
            gates = [sb(st, "gates%d" % i, [128, 4]) for i in range(2)]; rgates = [Res("gates%d" % i) for i in range(2)]
            xgb = [sb(st, "xgb%d" % i, [128, D], BF16) for i in range(4)]; rxgb = [Res("xgb%d" % i) for i in range(4)]
            afg = [sb(st, "afg%d" % i, [128, NE]) for i in range(4)]; rafg = [Res("afg%d" % i) for i in range(4)]
            gj = sb(st, "gj", [128, NE]); rgj = Res("gj")
            xgT = sb(st, "xgT", [128, 16, CAP], BF16); rxgT = Res("xgT")
            hTm = sb(st, "hTm", [128, 32, CAP], BF16); rhTm = Res("hTm")
            sg = [sb(st, "sg%d" % i, [128, CAP]) for i in range(4)]; rsg = [Res("sg%d" % i) for i in range(4)]
            eo = [sb(st, "eo%d" % i, [128, D]) for i in range(4)]; reo = [Res("eo%d" % i) for i in range(4)]
            cn = {"oh": 0, "tr": 0, "gu": 0}

            def prep_a(ex_):
                ei = ex_ % 2
                for tg in range(NT):
                    oi = cn["oh"] % 2; cn["oh"] += 1
                    cx.op("dve", lambda e: e.tensor_scalar(out=OH[oi][:], in0=iot[:], scalar1=pmTok[:, tg, ex_:ex_ + 1], scalar2=None, op0=ALU.is_equal),
                          reads=[rpm], writes=[rOH[oi]])
                    cx.op("pe", lambda e: e.matmul(PS[4][0:2, :], tokid[:, tg, :], OH[oi][:], start=(tg == 0), stop=(tg == NT - 1)),
                          reads=[rOH[oi]], writes=[rPS[4]])
                cx.op("dve", lambda e: e.tensor_copy(idr[:], PS[4][0:2, :]), reads=[rPS[4]], writes=[ridr])
                for c in range(4):
                    cx.op("pe", lambda e: e.transpose(PS[4][:, c * 2:(c + 1) * 2], idr[0:2, c * 128:(c + 1) * 128], identf[0:2, 0:2]),
                          reads=[ridr], writes=[rPS[4]])
                cx.op("dve", lambda e: e.tensor_copy(idp[:], PS[4][:, 0:8]), reads=[rPS[4]], writes=[ridf])
                pv = idp[:, :].rearrange("p (c two) -> p c two", two=2)
                cx.op("dve", lambda e: e.scalar_tensor_tensor(out=idf[:], in0=pv[:, :, 0], scalar=128.0, in1=pv[:, :, 1], op0=ALU.mult, op1=ALU.add),
                      reads=[ridf], writes=[ridf])
                cx.op("dve", lambda e: e.tensor_copy(idi[ei][:], idf[:]), reads=[ridf], writes=[ridi[ei]])
                cx.op("dve", lambda e: e.tensor_scalar(out=idl[:], in0=idf[:], scalar1=offc[:, 0:1], scalar2=None, op0=ALU.add), reads=[ridf], writes=[ridf])
                cx.op("dve", lambda e: e.tensor_scalar(out=idm[:], in0=idl[:], scalar1=float(S), scalar2=-float(S), op0=ALU.is_ge, op1=ALU.mult),
                      reads=[ridf], writes=[ridf])
                cx.op("dve", lambda e: e.tensor_tensor(out=idl[:], in0=idl[:], in1=idm[:], op=ALU.add), reads=[ridf], writes=[ridf])
                cx.op("dve", lambda e: e.tensor_copy(idc[ei][:], idl[:]), reads=[ridf], writes=[ridc[ei]])
                for c in range(4):
                    cx.dma("pool", lambda e: e.indirect_dma_start(out=xgb[c][:], out_offset=None, in_=H2f[:, :],
                                                                  in_offset=bass.IndirectOffsetOnAxis(ap=idi[ei][:, c:c + 1], axis=0)),
                           reads=[ridi[ei], rH2], writes=[rxgb[c]])
                    cx.dma("pool", lambda e: e.indirect_dma_start(out=afg[c][:], out_offset=None, in_=AFFf[:, :],
                                                                  in_offset=bass.IndirectOffsetOnAxis(ap=idi[ei][:, c:c + 1], axis=0)),
                           reads=[ridi[ei], rH2], writes=[rafg[c]])
                    cx.op("dve", lambda e: e.scalar_tensor_tensor(out=gj[:], in0=afg[c][:], scalar=1.0, in1=selB[:, ex_, :], op0=ALU.mult, op1=ALU.mult,
                                                                  accum_out=gates[ei][:, c:c + 1]),
                          reads=[rafg[c]], writes=[rgj, rgates[ei]])

            def prep_b(ex_):
                for c in range(4):
                    for k4 in range(4):
                        pi = cn["tr"] % 2; cn["tr"] += 1
                        for kk in range(4):
                            k = k4 * 4 + kk
                            cx.op("pe", lambda e: e.transpose(PT[pi][:, kk * 128:(kk + 1) * 128], xgb[c][:, k * 128:(k + 1) * 128], identb[:]),
                                  reads=[rxgb[c]], writes=[rPT[pi]])
                        dst = xgT[:, k4 * 4:(k4 + 1) * 4, c * 128:(c + 1) * 128]
                        srcp = PT[pi][:, :].rearrange("p (k c) -> p k c", k=4)
                        if pi == 1:
                            cx.op("dve", lambda e: e.tensor_copy(dst, srcp), reads=[rPT[pi]], writes=[rxgT])
                        else:
                            cx.op("act", lambda e: e.activation(out=dst, in_=srcp, func=AF.Copy), reads=[rPT[pi]], writes=[rxgT])

            specs = []
            for ex_ in range(NEL):
                wg_v = w_gate.ap()[ex_].rearrange("(k p) c -> p k c", p=128)
                wu_v = w_up.ap()[ex_].rearrange("(k p) c -> p k c", p=128)
                wd_v = w_down.ap()[ex_].rearrange("(k p) c -> p k c", p=128)
                for f4 in range(8):
                    for wv_ in (wg_v, wu_v):
                        for kh in range(2):
                            specs.append((wv_[:, kh * 8:(kh + 1) * 8, f4 * 512:(f4 + 1) * 512], 8))
                for c4 in range(4):
                    for q in range(4):
                        specs.append((wd_v[:, q * 8:(q + 1) * 8, c4 * 512:(c4 + 1) * 512], 8))
            ws = WStream(specs)
            prep_a(0)
            prep_b(0)
            for ex_ in range(NEL):
                ei = ex_ % 2
                if ex_ + 1 < NEL:
                    prep_a(ex_ + 1)
                for f4 in range(8):
                    for which in ("g", "u"):
                        banks = [(cn["gu"] + j) % 6 for j in range(4)]
                        cn["gu"] = (cn["gu"] + 4) % 6
                        for kh in range(2):
                            wv, rw = ws.next()
                            for j in range(4):
                                pb = banks[j]
                                for k8 in range(8):
                                    cx.op("pe", lambda e: e.matmul(PS[pb][:, :], wv[:, k8, j * 128:(j + 1) * 128], xgT[:, kh * 8 + k8, :],
                                                                    start=(kh == 0 and k8 == 0), stop=(kh == 1 and k8 == 7)),
                                          reads=[rw, rxgT], writes=[rPS[pb]])
                        for j in range(4):
                            pb = banks[j]
                            if which == "g":
                                cx.op("act", lambda e: e.activation(out=sg[j][:], in_=PS[pb][:, :], func=AF.Silu), reads=[rPS[pb]], writes=[rsg[j]])
                            else:
                                cx.op("dve", lambda e: e.tensor_tensor(out=hTm[:, f4 * 4 + j, :], in0=sg[j][:], in1=PS[pb][:, :], op=ALU.mult),
                                      reads=[rsg[j], rPS[pb]], writes=[rhTm])
                if ex_ + 1 < NEL:
                    prep_b(ex_ + 1)
                for c4 in range(4):
                    for q in range(4):
                        wdv, rwd = ws.next()
                        for f8 in range(8):
                            f = q * 8 + f8
                            for t in range(4):
                                pb = [4, 5, 2, 3][t]
                                cx.op("pe", lambda e: e.matmul(PS[pb][:, :], hTm[:, f, t * 128:(t + 1) * 128], wdv[:, f8, :],
                                                                start=(f == 0), stop=(f == 31)), reads=[rhTm, rwd], writes=[rPS[pb]])
                    for t in range(4):
                        pb = [4, 5, 2, 3][t]
                        if t % 2 == 0:
                            cx.op("act", lambda e: e.activation(out=eo[t][:, c4 * 512:(c4 + 1) * 512], in_=PS[pb][:, :], func=AF.Copy, scale=gates[ei][:, t:t + 1]),
                                  reads=[rPS[pb], rgates[ei]], writes=[reo[t]])
                        else:
                            cx.op("dve", lambda e: e.tensor_scalar(out=eo[t][:, c4 * 512:(c4 + 1) * 512], in0=PS[pb][:, :], scalar1=gates[ei][:, t:t + 1],
                                                                   scalar2=None, op0=ALU.mult), reads=[rPS[pb], rgates[ei]], writes=[reo[t]])
                for c in range(4):
                    cx.dma("pool", lambda e: e.indirect_dma_start(out=Yr[:, :], out_offset=bass.IndirectOffsetOnAxis(ap=idc[ei][:, c:c + 1], axis=0),
                                                                  in_=eo[c][:], in_offset=None, compute_op=ALU.add),
                           reads=[reo[c], ridc[ei]], writes=[rY])
            cx.barrier()

        for k in range(8):
            cx.coll("cc%d" % (5 + k), lambda e: e.collective_compute("AllGather", ALU.bypass, replica_groups=PAIRS,
                                                                    ins=[Yr[SH + k * 256:SH + (k + 1) * 256, :]], outs=[Gst[k].ap().opt()]),
                    writes=[rX])
        cx.barrier()

        outv = out.ap().rearrange("(t p) d -> t p d", p=128)
        with ExitStack() as st:
            gfi = sb(st, "gfi", [128, D])
            s01 = sb(st, "s01", [128, 2])
            rK = Res("p6c")
            cx.dma("sp", lambda e: e.dma_start(out=gfi[:], in_=g_fin[:, :]), writes=[rK])
            cx.dma("sp", lambda e: e.dma_start(out=s01[:], in_=c_s01[:, :]), writes=[rK])
            cx.barrier()
            rK.const = True
            yt = [sb(st, "yt%d" % i, [128, D]) for i in range(2)]; ryt = [Res("yt%d" % i) for i in range(2)]
            ga = [sb(st, "ga%d" % i, [128, D]) for i in range(2)]; rga = [Res("ga%d" % i) for i in range(2)]
            gb = [sb(st, "gb%d" % i, [128, D]) for i in range(2)]; rgb = [Res("gb%d" % i) for i in range(2)]
            ot = [sb(st, "ot%d" % i, [128, D]) for i in range(2)]; rot = [Res("ot%d" % i) for i in range(2)]
            ss = sb(st, "ss6", [128, 1]); std = sb(st, "std6", [128, 1]); rstd = sb(st, "rstd6", [128, 1]); rss = Res("ss6")
            for tg in range(NTH):
                i = tg % 2
                k = tg // 2; sub = tg % 2
                cx.dma("sp", lambda e: e.dma_start(out=yt[i][:], in_=yv[tg]), writes=[ryt[i]])
                cx.dma("sp", lambda e: e.dma_start(out=ga[i][:], in_=Gst[k][sub * 128:(sub + 1) * 128, :]), writes=[rga[i]])
                cx.dma("sp", lambda e: e.dma_start(out=gb[i][:], in_=Gst[k][256 + sub * 128:256 + (sub + 1) * 128, :]), writes=[rgb[i]])
                cx.op("dve", lambda e: e.scalar_tensor_tensor(out=yt[i][:], in0=ga[i][:], scalar=s01[:, 0:1], in1=yt[i][:], op0=ALU.mult, op1=ALU.add),
                      reads=[rga[i], ryt[i]], writes=[ryt[i]])
                cx.op("dve", lambda e: e.scalar_tensor_tensor(out=yt[i][:], in0=gb[i][:], scalar=s01[:, 1:2], in1=yt[i][:], op0=ALU.mult, op1=ALU.add),
                      reads=[rgb[i], ryt[i]], writes=[ryt[i]])
                rmsnorm_rstd((ss, rss, std, rstd), yt[i][:], ryt[i], ot[i][:], rot[i])
                cx.op("dve", lambda e: e.scalar_tensor_tensor(out=ot[i][:], in0=yt[i][:], scalar=rstd[:, 0:1], in1=gfi[:], op0=ALU.mult, op1=ALU.mult),
                      reads=[ryt[i], rss], writes=[rot[i]])
                cx.dma("pool", lambda e: e.dma_start(out=outv[tg], in_=ot[i][:]), reads=[rot[i]])
            cx.barrier()
    return nc


def _consts():
    bf = ml_dtypes.bfloat16
    c = {}
    c["c_identb"] = np.eye(128, dtype=np.float32).astype(bf)
    c["c_identf"] = np.eye(128, dtype=np.float32)
    rotT = np.zeros((128, 128), np.float32)
    for base in (0, 64):
        for i in range(32):
            m = base + i
            rotT[m + 32, m] = -1.0
            rotT[m, m + 32] = 1.0
    c["c_rotT"] = rotT
    c["c_ones"] = np.ones((128, 128), np.float32)
    c["c_iota"] = np.ascontiguousarray(np.broadcast_to(np.arange(CAP, dtype=np.float32)[None, :], (128, CAP)))
    tok = np.zeros((128, NT, 2), np.float32)
    tok[:, :, 0] = np.arange(NT)[None, :]
    tok[:, :, 1] = np.arange(128)[:, None]
    c["c_tokid"] = np.ascontiguousarray(tok.reshape(128, NT * 2)).astype(bf)
    c["c_zero"] = np.zeros((128, D), np.float32)
    return c


def _core_consts(r):
    c = {}
    half = 64
    inv_freq = (10000.0 ** (-np.arange(0, half, 2, dtype=np.float32) / half)).astype(np.float32)
    own = np.arange(r * SH, (r + 1) * SH)
    oth = np.arange((1 - r) * SH, (2 - r) * SH)
    pos = np.concatenate([own, oth])
    row = (pos // 64).astype(np.float32)
    colp = (pos % 64).astype(np.float32)
    ang_r = (row[None, :] * inv_freq[:, None]).astype(np.float32)
    ang_c = (colp[None, :] * inv_freq[:, None]).astype(np.float32)
    c["c_cos"] = np.ascontiguousarray(np.concatenate([np.cos(ang_r), np.cos(ang_r), np.cos(ang_c), np.cos(ang_c)], 0).astype(np.float32))
    c["c_sin"] = np.ascontiguousarray(np.concatenate([np.sin(ang_r), np.sin(ang_r), np.sin(ang_c), np.sin(ang_c)], 0).astype(np.float32))
    slopes = (2.0 ** (-8.0 * np.arange(1, 9) / 8)).astype(np.float32)
    qi = np.arange(128)[:, None]
    kj = np.arange(384)[None, :]
    dist = np.abs(qi + 128 - kj).astype(np.float32)
    mid = np.where((dist <= 128)[:, None, :], -slopes[None, :, None] * dist[:, None, :], np.float32(-1e30)).astype(np.float32)
    first = mid.copy()
    last = mid.copy()
    if r == 0:
        first[:, :, 0:128] = -1e30
    else:
        last[:, :, 256:384] = -1e30
    c["c_bias"] = np.ascontiguousarray(np.stack([first, mid, last], 1).reshape(128, 3 * 8 * 384))
    sel = np.zeros((NE, NEL), np.float32)
    for j in range(NEL):
        sel[r * NEL + j, j] = 1.0
    c["c_sel16"] = sel
    c["c_selB"] = np.ascontiguousarray(np.broadcast_to(sel.T.reshape(1, NEL * NE), (128, NEL * NE)))
    c["c_off"] = np.full((128, 1), float(r * SH), np.float32)
    s01 = np.zeros((128, 2), np.float32)
    s01[:, 1 - r] = 1.0
    c["c_s01"] = s01
    return c


_NC_CACHE = {}


def kernel(x, norm_mix, w_in, sink_a, q_norm_b, k_norm_b, w_out, norm_ffn,
           w_router, w_gate, w_up, w_down, norm_final):
    f = lambda a: np.ascontiguousarray(np.asarray(a, dtype=np.float32))
    x = f(x)
    B = x.shape[0]
    if "nc" not in _NC_CACHE:
        _NC_CACHE["nc"] = build_nc()
    nc = _NC_CACHE["nc"]
    bc = lambda v, n: np.ascontiguousarray(np.broadcast_to(f(v).reshape(1, n), (128, n)))
    wg, wu, wd = f(w_gate)[0], f(w_up)[0], f(w_down)[0]
    shared = dict(
        w_in=f(w_in)[0], w_out=f(w_out)[0], w_router=f(w_router)[0],
        g_mix=bc(norm_mix, D), g_ffn=bc(norm_ffn, D), g_fin=bc(norm_final, D),
        sink_b=bc(sink_a, 8),
        gq_col=np.ascontiguousarray(f(q_norm_b).reshape(128, 1)),
        gk_col=np.ascontiguousarray(f(k_norm_b).reshape(128, 1)),
    )
    shared.update(_consts())
    cc = [_core_consts(0), _core_consts(1)]
    in_maps = []
    for c in range(N_CORES):
        b, r = c // 2, c % 2
        m = dict(shared)
        m.update(cc[r])
        m["x_own"] = x[b, r * SH:(r + 1) * SH]
        m["x_oth"] = x[b, (1 - r) * SH:(2 - r) * SH]
        m["w_gate"] = wg[r * NEL:(r + 1) * NEL]
        m["w_up"] = wu[r * NEL:(r + 1) * NEL]
        m["w_down"] = wd[r * NEL:(r + 1) * NEL]
        in_maps.append(m)
    res = run_bass_kernel_spmd(nc, in_maps, core_ids=list(range(N_CORES)))
    o = np.empty((B, S, D), np.float32)
    for c in range(N_CORES):
        b, r = c // 2, c % 2
        o[b, r * SH:(r + 1) * SH] = np.asarray(res.results[c]["out"], dtype=np.float32)
    return o
```

```python
import numpy as np
import ml_dtypes
from contextlib import ExitStack
import concourse.bass as bass
import concourse.mybir as mybir
from concourse.bass_utils import run_bass_kernel_spmd

F32 = mybir.dt.float32
BF16 = mybir.dt.bfloat16
I32 = mybir.dt.int32
U32 = mybir.dt.uint32
AF = mybir.ActivationFunctionType
ALU = mybir.AluOpType
AX = mybir.AxisListType

D = 2048
S = 4096
SH = 2048
NT = S // 128
NTH = SH // 128
NEL = 8
SA = SH + 256
HD = 128
DIN = 3072
NE = 16
CAP = 512
DFF = 4096
EPS = 1e-6
SCALE = HD ** -0.5
N_CORES = 8
DBG = {}


class Res:
    __slots__ = ("w", "r", "const", "name")

    def __init__(self, name, const=False):
        self.w = None
        self.r = {}
        self.const = const
        self.name = name


class Ctx:
    def __init__(self, nc, es):
        self.nc = nc
        self.eng = dict(pe=nc.tensor, dve=nc.vector, act=nc.scalar, pool=nc.gpsimd, sp=nc.sync)
        self.semobj = {}
        for k in self.eng:
            self.semobj[k] = es.enter_context(nc.semaphore("s_" + k))
        self.cnt = {k: 0 for k in self.eng}
        self.known = {k: {} for k in self.eng}
        self.dq = {"sp": 14, "pool": 10, "act": 8}
        self.dval = {}
        self.drr = {}
        self.ccv = {}
        for i in range(16):
            self.semobj["cc%d" % i] = es.enter_context(nc.semaphore("cc%d" % i))
        for q, n in self.dq.items():
            self.dval[q] = [0] * n
            self.drr[q] = 0
            for i in range(n):
                self.semobj[(q, i)] = es.enter_context(nc.semaphore("d_%s%d" % (q, i)))

    def wait(self, e, tok):
        if tok is None:
            return
        s, v, prod = tok
        if prod == "pe" and e == "pe":
            return
        kn = self.known[e]
        if kn.get(s, 0) >= v:
            return
        self.eng[e].wait_ge(self.semobj[s], v)
        kn[s] = v

    def deps(self, e, reads, writes):
        for r in reads:
            self.wait(e, r.w)
        for w in writes:
            self.wait(e, w.w)
            for s, (v, prod) in w.r.items():
                self.wait(e, (s, v, prod))

    def commit(self, tok, reads, writes):
        s, v, prod = tok
        for r in reads:
            if not r.const:
                r.r[s] = (v, prod)
        for w in writes:
            w.w = tok
            w.r = {}

    def op(self, e, fn, reads=(), writes=()):
        self.deps(e, reads, writes)
        ins = fn(self.eng[e])
        self.cnt[e] += 1
        ins.then_inc(self.semobj[e], 1)
        tok = (e, self.cnt[e], e)
        self.known[e][e] = max(self.known[e].get(e, 0), 0)
        self.commit(tok, reads, writes)
        return tok

    def dma(self, q, fn, reads=(), writes=()):
        n = self.dq[q]
        i = self.drr[q]
        self.drr[q] = (i + 1) % n
        key = (q, i)
        prev = self.dval[q][i]
        if prev:
            self.wait(q, (key, prev, "dma"))
        self.deps(q, reads, writes)
        ins = fn(self.eng[q])
        v = prev + 16
        ins.then_inc(self.semobj[key], 16)
        self.dval[q][i] = v
        tok = (key, v, "dma")
        self.commit(tok, reads, writes)
        return tok

    def coll(self, semname, fn, reads=(), writes=()):
        self.deps("pool", reads, writes)
        ins = fn(self.eng["pool"])
        ins.then_inc(self.semobj[semname])
        self.ccv[semname] = self.ccv.get(semname, 0) + 1
        tok = (semname, self.ccv[semname], "dma")
        self.commit(tok, reads, writes)
        return tok

    def barrier(self):
        toks = [(k, self.cnt[k], k) for k in self.eng if self.cnt[k] > 0]
        for q, n in self.dq.items():
            for i in range(n):
                if self.dval[q][i]:
                    toks.append(((q, i), self.dval[q][i], "dma"))
        for nm, v in self.ccv.items():
            toks.append((nm, v, "dma"))
        for e in self.eng:
            for t in toks:
                if t[0] == e and e == "pe":
                    continue
                self.wait(e, t)


def build_nc():
    nc = bass.Bass("TRN2", target_bir_lowering=False)

    def din(name, shape, dt=F32):
        return nc.dram_tensor(name, list(shape), dt, kind="ExternalInput")

    x_own = din("x_own", [SH, D])
    x_oth = din("x_oth", [SH, D])
    w_in = din("w_in", [D, DIN])
    w_out = din("w_out", [D, D])
    w_router = din("w_router", [D, NE])
    w_gate = din("w_gate", [NEL, D, DFF])
    w_up = din("w_up", [NEL, D, DFF])
    w_down = din("w_down", [NEL, DFF, D])
    g_mix = din("g_mix", [128, D])
    g_ffn = din("g_ffn", [128, D])
    g_fin = din("g_fin", [128, D])
    sink_b = din("sink_b", [128, 8])
    gq_col = din("gq_col", [128, 1])
    gk_col = din("gk_col", [128, 1])
    c_identb = din("c_identb", [128, 128], BF16)
    c_identf = din("c_identf", [128, 128])
    c_rotT = din("c_rotT", [128, 128])
    c_ones = din("c_ones", [128, 128])
    c_cos = din("c_cos", [128, S])
    c_sin = din("c_sin", [128, S])
    c_bias = din("c_bias", [128, 3 * 8 * 384])
    c_iota = din("c_iota", [128, CAP])
    c_tokid = din("c_tokid", [128, NT * 2], BF16)
    c_sel16 = din("c_sel16", [NE, NEL])
    c_selB = din("c_selB", [128, NEL * NE])
    c_off = din("c_off", [128, 1])
    c_s01 = din("c_s01", [128, 2])
    c_zero = din("c_zero", [128, D])
    out = nc.dram_tensor("out", [SH, D], F32, kind="ExternalOutput")

    QTd = nc.dram_tensor("QTd", [16, 128, SH], BF16)
    KTB = nc.dram_tensor("KTB", [2, 128, S], BF16)
    VBs = nc.dram_tensor("VBs", [S, 256], BF16)
    KTA = nc.dram_tensor("KTA", [2, 128, SA], BF16)
    VAs = nc.dram_tensor("VAs", [SA, 256], BF16)
    OS = nc.dram_tensor("OS", [SH, D], BF16)
    Yr = nc.dram_tensor("Yr", [S, D], F32)
    H2h = nc.dram_tensor("H2h", [SH, D], BF16)
    AFFh = nc.dram_tensor("AFFh", [SH, NE], F32)
    H2st = [nc.dram_tensor("H2st%d" % k, [1024, D], BF16) for k in range(4)]
    H2f = nc.dram_tensor("H2f", [S, D], BF16)
    AFFf = nc.dram_tensor("AFFf", [S, NE], F32)
    Gst = [nc.dram_tensor("Gst%d" % k, [512, D], F32) for k in range(8)]
    PAIRS = [[0, 1], [2, 3], [4, 5], [6, 7]]

    with ExitStack() as es:
        cx = Ctx(nc, es)

        def sb(st, name, shape, dt=F32):
            return st.enter_context(nc.sbuf_tensor(name, list(shape), dt))

        PS = [es.enter_context(nc.psum_tensor("ps%d" % i, [128, 512], F32)) for i in range(6)]
        PT = [es.enter_context(nc.psum_tensor("pt%d" % i, [128, 512], BF16)) for i in range(2)]
        rPS = [Res("ps%d" % i) for i in range(6)]
        rPT = [Res("pt%d" % i) for i in range(2)]

        identb = sb(es, "identb", [128, 128], BF16)
        identf = sb(es, "identf", [128, 128])
        ones = sb(es, "ones", [128, 128])
        sinkb = sb(es, "sinkb", [128, 8])
        rC = Res("consts")
        cx.dma("sp", lambda e: e.dma_start(out=identb[:], in_=c_identb[:, :]), writes=[rC])
        cx.dma("sp", lambda e: e.dma_start(out=identf[:], in_=c_identf[:, :]), writes=[rC])
        cx.dma("sp", lambda e: e.dma_start(out=ones[:], in_=c_ones[:, :]), writes=[rC])
        cx.dma("sp", lambda e: e.dma_start(out=sinkb[:], in_=sink_b[:, :]), writes=[rC])
        cx.barrier()
        rC.const = True

        NSTG = 3
        NWB = 3
        wst = [sb(es, "wst%d" % i, [128, 4096]) for i in range(NSTG)]
        wbf = [sb(es, "wbf%d" % i, [128, 4096], BF16) for i in range(NWB)]
        rwst = [Res("wst%d" % i) for i in range(NSTG)]
        rwbf = [Res("wbf%d" % i) for i in range(NWB)]
        wstate = {"i": 0, "c": 0}
        cast_eng = ["act", "dve"]

        def wgroup_load(src_ap, k):
            i = wstate["i"] % NSTG
            wstate["i"] += 1
            dst = wst[i][:, :].rearrange("p (k c) -> p k c", k=k)
            cx.dma("sp", lambda e: e.dma_start(out=dst, in_=src_ap), writes=[rwst[i]])
            return i

        def wgroup_cast(i):
            j = wstate["c"] % NWB
            ce = cast_eng[wstate["c"] % len(cast_eng)]
            wstate["c"] += 1
            if ce == "act":
                cx.op("act", lambda e: e.activation(out=wbf[j][:], in_=wst[i][:], func=AF.Copy),
                      reads=[rwst[i]], writes=[rwbf[j]])
            else:
                cx.op(ce, lambda e: e.tensor_copy(wbf[j][:], wst[i][:]), reads=[rwst[i]], writes=[rwbf[j]])
            return j

        class WStream:
            def __init__(self, specs):
                self.specs = specs
                self.n = len(specs)
                self.loads = []
                self.ready = []
                self.pos = 0
                self._fill()

            def _fill(self):
                while self.pos < self.n and len(self.loads) < NSTG - 1:
                    ap, k = self.specs[self.pos]
                    self.pos += 1
                    self.loads.append((wgroup_load(ap, k), k))

            def _cast_one(self):
                if self.loads:
                    i, k = self.loads.pop(0)
                    self.ready.append((wgroup_cast(i), k))
                    self._fill()

            def next(self):
                if not self.ready:
                    self._cast_one()
                j, k = self.ready.pop(0)
                self._cast_one()
                return wbf[j][:, :].rearrange("p (k c) -> p k c", k=k), rwbf[j]

        epsc = sb(es, "epsc", [128, 1])
        eps128 = sb(es, "eps128", [128, 1])
        rE = Res("eps")
        cx.op("dve", lambda e: e.memset(epsc[:], EPS), writes=[rE])
        cx.op("dve", lambda e: e.memset(eps128[:], EPS), writes=[rE])
        cx.barrier()
        rE.const = True

        def rmsnorm_rstd(st_tiles, src, rsrc, junk, rjunk):
            ss, rss, std, rstd = st_tiles
            cx.op("act", lambda e: e.activation(out=junk, in_=src, func=AF.Square, accum_out=ss[:, 0:1]),
                  reads=[rsrc], writes=[rjunk, rss])
            cx.op("act", lambda e: e.activation(out=std[:, 0:1], in_=ss[:, 0:1], func=AF.Sqrt, scale=1.0 / D, bias=epsc[:, 0:1]),
                  reads=[rss], writes=[rss])
            cx.op("dve", lambda e: e.reciprocal(rstd[:, 0:1], std[:, 0:1]), reads=[rss], writes=[rss])

        xov = x_own.ap().rearrange("(t p) d -> t p d", p=128)
        xtv = x_oth.ap().rearrange("(t p) d -> t p d", p=128)
        w_in_v = w_in.ap().rearrange("(k p) c -> p k c", p=128)
        w_out_v = w_out.ap().rearrange("(k p) c -> p k c", p=128)

        with ExitStack() as st:
            gm = sb(st, "gm", [128, D])
            cosT = sb(st, "cosT", [128, S])
            sinT = sb(st, "sinT", [128, S])
            rotT = sb(st, "rotT", [128, 128])
            gqc = sb(st, "gqc", [128, 1])
            gkc = sb(st, "gkc", [128, 1])
            rK = Res("p1c")
            for dst, src in ((gm, g_mix), (cosT, c_cos), (sinT, c_sin), (rotT, c_rotT), (gqc, gq_col), (gkc, gk_col)):
                cx.dma("sp", (lambda d_, s_: (lambda e: e.dma_start(out=d_[:], in_=s_[:, :])))(dst, src), writes=[rK])
            cx.barrier()
            rK.const = True
            xt = [sb(st, "xt%d" % i, [128, D]) for i in range(2)]
            rxt = [Res("xt%d" % i) for i in range(2)]
            hb = [sb(st, "hb%d" % i, [128, D], BF16) for i in range(2)]
            rhb = [Res("hb%d" % i) for i in range(2)]
            hT = [sb(st, "hT%d" % i, [128, 16, 512], BF16) for i in range(2)]
            rhT = [Res("hT%d" % i) for i in range(2)]
            ss = sb(st, "ss", [128, 1]); std = sb(st, "std", [128, 1]); rstd = sb(st, "rstd", [128, 1])
            rss = Res("ss")
            qf = sb(st, "qf", [128, 512]); rqf = Res("qf")
            sq = sb(st, "sq", [128, 512]); rsq = Res("sq")
            sd = sb(st, "sd", [128, 512]); rsd = Res("sd")
            rs_ = sb(st, "rs_", [128, 512]); rrs = Res("rs_")
            qn = sb(st, "qn", [128, 512]); rqn = Res("qn")
            t1 = sb(st, "t1", [128, 512]); rt1 = Res("t1")
            t2 = sb(st, "t2", [128, 512]); rt2 = Res("t2")
            ob = [sb(st, "ob%d" % i, [128, 512], BF16) for i in range(3)]
            rob = [Res("ob%d" % i) for i in range(3)]
            obi = 0
            tcount = 0
            for ci in range(8):
                own = ci < 4
                hTc = hT[ci % 2]; rhTc = rhT[ci % 2]
                for t in range(4):
                    tg = ci * 4 + t
                    xi = tg % 2
                    src_x = xov[tg] if own else xtv[tg - 16]
                    cx.dma("sp", lambda e: e.dma_start(out=xt[xi][:], in_=src_x), writes=[rxt[xi]])
                    rmsnorm_rstd((ss, rss, std, rstd), xt[xi][:], rxt[xi], hb[xi][:], rhb[xi])
                    cx.op("dve", lambda e: e.scalar_tensor_tensor(out=hb[xi][:], in0=xt[xi][:], scalar=rstd[:, 0:1], in1=gm[:],
                                                                  op0=ALU.mult, op1=ALU.mult),
                          reads=[rxt[xi], rss], writes=[rhb[xi]])
                    for k4 in range(4):
                        pi = tcount % 2; tcount += 1
                        for kk in range(4):
                            k = k4 * 4 + kk
                            cx.op("pe", lambda e: e.transpose(PT[pi][:, kk * 128:(kk + 1) * 128], hb[xi][:, k * 128:(k + 1) * 128], identb[:]),
                                  reads=[rhb[xi]], writes=[rPT[pi]])
                        dst = hTc[:, k4 * 4:(k4 + 1) * 4, t * 128:(t + 1) * 128]
                        srcp = PT[pi][:, :].rearrange("p (k c) -> p k c", k=4)
                        if pi == 1:
                            cx.op("dve", lambda e: e.tensor_copy(dst, srcp), reads=[rPT[pi]], writes=[rhTc])
                        else:
                            cx.op("act", lambda e: e.activation(out=dst, in_=srcp, func=AF.Copy), reads=[rPT[pi]], writes=[rhTc])
                if own:
                    cgs = list(range(12))
                elif ci in (4, 7):
                    cgs = [4, 5, 10, 11]
                else:
                    cgs = [10, 11]
                ws = WStream([(w_in_v[:, :, cg * 256:(cg + 1) * 256], 16) for cg in cgs])
                for cg in cgs:
                    wv, rw = ws.next()
                    if cg in (5, 11):
                        for t in range(4):
                            if cg == 5 and not own and not ((ci == 4 and t == 0) or (ci == 7 and t == 3)):
                                continue
                            pb = (t % 2)
                            for k in range(16):
                                cx.op("pe", lambda e: e.matmul(PS[pb][:, 0:256], hTc[:, k, t * 128:(t + 1) * 128], wv[:, k, :],
                                                                start=(k == 0), stop=(k == 15)),
                                      reads=[rhTc, rw], writes=[rPS[pb]])
                            oi = obi % 3; obi += 1
                            cx.op("act", lambda e: e.activation(out=ob[oi][:, 0:256], in_=PS[pb][:, 0:256], func=AF.Copy),
                                  reads=[rPS[pb]], writes=[rob[oi]])
                            if cg == 11:
                                r0 = ci * 512 + t * 128
                                cx.dma("pool", lambda e: e.dma_start(out=VBs[r0:r0 + 128, :], in_=ob[oi][:, 0:256]), reads=[rob[oi]])
                            else:
                                if own:
                                    r0 = 128 + ci * 512 + t * 128
                                elif ci == 4:
                                    r0 = SA - 128
                                else:
                                    r0 = 0
                                cx.dma("pool", lambda e: e.dma_start(out=VAs[r0:r0 + 128, :], in_=ob[oi][:, 0:256]), reads=[rob[oi]])
                        continue
                    for half in range(2):
                        col = cg * 256 + half * 128
                        if col < 1024:
                            typ = "A"; dsts = [(QTd[col // 128, :, ci * 512:(ci + 1) * 512], 0, 512)]
                        elif col < 1280:
                            typ = "A"; g_ = (col - 1024) // 128
                            if own:
                                dsts = [(KTA[g_, :, 128 + ci * 512:128 + (ci + 1) * 512], 0, 512)]
                            elif ci == 4:
                                dsts = [(KTA[g_, :, SA - 128:SA], 0, 128)]
                            else:
                                dsts = [(KTA[g_, :, 0:128], 384, 512)]
                        elif col < 2560:
                            typ = "Bq"; dsts = [(QTd[8 + (col - 1536) // 128, :, ci * 512:(ci + 1) * 512], 0, 512)]
                        else:
                            typ = "Bk"; dsts = [(KTB[(col - 2560) // 128, :, ci * 512:(ci + 1) * 512], 0, 512)]
                        pb = 2 + (half % 2)
                        for k in range(16):
                            cx.op("pe", lambda e: e.matmul(PS[pb][:, :], wv[:, k, half * 128:(half + 1) * 128], hTc[:, k, :],
                                                            start=(k == 0), stop=(k == 15)),
                                  reads=[rhTc, rw], writes=[rPS[pb]])
                        oi = obi % 3; obi += 1
                        if typ == "A":
                            cx.op("act", lambda e: e.activation(out=ob[oi][:], in_=PS[pb][:, :], func=AF.Copy),
                                  reads=[rPS[pb]], writes=[rob[oi]])
                        else:
                            gc = gqc if typ == "Bq" else gkc
                            cx.op("dve", lambda e: e.tensor_copy(qf[:], PS[pb][:, :]), reads=[rPS[pb]], writes=[rqf])
                            cx.op("pool", lambda e: e.tensor_tensor(out=sq[:], in0=qf[:], in1=qf[:], op=ALU.mult), reads=[rqf], writes=[rsq])
                            cx.op("pe", lambda e: e.matmul(PS[4][:, :], ones[:], sq[:], start=True, stop=True), reads=[rsq], writes=[rPS[4]])
                            cx.op("act", lambda e: e.activation(out=sd[:], in_=PS[4][:, :], func=AF.Sqrt, scale=1.0 / HD, bias=eps128[:, 0:1]),
                                  reads=[rPS[4]], writes=[rsd])
                            cx.op("dve", lambda e: e.reciprocal(rs_[:], sd[:]), reads=[rsd], writes=[rrs])
                            cx.op("dve", lambda e: e.scalar_tensor_tensor(out=qn[:], in0=qf[:], scalar=gc[:, 0:1], in1=rs_[:], op0=ALU.mult, op1=ALU.mult),
                                  reads=[rqf, rrs], writes=[rqn])
                            cx.op("pe", lambda e: e.matmul(PS[4][:, :], rotT[:], qn[:], start=True, stop=True), reads=[rqn], writes=[rPS[4]])
                            cx.op("pool", lambda e: e.tensor_tensor(out=t1[:], in0=qn[:], in1=cosT[:, ci * 512:(ci + 1) * 512], op=ALU.mult),
                                  reads=[rqn], writes=[rt1])
                            cx.op("dve", lambda e: e.tensor_tensor(out=t2[:], in0=PS[4][:, :], in1=sinT[:, ci * 512:(ci + 1) * 512], op=ALU.mult),
                                  reads=[rPS[4]], writes=[rt2])
                            cx.op("dve", lambda e: e.tensor_tensor(out=ob[oi][:], in0=t1[:], in1=t2[:], op=ALU.add),
                                  reads=[rt1, rt2], writes=[rob[oi]])
                        for (dap, c0_, c1_) in dsts:
                            cx.dma("pool", lambda e: e.dma_start(out=dap, in_=ob[oi][:, c0_:c1_]), reads=[rob[oi]])
            cx.barrier()

        with ExitStack() as st:
            biasA = sb(st, "biasA", [128, 24, 384])
            negMB = sb(st, "negMB", [128, 1])
            gqa = sb(st, "gqa", [128, 2]); gneg = sb(st, "gneg", [128, 2]); rowm = sb(st, "rowm", [2, 128]); mx = sb(st, "mx", [2, 1]); mprod = sb(st, "mprod", [1, 1])
            rK = Res("p2c")
            cx.dma("sp", lambda e: e.dma_start(out=biasA[:].rearrange("p h c -> p (h c)"), in_=c_bias[:, :]), writes=[rK])
            cx.dma("sp", lambda e: e.dma_start(out=gqa[:, 0:1], in_=gq_col[:, :]), writes=[rK])
            cx.dma("sp", lambda e: e.dma_start(out=gqa[:, 1:2], in_=gk_col[:, :]), writes=[rK])
            cx.op("dve", lambda e: e.tensor_scalar(out=gneg[:], in0=gqa[:], scalar1=-1.0, scalar2=None, op0=ALU.mult), reads=[rK], writes=[rK])
            cx.op("dve", lambda e: e.tensor_tensor(out=gqa[:], in0=gqa[:], in1=gneg[:], op=ALU.max), reads=[rK], writes=[rK])
            cx.op("pe", lambda e: e.transpose(PS[4][0:2, 0:128], gqa[:, 0:2], identf[:]), reads=[rK], writes=[rPS[4]])
            cx.op("dve", lambda e: e.tensor_copy(rowm[:], PS[4][0:2, 0:128]), reads=[rPS[4]], writes=[rK])
            cx.op("dve", lambda e: e.reduce_max(out=mx[:, 0:1], in_=rowm[:], axis=AX.X), reads=[rK], writes=[rK])
            cx.op("pe", lambda e: e.transpose(PS[4][0:1, 0:2], mx[0:2, 0:1], identf[0:2, 0:2]), reads=[rK], writes=[rPS[4]])
            cx.op("dve", lambda e: e.tensor_copy(rowm[0:1, 0:2], PS[4][0:1, 0:2]), reads=[rPS[4]], writes=[rK])
            cx.op("dve", lambda e: e.scalar_tensor_tensor(out=mprod[:], in0=rowm[0:1, 0:1], scalar=-SCALE * HD, in1=rowm[0:1, 1:2],
                                                          op0=ALU.mult, op1=ALU.mult), reads=[rK], writes=[rK])
            cx.op("pe", lambda e: e.matmul(PS[4][:, 0:1], ones[0:1, :], mprod[0:1, 0:1], start=True, stop=True), reads=[rK], writes=[rPS[4]])
            cx.op("dve", lambda e: e.tensor_copy(negMB[:], PS[4][:, 0:1]), reads=[rPS[4]], writes=[rK])
            cx.barrier()
            rK.const = True

            KT = sb(st, "KT", [128, S], BF16); rKT = Res("KT")
            Vt = sb(st, "Vt", [128, NT, 128], BF16); rVt = Res("Vt")
            QT = [sb(st, "QT%d" % i, [128, SH], BF16) for i in range(4)]
            rQT = [Res("QT%d" % i) for i in range(4)]
            sbt = [sb(st, "sbt%d" % i, [128, 384]) for i in range(2)]; rsbt = [Res("sbt%d" % i) for i in range(2)]
            Pb = [sb(st, "Pb%d" % i, [128, 512], BF16) for i in range(3)]; rPb = [Res("Pb%d" % i) for i in range(3)]
            PTs = [sb(st, "PTs%d" % i, [128, 512], BF16) for i in range(3)]; rPTs = [Res("PTs%d" % i) for i in range(3)]
            Og = [sb(st, "Og%d" % i, [128, 512], BF16) for i in range(2)]; rOg = [Res("Og%d" % i) for i in range(2)]
            sm = [sb(st, "smA%d" % i, [128, 16]) for i in range(4)]; rsm = [Res("smA%d" % i) for i in range(4)]
            tix = {"i": 0}

            for grp in ("A", "B"):
                for g in range(2):
                    if grp == "A":
                        cx.dma("sp", lambda e: e.dma_start(out=KT[:, 0:SA], in_=KTA[g, :, :]), writes=[rKT])
                        vsrc = VAs.ap().rearrange("(t p) c -> p t c", p=128)
                        for vq in range(3):
                            cx.dma("sp", lambda e: e.dma_start(out=Vt[:, vq * 6:(vq + 1) * 6, :], in_=vsrc[:, vq * 6:(vq + 1) * 6, g * 128:(g + 1) * 128]),
                                   writes=[rVt])
                    else:
                        cx.dma("sp", lambda e: e.dma_start(out=KT[:], in_=KTB[g, :, :]), writes=[rKT])
                        vsrc = VBs.ap().rearrange("(t p) c -> p t c", p=128)
                        for vq in range(4):
                            cx.dma("sp", lambda e: e.dma_start(out=Vt[:, vq * 8:(vq + 1) * 8, :], in_=vsrc[:, vq * 8:(vq + 1) * 8, g * 128:(g + 1) * 128]),
                                   writes=[rVt])
                    for hh in range(4):
                        qcc = (g * 4 + hh) if grp == "A" else (8 + g * 4 + hh)
                        cx.dma("sp", (lambda hh_, qcc_: (lambda e: e.dma_start(out=QT[hh_][:], in_=QTd[qcc_, :, :])))(hh, qcc), writes=[rQT[hh]])
                    tiles = []
                    for n in range(NTH):
                        for hh in range(4):
                            for kc in range(1 if grp == "A" else 8):
                                i = tix["i"]; tix["i"] += 1
                                nh = n * 4 + hh
                                tiles.append(dict(n=n, hh=hh, kc=kc, first=(kc == 0), last=(grp == "A" or kc == 7),
                                                  psi=i % 2, pi=i % 3, pti=i % 2, si=i % 2, smi=nh % 4, po=2 + (nh % 2), ogi=n % 2))

                    def s1(T):
                        n, hh, kc = T["n"], T["hh"], T["kc"]
                        h = g * 4 + hh
                        psi, pi, si = T["psi"], T["pi"], T["si"]
                        smt = sm[T["smi"]]; rsmt = rsm[T["smi"]]
                        if grp == "A":
                            W = 384
                            bi = 0 if n == 0 else (2 if n == NTH - 1 else 1)
                            cx.op("pe", lambda e: e.matmul(PS[psi][:, 0:W], QT[hh][:, n * 128:(n + 1) * 128], KT[:, n * 128:n * 128 + W],
                                                            start=True, stop=True), reads=[rQT[hh], rKT], writes=[rPS[psi]])
                            cx.op("dve", lambda e: e.scalar_tensor_tensor(out=sbt[si][:, 0:W], in0=PS[psi][:, 0:W], scalar=SCALE,
                                                                          in1=biasA[:, bi * 8 + h, :], op0=ALU.mult, op1=ALU.add),
                                  reads=[rPS[psi]], writes=[rsbt[si]])
                            cx.op("dve", lambda e: e.reduce_max(out=smt[:, 0:1], in_=sbt[si][:, 0:W], axis=AX.X), reads=[rsbt[si]], writes=[rsmt])
                            cx.op("dve", lambda e: e.tensor_tensor(out=smt[:, 1:2], in0=smt[:, 0:1], in1=sinkb[:, h:h + 1], op=ALU.max),
                                  reads=[rsmt], writes=[rsmt])
                            cx.op("dve", lambda e: e.tensor_scalar(out=smt[:, 2:3], in0=smt[:, 1:2], scalar1=-1.0, scalar2=None, op0=ALU.mult),
                                  reads=[rsmt], writes=[rsmt])
                            cx.op("act", lambda e: e.activation(out=Pb[pi][:, 0:W], in_=sbt[si][:, 0:W], func=AF.Exp, bias=smt[:, 2:3],
                                                                accum_out=smt[:, 3:4]), reads=[rsbt[si], rsmt], writes=[rPb[pi], rsmt])
                            cx.op("act", lambda e: e.activation(out=smt[:, 4:5], in_=sinkb[:, h:h + 1], func=AF.Exp, bias=smt[:, 2:3]),
                                  reads=[rsmt], writes=[rsmt])
                            cx.op("dve", lambda e: e.tensor_tensor(out=smt[:, 5:6], in0=smt[:, 3:4], in1=smt[:, 4:5], op=ALU.add),
                                  reads=[rsmt], writes=[rsmt])
                            cx.op("dve", lambda e: e.reciprocal(smt[:, 6:7], smt[:, 5:6]), reads=[rsmt], writes=[rsmt])
                        else:
                            cx.op("pe", lambda e: e.matmul(PS[psi][:, :], QT[hh][:, n * 128:(n + 1) * 128], KT[:, kc * 512:(kc + 1) * 512],
                                                            start=True, stop=True), reads=[rQT[hh], rKT], writes=[rPS[psi]])
                            wr_ = [rPb[pi]] + ([rsmt] if (T["first"] or T["last"]) else [])
                            cx.op("act", lambda e: e.activation(out=Pb[pi][:], in_=PS[psi][:, :], func=AF.Exp, bias=negMB[:, 0:1], scale=SCALE,
                                                                accum_out=smt[:, 8 + kc:9 + kc]), reads=[rPS[psi]], writes=wr_)

                    def s2(T):
                        pi, pti = T["pi"], T["pti"]
                        nj = 3 if grp == "A" else 4
                        W = nj * 128
                        for j in range(nj):
                            cx.op("pe", lambda e: e.transpose(PT[pti][:, j * 128:(j + 1) * 128], Pb[pi][:, j * 128:(j + 1) * 128], identb[:]),
                                  reads=[rPb[pi]], writes=[rPT[pti]])
                        cx.op("dve", lambda e: e.tensor_copy(PTs[pi][:, 0:W], PT[pti][:, 0:W]), reads=[rPT[pti]], writes=[rPTs[pi]])

                    def s3(T):
                        n, hh, kc = T["n"], T["hh"], T["kc"]
                        pi, po, ogi = T["pi"], T["po"], T["ogi"]
                        smt = sm[T["smi"]]; rsmt = rsm[T["smi"]]
                        nj = 3 if grp == "A" else 4
                        for j in range(nj):
                            vb_ = (n + j) if grp == "A" else (kc * 4 + j)
                            cx.op("pe", lambda e: e.matmul(PS[po][:, 0:128], PTs[pi][:, j * 128:(j + 1) * 128], Vt[:, vb_, :],
                                                            start=(T["first"] and j == 0), stop=(T["last"] and j == nj - 1)),
                                  reads=[rPTs[pi], rVt], writes=[rPS[po]])
                        if T["last"]:
                            if grp == "B":
                                cx.op("dve", lambda e: e.reduce_sum(out=smt[:, 5:6], in_=smt[:, 8:16], axis=AX.X), reads=[rsmt], writes=[rsmt])
                                cx.op("dve", lambda e: e.reciprocal(smt[:, 6:7], smt[:, 5:6]), reads=[rsmt], writes=[rsmt])
                            cx.op("act", lambda e: e.activation(out=Og[ogi][:, hh * 128:(hh + 1) * 128], in_=PS[po][:, 0:128], func=AF.Copy,
                                                                scale=smt[:, 6:7]), reads=[rPS[po], rsmt], writes=[rOg[ogi]])
                            if hh == 3:
                                c0 = (0 if grp == "A" else 1024) + g * 512
                                cx.dma("pool", lambda e: e.dma_start(out=OS[n * 128:(n + 1) * 128, c0:c0 + 512], in_=Og[ogi][:]), reads=[rOg[ogi]])

                    L = len(tiles)
                    for i in range(L + 2):
                        if i < L:
                            s1(tiles[i])
                        if 0 <= i - 1 < L:
                            s2(tiles[i - 1])
                        if 0 <= i - 2 < L:
                            s3(tiles[i - 2])
            cx.barrier()

        osv = OS.ap().rearrange("(t p) d -> t p d", p=128)
        yv = Yr.ap().rearrange("(t p) d -> t p d", p=128)
        h2hv = H2h.ap().rearrange("(t p) d -> t p d", p=128)
        affhv = AFFh.ap().rearrange("(t p) d -> t p d", p=128)
        with ExitStack() as st:
            gf = sb(st, "gf", [128, D])
            wr = sb(st, "wr", [128, 16, NE])
            rK = Res("p3c")
            cx.dma("sp", lambda e: e.dma_start(out=gf[:], in_=g_ffn[:, :]), writes=[rK])
            cx.dma("sp", lambda e: e.dma_start(out=wr[:], in_=w_router.ap().rearrange("(k p) c -> p k c", p=128)), writes=[rK])
            cx.barrier()
            rK.const = True
            x1 = [sb(st, "x1_%d" % i, [128, D]) for i in range(4)]; rx1 = [Res("x1_%d" % i) for i in range(4)]
            cx.dma("sp", lambda e: e.dma_start(out=x1[3][:], in_=c_zero[:, :]), writes=[rx1[3]])
            for tg in range(NTH, NT):
                cx.dma("pool", lambda e: e.dma_start(out=yv[tg], in_=x1[3][:]), reads=[rx1[3]])
            otb = [sb(st, "otb%d" % i, [128, D], BF16) for i in range(2)]; rotb = [Res("otb%d" % i) for i in range(2)]
            oT = sb(st, "oT", [128, 16, 512], BF16); roT = Res("oT")
            h2t = [sb(st, "h2t%d" % i, [128, D]) for i in range(2)]; rh2t = [Res("h2t%d" % i) for i in range(2)]
            h2b = [sb(st, "h2b%d" % i, [128, D], BF16) for i in range(2)]; rh2b = [Res("h2b%d" % i) for i in range(2)]
            aft = [sb(st, "aft%d" % i, [128, NE]) for i in range(2)]; raft = [Res("aft%d" % i) for i in range(2)]
            h2T = sb(st, "h2T", [128, 16, 128]); rh2T = Res("h2T")
            jk = sb(st, "jk", [128, D], BF16); rjk = Res("jk")
            ss = sb(st, "ss3", [128, 1]); std = sb(st, "std3", [128, 1]); rstd = sb(st, "rstd3", [128, 1]); rss = Res("ss3")
            sm3 = sb(st, "sm3", [128, 8]); rsm3 = Res("sm3")
            ex = sb(st, "ex", [128, NE]); rex = Res("ex")
            tcount = 0
            for tc in range(4):
                for t in range(4):
                    tg = tc * 4 + t
                    oi = tg % 2
                    cx.dma("sp", lambda e: e.dma_start(out=otb[oi][:], in_=osv[tg]), writes=[rotb[oi]])
                    cx.dma("sp", lambda e: e.dma_start(out=x1[t][:], in_=xov[tg]), writes=[rx1[t]])
                    for k4 in range(4):
                        pi = tcount % 2; tcount += 1
                        for kk in range(4):
                            k = k4 * 4 + kk
                            cx.op("pe", lambda e: e.transpose(PT[pi][:, kk * 128:(kk + 1) * 128], otb[oi][:, k * 128:(k + 1) * 128], identb[:]),
                                  reads=[rotb[oi]], writes=[rPT[pi]])
                        dst = oT[:, k4 * 4:(k4 + 1) * 4, t * 128:(t + 1) * 128]
                        srcp = PT[pi][:, :].rearrange("p (k c) -> p k c", k=4)
                        if pi == 1:
                            cx.op("dve", lambda e: e.tensor_copy(dst, srcp), reads=[rPT[pi]], writes=[roT])
                        else:
                            cx.op("act", lambda e: e.activation(out=dst, in_=srcp, func=AF.Copy), reads=[rPT[pi]], writes=[roT])
                ws = WStream([(w_out_v[:, :, cg * 256:(cg + 1) * 256], 16) for cg in range(8)])
                for cg in range(8):
                    wv, rw = ws.next()
                    for t in range(4):
                        pb = 2 + (t % 2)
                        for k in range(16):
                            cx.op("pe", lambda e: e.matmul(PS[pb][:, 0:256], oT[:, k, t * 128:(t + 1) * 128], wv[:, k, :], start=(k == 0), stop=(k == 15)),
                                  reads=[roT, rw], writes=[rPS[pb]])
                        cx.op("dve", lambda e: e.tensor_tensor(out=x1[t][:, cg * 256:(cg + 1) * 256], in0=PS[pb][:, 0:256],
                                                               in1=x1[t][:, cg * 256:(cg + 1) * 256], op=ALU.add),
                              reads=[rPS[pb], rx1[t]], writes=[rx1[t]])
                for t in range(4):
                    tg = tc * 4 + t
                    hi = tg % 2
                    cx.dma("pool", lambda e: e.dma_start(out=yv[tg], in_=x1[t][:]), reads=[rx1[t]])
                    rmsnorm_rstd((ss, rss, std, rstd), x1[t][:], rx1[t], jk[:], rjk)
                    cx.op("dve", lambda e: e.scalar_tensor_tensor(out=h2t[hi][:], in0=x1[t][:], scalar=rstd[:, 0:1], in1=gf[:],
                                                                  op0=ALU.mult, op1=ALU.mult), reads=[rx1[t], rss], writes=[rh2t[hi]])
                    cx.op("pool", lambda e: e.tensor_copy(h2b[hi][:], h2t[hi][:]), reads=[rh2t[hi]], writes=[rh2b[hi]])
                    cx.dma("pool", lambda e: e.dma_start(out=h2hv[tg], in_=h2b[hi][:]), reads=[rh2b[hi]])
                    for k4 in range(4):
                        pb = k4 % 2
                        for kk in range(4):
                            k = k4 * 4 + kk
                            cx.op("pe", lambda e: e.transpose(PS[pb][:, kk * 128:(kk + 1) * 128], h2t[hi][:, k * 128:(k + 1) * 128], identf[:]),
                                  reads=[rh2t[hi]], writes=[rPS[pb]])
                        dst = h2T[:, k4 * 4:(k4 + 1) * 4, :]
                        srcp = PS[pb][:, :].rearrange("p (k c) -> p k c", k=4)
                        if k4 % 2 == 0:
                            cx.op("dve", lambda e: e.tensor_copy(dst, srcp), reads=[rPS[pb]], writes=[rh2T])
                        else:
                            cx.op("act", lambda e: e.activation(out=dst, in_=srcp, func=AF.Copy), reads=[rPS[pb]], writes=[rh2T])
                    for k in range(16):
                        cx.op("pe", lambda e: e.matmul(PS[4][:, 0:NE], h2T[:, k, :], wr[:, k, :], start=(k == 0), stop=(k == 15)),
                              reads=[rh2T], writes=[rPS[4]])
                    cx.op("dve", lambda e: e.reduce_max(out=sm3[:, 0:1], in_=PS[4][:, 0:NE], axis=AX.X), reads=[rPS[4]], writes=[rsm3])
                    cx.op("dve", lambda e: e.tensor_scalar(out=sm3[:, 1:2], in0=sm3[:, 0:1], scalar1=-1.0, scalar2=None, op0=ALU.mult),
                          reads=[rsm3], writes=[rsm3])
                    cx.op("act", lambda e: e.activation(out=ex[:], in_=PS[4][:, 0:NE], func=AF.Exp, bias=sm3[:, 1:2], accum_out=sm3[:, 2:3]),
                          reads=[rPS[4], rsm3], writes=[rex, rsm3])
                    cx.op("dve", lambda e: e.reciprocal(sm3[:, 3:4], sm3[:, 2:3]), reads=[rsm3], writes=[rsm3])
                    cx.op("dve", lambda e: e.tensor_scalar(out=aft[hi][:], in0=ex[:], scalar1=sm3[:, 3:4], scalar2=None, op0=ALU.mult),
                          reads=[rex, rsm3], writes=[raft[hi]])
                    cx.dma("pool", lambda e: e.dma_start(out=affhv[tg], in_=aft[hi][:]), reads=[raft[hi]])
            cx.barrier()

        rX = Res("xchg")
        for k in range(4):
            cx.coll("cc%d" % k, lambda e: e.collective_compute("AllGather", ALU.bypass, replica_groups=PAIRS,
                                                               ins=[H2h[k * 512:(k + 1) * 512, :]], outs=[H2st[k].ap().opt()]),
                    writes=[rX])
        cx.coll("cc4", lambda e: e.collective_compute("AllGather", ALU.bypass, replica_groups=PAIRS,
                                                      ins=[AFFh.ap().opt()], outs=[AFFf.ap().opt()]), writes=[rX])
        cx.barrier()
        for k in range(4):
            for hf in range(2):
                r0 = hf * SH + k * 512
                cx.dma("sp", lambda e: e.dma_start(out=H2f[r0:r0 + 512, :], in_=H2st[k][hf * 512:(hf + 1) * 512, :]))
        cx.barrier()

        pmTok = sb(es, "pmTok", [128, NT, NEL])
        rpm = Res("pmTok")
        with ExitStack() as st:
            affAll = sb(st, "affAll", [128, NT, NE]); rAff = Res("affAll")
            sel16 = sb(st, "sel16", [NE, NEL])
            affT = sb(st, "affT", [NE, S]); raffT = Res("affT")
            junk = sb(st, "junk4", [NE, S]); rjunk = Res("junk4")
            onesr = sb(st, "onesr", [NE, S])
            thr = sb(st, "thr", [NE, 1], U32); cand = sb(st, "cand", [NE, 1], U32); selm = sb(st, "selm", [NE, 1], U32)
            cntt = sb(st, "cntt", [NE, 1])
            rT = Res("thr")
            afv = AFFf.ap().rearrange("(t p) c -> p t c", p=128)
            for vq in range(4):
                cx.dma("sp", lambda e: e.dma_start(out=affAll[:, vq * 8:(vq + 1) * 8, :], in_=afv[:, vq * 8:(vq + 1) * 8, :]), writes=[rAff])
            cx.dma("sp", lambda e: e.dma_start(out=sel16[:], in_=c_sel16[:, :]), writes=[rT])
            for tg in range(NT):
                pb = tg % 2
                cx.op("pe", lambda e: e.transpose(PS[pb][0:NE, 0:128], affAll[:, tg, :], identf[:]), reads=[rAff], writes=[rPS[pb]])
                cx.op("dve", lambda e: e.tensor_copy(affT[:, tg * 128:(tg + 1) * 128], PS[pb][0:NE, 0:128]), reads=[rPS[pb]], writes=[raffT])
            cx.op("dve", lambda e: e.memset(thr[:], 0), writes=[rT])
            cx.op("dve", lambda e: e.memset(onesr[:], 1.0), writes=[rT])
            for b in range(30, -1, -1):
                cx.op("dve", lambda e: e.tensor_scalar(out=cand[:], in0=thr[:], scalar1=(1 << b), scalar2=None, op0=ALU.bitwise_or),
                      reads=[rT], writes=[rT])
                cx.op("dve", lambda e: e.tensor_scalar(out=junk[:], in0=affT[:], scalar1=cand[:].bitcast(F32)[:, 0:1], scalar2=None,
                                                       op0=ALU.is_ge, op1=ALU.add, accum_out=cntt[:, 0:1]),
                      reads=[rT, raffT], writes=[rjunk, rT])
                cx.op("dve", lambda e: e.tensor_scalar(out=selm[:], in0=cntt[:], scalar1=float(CAP), scalar2=None, op0=ALU.is_ge),
                      reads=[rT], writes=[rT])
                cx.op("dve", lambda e: e.copy_predicated(out=thr[:], mask=selm[:], data=cand[:]), reads=[rT], writes=[rT])
            cx.op("dve", lambda e: e.tensor_scalar(out=junk[:], in0=affT[:], scalar1=thr[:].bitcast(F32)[:, 0:1], scalar2=None, op0=ALU.is_ge),
                  reads=[rT, raffT], writes=[rjunk])
            cx.op("dve", lambda e: e.tensor_tensor_scan(out=affT[:], data0=onesr[:], data1=junk[:], initial=0.0, op0=ALU.mult, op1=ALU.add),
                  reads=[rjunk, rT], writes=[raffT])
            cx.op("dve", lambda e: e.tensor_tensor(out=affT[:], in0=affT[:], in1=junk[:], op=ALU.mult), reads=[rjunk, raffT], writes=[raffT])
            cx.op("dve", lambda e: e.tensor_scalar(out=affT[:], in0=affT[:], scalar1=-1.0, scalar2=None, op0=ALU.add), reads=[raffT], writes=[raffT])
            for tg in range(NT):
                pb = tg % 2
                cx.op("pe", lambda e: e.matmul(PS[pb][:, 0:NEL], affT[:, tg * 128:(tg + 1) * 128], sel16[:, :], start=True, stop=True),
                      reads=[raffT, rT], writes=[rPS[pb]])
                cx.op("dve", lambda e: e.tensor_copy(pmTok[:, tg, :], PS[pb][:, 0:NEL]), reads=[rPS[pb]], writes=[rpm])
            cx.barrier()

        rY = Res("Y")
        rH2 = Res("H2", const=True)
        with ExitStack() as st:
            iot = sb(st, "iot", [128, CAP])
            tokid = sb(st, "tokid", [128, NT, 2], BF16)
            selB = sb(st, "selB", [128, NEL, NE])
            offc = sb(st, "offc", [128, 1])
            rK = Res("p5c")
            cx.dma("sp", lambda e: e.dma_start(out=iot[:], in_=c_iota[:, :]), writes=[rK])
            cx.dma("sp", lambda e: e.dma_start(out=tokid[:].rearrange("p t c -> p (t c)"), in_=c_tokid[:, :]), writes=[rK])
            cx.dma("sp", lambda e: e.dma_start(out=selB[:].rearrange("p j c -> p (j c)"), in_=c_selB[:, :]), writes=[rK])
            cx.dma("sp", lambda e: e.dma_start(out=offc[:], in_=c_off[:, :]), writes=[rK])
            cx.barrier()
            rK.const = True
            OH = [sb(st, "OH%d" % i, [128, CAP], BF16) for i in range(2)]; rOH = [Res("OH%d" % i) for i in range(2)]
            idr = sb(st, "idr", [2, CAP]); ridr = Res("idr")
            idf = sb(st, "idf", [128, 4]); idp = sb(st, "idp", [128, 8]); idl = sb(st, "idl", [128, 4]); idm = sb(st, "idm", [128, 4])
            ridf = Res("idf")
            idi = [sb(st, "idi%d" % i, [128, 4], I32) for i in range(2)]; ridi = [Res("idi%d" % i) for i in range(2)]
            idc = [sb(st, "idc%d" % i, [128, 4], I32) for i in range(2)]; ridc = [Res("idc%d" % i) for i in range(2)]
            gates = [sb(st, "gates%d" % i, [128, 4]) for i in range(2)]; rgates = [Res("gates%d" % i) for i in range(2)]
            xgb = [sb(st, "xgb%d" % i, [128, D], BF16) for i in range(4)]; rxgb = [Res("xgb%d" % i) for i in range(4)]
            afg = [sb(st, "afg%d" % i, [128, NE]) for i in range(4)]; rafg = [Res("afg%d" % i) for i in range(4)]
            gj = sb(st, "gj", [128, NE]); rgj = Res("gj")
            xgT = sb(st, "xgT", [128, 16, CAP], BF16); rxgT = Res("xgT")
            hTm = sb(st, "hTm", [128, 32, CAP], BF16); rhTm = Res("hTm")
            sg = [sb(st, "sg%d" % i, [128, CAP]) for i in range(4)]; rsg = [Res("sg%d" % i) for i in range(4)]
            eo = [sb(st, "eo%d" % i, [128, D]) for i in range(4)]; reo = [Res("eo%d" % i) for i in range(4)]
            cn = {"oh": 0, "tr": 0, "gu": 0}

            def prep_a(ex_):
                ei = ex_ % 2
                for tg in range(NT):
                    oi = cn["oh"] % 2; cn["oh"] += 1
                    cx.op("dve", lambda e: e.tensor_scalar(out=OH[oi][:], in0=iot[:], scalar1=pmTok[:, tg, ex_:ex_ + 1], scalar2=None, op0=ALU.is_equal),
                          reads=[rpm], writes=[rOH[oi]])
                    cx.op("pe", lambda e: e.matmul(PS[4][0:2, :], tokid[:, tg, :], OH[oi][:], start=(tg == 0), stop=(tg == NT - 1)),
                          reads=[rOH[oi]], writes=[rPS[4]])
                cx.op("dve", lambda e: e.tensor_copy(idr[:], PS[4][0:2, :]), reads=[rPS[4]], writes=[ridr])
                for c in range(4):
                    cx.op("pe", lambda e: e.transpose(PS[4][:, c * 2:(c + 1) * 2], idr[0:2, c * 128:(c + 1) * 128], identf[0:2, 0:2]),
                          reads=[ridr], writes=[rPS[4]])
                cx.op("dve", lambda e: e.tensor_copy(idp[:], PS[4][:, 0:8]), reads=[rPS[4]], writes=[ridf])
                pv = idp[:, :].rearrange("p (c two) -> p c two", two=2)
                cx.op("dve", lambda e: e.scalar_tensor_tensor(out=idf[:], in0=pv[:, :, 0], scalar=128.0, in1=pv[:, :, 1], op0=ALU.mult, op1=ALU.add),
                      reads=[ridf], writes=[ridf])
                cx.op("dve", lambda e: e.tensor_copy(idi[ei][:], idf[:]), reads=[ridf], writes=[ridi[ei]])
                cx.op("dve", lambda e: e.tensor_scalar(out=idl[:], in0=idf[:], scalar1=offc[:, 0:1], scalar2=None, op0=ALU.add), reads=[ridf], writes=[ridf])
                cx.op("dve", lambda e: e.tensor_scalar(out=idm[:], in0=idl[:], scalar1=float(S), scalar2=-float(S), op0=ALU.is_ge, op1=ALU.mult),
                      reads=[ridf], writes=[ridf])
                cx.op("dve", lambda e: e.tensor_tensor(out=idl[:], in0=idl[:], in1=idm[:], op=ALU.add), reads=[ridf], writes=[ridf])
                cx.op("dve", lambda e: e.tensor_copy(idc[ei][:], idl[:]), reads=[ridf], writes=[ridc[ei]])
                for c in range(4):
                    cx.dma("pool", lambda e: e.indirect_dma_start(out=xgb[c][:], out_offset=None, in_=H2f[:, :],
                                                                  in_offset=bass.IndirectOffsetOnAxis(ap=idi[ei][:, c:c + 1], axis=0)),
                           reads=[ridi[ei], rH2], writes=[rxgb[c]])
                    cx.dma("pool", lambda e: e.indirect_dma_start(out=afg[c][:], out_offset=None, in_=AFFf[:, :],
                                                                  in_offset=bass.IndirectOffsetOnAxis(ap=idi[ei][:, c:c + 1], axis=0)),
                           reads=[ridi[ei], rH2], writes=[rafg[c]])
                    cx.op("dve", lambda e: e.scalar_tensor_tensor(out=gj[:], in0=afg[c][:], scalar=1.0, in1=selB[:, ex_, :], op0=ALU.mult, op1=ALU.mult,
                                                                  accum_out=gates[ei][:, c:c + 1]),
                          reads=[rafg[c]], writes=[rgj, rgates[ei]])

            def prep_b(ex_):
                for c in range(4):
                    for k4 in range(4):
                        pi = cn["tr"] % 2; cn["tr"] += 1
                        for kk in range(4):
                            k = k4 * 4 + kk
                            cx.op("pe", lambda e: e.transpose(PT[pi][:, kk * 128:(kk + 1) * 128], xgb[c][:, k * 128:(k + 1) * 128], identb[:]),
                                  reads=[rxgb[c]], writes=[rPT[pi]])
                        dst = xgT[:, k4 * 4:(k4 + 1) * 4, c * 128:(c + 1) * 128]
                        srcp = PT[pi][:, :].rearrange("p (k c) -> p k c", k=4)
                        if pi == 1:
                            cx.op("dve", lambda e: e.tensor_copy(dst, srcp), reads=[rPT[pi]], writes=[rxgT])
                        else:
                            cx.op("act", lambda e: e.activation(out=dst, in_=srcp, func=AF.Copy), reads=[rPT[pi]], writes=[rxgT])

            specs = []
            for ex_ in range(NEL):
                wg_v = w_gate.ap()[ex_].rearrange("(k p) c -> p k c", p=128)
                wu_v = w_up.ap()[ex_].rearrange("(k p) c -> p k c", p=128)
                wd_v = w_down.ap()[ex_].rearrange("(k p) c -> p k c", p=128)
                for f4 in range(8):
                    for wv_ in (wg_v, wu_v):
                        for kh in range(2):
                            specs.append((wv_[:, kh * 8:(kh + 1) * 8, f4 * 512:(f4 + 1) * 512], 8))
                for c4 in range(4):
                    for q in range(4):
                        specs.append((wd_v[:, q * 8:(q + 1) * 8, c4 * 512:(c4 + 1) * 512], 8))
            ws = WStream(specs)
            prep_a(0)
            prep_b(0)
            for ex_ in range(NEL):
                ei = ex_ % 2
                if ex_ + 1 < NEL:
                    prep_a(ex_ + 1)
                for f4 in range(8):
                    for which in ("g", "u"):
                        banks = [(cn["gu"] + j) % 6 for j in range(4)]
                        cn["gu"] = (cn["gu"] + 4) % 6
                        for kh in range(2):
                            wv, rw = ws.next()
                            for j in range(4):
                                pb = banks[j]
                                for k8 in range(8):
                                    cx.op("pe", lambda e: e.matmul(PS[pb][:, :], wv[:, k8, j * 128:(j + 1) * 128], xgT[:, kh * 8 + k8, :],
                                                                    start=(kh == 0 and k8 == 0), stop=(kh == 1 and k8 == 7)),
                                          reads=[rw, rxgT], writes=[rPS[pb]])
                        for j in range(4):
                            pb = banks[j]
                            if which == "g":
                                cx.op("act", lambda e: e.activation(out=sg[j][:], in_=PS[pb][:, :], func=AF.Silu), reads=[rPS[pb]], writes=[rsg[j]])
                            else:
                                cx.op("dve", lambda e: e.tensor_tensor(out=hTm[:, f4 * 4 + j, :], in0=sg[j][:], in1=PS[pb][:, :], op=ALU.mult),
                                      reads=[rsg[j], rPS[pb]], writes=[rhTm])
                if ex_ + 1 < NEL:
                    prep_b(ex_ + 1)
                for c4 in range(4):
                    for q in range(4):
                        wdv, rwd = ws.next()
                        for f8 in range(8):
                            f = q * 8 + f8
                            for t in range(4):
                                pb = [4, 5, 2, 3][t]
                                cx.op("pe", lambda e: e.matmul(PS[pb][:, :], hTm[:, f, t * 128:(t + 1) * 128], wdv[:, f8, :],
                                                                start=(f == 0), stop=(f == 31)), reads=[rhTm, rwd], writes=[rPS[pb]])
                    for t in range(4):
                        pb = [4, 5, 2, 3][t]
                        if t % 2 == 0:
                            cx.op("act", lambda e: e.activation(out=eo[t][:, c4 * 512:(c4 + 1) * 512], in_=PS[pb][:, :], func=AF.Copy, scale=gates[ei][:, t:t + 1]),
                                  reads=[rPS[pb], rgates[ei]], writes=[reo[t]])
                        else:
                            cx.op("dve", lambda e: e.tensor_scalar(out=eo[t][:, c4 * 512:(c4 + 1) * 512], in0=PS[pb][:, :], scalar1=gates[ei][:, t:t + 1],
                                                                   scalar2=None, op0=ALU.mult), reads=[rPS[pb], rgates[ei]], writes=[reo[t]])
                for c in range(4):
                    cx.dma("pool", lambda e: e.indirect_dma_start(out=Yr[:, :], out_offset=bass.IndirectOffsetOnAxis(ap=idc[ei][:, c:c + 1], axis=0),
                                                                  in_=eo[c][:], in_offset=None, compute_op=ALU.add),
                           reads=[reo[c], ridc[ei]], writes=[rY])
            cx.barrier()

        for k in range(8):
            cx.coll("cc%d" % (5 + k), lambda e: e.collective_compute("AllGather", ALU.bypass, replica_groups=PAIRS,
                                                                    ins=[Yr[SH + k * 256:SH + (k + 1) * 256, :]], outs=[Gst[k].ap().opt()]),
                    writes=[rX])
        cx.barrier()

        outv = out.ap().rearrange("(t p) d -> t p d", p=128)
        with ExitStack() as st:
            gfi = sb(st, "gfi", [128, D])
            s01 = sb(st, "s01", [128, 2])
            rK = Res("p6c")
            cx.dma("sp", lambda e: e.dma_start(out=gfi[:], in_=g_fin[:, :]), writes=[rK])
            cx.dma("sp", lambda e: e.dma_start(out=s01[:], in_=c_s01[:, :]), writes=[rK])
            cx.barrier()
            rK.const = True
            yt = [sb(st, "yt%d" % i, [128, D]) for i in range(2)]; ryt = [Res("yt%d" % i) for i in range(2)]
            ga = [sb(st, "ga%d" % i, [128, D]) for i in range(2)]; rga = [Res("ga%d" % i) for i in range(2)]
            gb = [sb(st, "gb%d" % i, [128, D]) for i in range(2)]; rgb = [Res("gb%d" % i) for i in range(2)]
            ot = [sb(st, "ot%d" % i, [128, D]) for i in range(2)]; rot = [Res("ot%d" % i) for i in range(2)]
            ss = sb(st, "ss6", [128, 1]); std = sb(st, "std6", [128, 1]); rstd = sb(st, "rstd6", [128, 1]); rss = Res("ss6")
            for tg in range(NTH):
                i = tg % 2
                k = tg // 2; sub = tg % 2
                cx.dma("sp", lambda e: e.dma_start(out=yt[i][:], in_=yv[tg]), writes=[ryt[i]])
                cx.dma("sp", lambda e: e.dma_start(out=ga[i][:], in_=Gst[k][sub * 128:(sub + 1) * 128, :]), writes=[rga[i]])
                cx.dma("sp", lambda e: e.dma_start(out=gb[i][:], in_=Gst[k][256 + sub * 128:256 + (sub + 1) * 128, :]), writes=[rgb[i]])
                cx.op("dve", lambda e: e.scalar_tensor_tensor(out=yt[i][:], in0=ga[i][:], scalar=s01[:, 0:1], in1=yt[i][:], op0=ALU.mult, op1=ALU.add),
                      reads=[rga[i], ryt[i]], writes=[ryt[i]])
                cx.op("dve", lambda e: e.scalar_tensor_tensor(out=yt[i][:], in0=gb[i][:], scalar=s01[:, 1:2], in1=yt[i][:], op0=ALU.mult, op1=ALU.add),
                      reads=[rgb[i], ryt[i]], writes=[ryt[i]])
                rmsnorm_rstd((ss, rss, std, rstd), yt[i][:], ryt[i], ot[i][:], rot[i])
                cx.op("dve", lambda e: e.scalar_tensor_tensor(out=ot[i][:], in0=yt[i][:], scalar=rstd[:, 0:1], in1=gfi[:], op0=ALU.mult, op1=ALU.mult),
                      reads=[ryt[i], rss], writes=[rot[i]])
                cx.dma("pool", lambda e: e.dma_start(out=outv[tg], in_=ot[i][:]), reads=[rot[i]])
            cx.barrier()
    return nc


def _consts():
    bf = ml_dtypes.bfloat16
    c = {}
    c["c_identb"] = np.eye(128, dtype=np.float32).astype(bf)
    c["c_identf"] = np.eye(128, dtype=np.float32)
    rotT = np.zeros((128, 128), np.float32)
    for base in (0, 64):
        for i in range(32):
            m = base + i
            rotT[m + 32, m] = -1.0
            rotT[m, m + 32] = 1.0
    c["c_rotT"] = rotT
    c["c_ones"] = np.ones((128, 128), np.float32)
    c["c_iota"] = np.ascontiguousarray(np.broadcast_to(np.arange(CAP, dtype=np.float32)[None, :], (128, CAP)))
    tok = np.zeros((128, NT, 2), np.float32)
    tok[:, :, 0] = np.arange(NT)[None, :]
    tok[:, :, 1] = np.arange(128)[:, None]
    c["c_tokid"] = np.ascontiguousarray(tok.reshape(128, NT * 2)).astype(bf)
    c["c_zero"] = np.zeros((128, D), np.float32)
    return c


def _core_consts(r):
    c = {}
    half = 64
    inv_freq = (10000.0 ** (-np.arange(0, half, 2, dtype=np.float32) / half)).astype(np.float32)
    own = np.arange(r * SH, (r + 1) * SH)
    oth = np.arange((1 - r) * SH, (2 - r) * SH)
    pos = np.concatenate([own, oth])
    row = (pos // 64).astype(np.float32)
    colp = (pos % 64).astype(np.float32)
    ang_r = (row[None, :] * inv_freq[:, None]).astype(np.float32)
    ang_c = (colp[None, :] * inv_freq[:, None]).astype(np.float32)
    c["c_cos"] = np.ascontiguousarray(np.concatenate([np.cos(ang_r), np.cos(ang_r), np.cos(ang_c), np.cos(ang_c)], 0).astype(np.float32))
    c["c_sin"] = np.ascontiguousarray(np.concatenate([np.sin(ang_r), np.sin(ang_r), np.sin(ang_c), np.sin(ang_c)], 0).astype(np.float32))
    slopes = (2.0 ** (-8.0 * np.arange(1, 9) / 8)).astype(np.float32)
    qi = np.arange(128)[:, None]
    kj = np.arange(384)[None, :]
    dist = np.abs(qi + 128 - kj).astype(np.float32)
    mid = np.where((dist <= 128)[:, None, :], -slopes[None, :, None] * dist[:, None, :], np.float32(-1e30)).astype(np.float32)
    first = mid.copy()
    last = mid.copy()
    if r == 0:
        first[:, :, 0:128] = -1e30
    else:
        last[:, :, 256:384] = -1e30
    c["c_bias"] = np.ascontiguousarray(np.stack([first, mid, last], 1).reshape(128, 3 * 8 * 384))
    sel = np.zeros((NE, NEL), np.float32)
    for j in range(NEL):
        sel[r * NEL + j, j] = 1.0
    c["c_sel16"] = sel
    c["c_selB"] = np.ascontiguousarray(np.broadcast_to(sel.T.reshape(1, NEL * NE), (128, NEL * NE)))
    c["c_off"] = np.full((128, 1), float(r * SH), np.float32)
    s01 = np.zeros((128, 2), np.float32)
    s01[:, 1 - r] = 1.0
    c["c_s01"] = s01
    return c


_NC_CACHE = {}


def kernel(x, norm_mix, w_in, sink_a, q_norm_b, k_norm_b, w_out, norm_ffn,
           w_router, w_gate, w_up, w_down, norm_final):
    f = lambda a: np.ascontiguousarray(np.asarray(a, dtype=np.float32))
    x = f(x)
    B = x.shape[0]
    if "nc" not in _NC_CACHE:
        _NC_CACHE["nc"] = build_nc()
    nc = _NC_CACHE["nc"]
    bc = lambda v, n: np.ascontiguousarray(np.broadcast_to(f(v).reshape(1, n), (128, n)))
    wg, wu, wd = f(w_gate)[0], f(w_up)[0], f(w_down)[0]
    shared = dict(
        w_in=f(w_in)[0], w_out=f(w_out)[0], w_router=f(w_router)[0],
        g_mix=bc(norm_mix, D), g_ffn=bc(norm_ffn, D), g_fin=bc(norm_final, D),
        sink_b=bc(sink_a, 8),
        gq_col=np.ascontiguousarray(f(q_norm_b).reshape(128, 1)),
        gk_col=np.ascontiguousarray(f(k_norm_b).reshape(128, 1)),
    )
    shared.update(_consts())
    cc = [_core_consts(0), _core_consts(1)]
    in_maps = []
    for c in range(N_CORES):
        b, r = c // 2, c % 2
        m = dict(shared)
        m.update(cc[r])
        m["x_own"] = x[b, r * SH:(r + 1) * SH]
        m["x_oth"] = x[b, (1 - r) * SH:(2 - r) * SH]
        m["w_gate"] = wg[r * NEL:(r + 1) * NEL]
        m["w_up"] = wu[r * NEL:(r + 1) * NEL]
        m["w_down"] = wd[r * NEL:(r + 1) * NEL]
        in_maps.append(m)
    res = run_bass_kernel_spmd(nc, in_maps, core_ids=list(range(N_CORES)))
    o = np.empty((B, S, D), np.float32)
    for c in range(N_CORES):
        b, r = c // 2, c % 2
        o[b, r * SH:(r + 1) * SH] = np.asarray(res.results[c]["out"], dtype=np.float32)
    return o
```
